# Optimizing a Trainium2 kernel written in Bass

```python
import math
import jax, jax.numpy as jnp
from jax import lax
import numpy as np

D_MODEL = 1024
BATCH = 16
SEQ = 2048
DEPTH = 2

A_GROUPS = 4
A_GROUP_DIM = 128
A_WIDTH = A_GROUPS * A_GROUP_DIM
A_CHUNK = 128
MLA_HEADS = 8
QK_NOPE = 64
QK_ROPE = 32
QK_HEAD = QK_NOPE + QK_ROPE
V_HEAD = 64
Q_LORA = 384
KV_LORA = 256
ROPE_THETA = 10000.0
Q_BLOCK = 128
MAX_POS_OFFSET = 4096
AB_IN = 2 * A_WIDTH + Q_LORA + KV_LORA + QK_ROPE
AB_OUT = A_WIDTH + MLA_HEADS * V_HEAD
S5_GROUP_DIM = 16
S5_GROUPS = D_MODEL // S5_GROUP_DIM
S5_STATE = 64
C_WIDTH = S5_GROUPS * S5_GROUP_DIM
S5_MIN_STEP = 1e-3
S5_MAX_STEP = 1e-1
D_FF = 2816
N_EXPERTS = 8
TOP_K = 2
D_EXPERT = 3584
LN_EPS = 1e-5
RMS_EPS = 1e-6
DEEPNORM_ALPHA = (2.0 * DEPTH) ** 0.25
DEEPNORM_BETA = (8.0 * DEPTH) ** -0.25

kernel_name = 'hybrid_gmlp_mla_s5_moe_deepnorm'


def layernorm(x, g, b):
    xf = x.astype(jnp.float32)
    mu = jnp.mean(xf, axis=-1, keepdims=True)
    var = jnp.mean(jnp.square(xf - mu), axis=-1, keepdims=True)
    return ((xf - mu) * lax.rsqrt(var + LN_EPS)).astype(x.dtype) * g + b


def rmsnorm(x, g):
    xf = x.astype(jnp.float32)
    ms = jnp.mean(jnp.square(xf), axis=-1, keepdims=True)
    return (xf * lax.rsqrt(ms + RMS_EPS)).astype(x.dtype) * g


def rope_cos_sin(positions, dtype):
    inv_freq = 1.0 / (ROPE_THETA ** (jnp.arange(0, QK_ROPE, 2, dtype=jnp.float32) / QK_ROPE))
    ang = positions.astype(jnp.float32)[..., None] * inv_freq
    return jnp.cos(ang).astype(dtype), jnp.sin(ang).astype(dtype)


def apply_rope(x, cos, sin):
    half = QK_ROPE // 2
    x1, x2 = x[..., :half], x[..., half:]
    return jnp.concatenate([x1 * cos - x2 * sin, x1 * sin + x2 * cos], axis=-1)


def gmlp_spatial_gating(z, ln_g, ln_b, w_s, b_s):
    bsz, seq, _ = z.shape
    u, v = jnp.split(z, 2, axis=-1)
    v = layernorm(v, ln_g, ln_b)
    v = v.reshape(bsz, seq // A_CHUNK, A_CHUNK, A_GROUPS, A_GROUP_DIM)
    w = jnp.tril(w_s)
    mixed = jnp.einsum('hts,bnshc->bnthc', w, v) + b_s.T[:, :, None]
    return u * mixed.reshape(bsz, seq, A_WIDTH)


def mla_attention(c_q, c_kv, k_pe, positions, q_norm, w_uq, kv_norm, w_ukv):
    bsz, seq, _ = c_q.shape
    q = (rmsnorm(c_q, q_norm) @ w_uq).reshape(bsz, seq, MLA_HEADS, QK_HEAD)
    q_nope, q_pe = q[..., :QK_NOPE], q[..., QK_NOPE:]
    kv = (rmsnorm(c_kv, kv_norm) @ w_ukv).reshape(bsz, seq, MLA_HEADS, QK_NOPE + V_HEAD)
    k_nope, v = kv[..., :QK_NOPE], kv[..., QK_NOPE:]
    cos, sin = rope_cos_sin(positions, c_q.dtype)
    q_pe = apply_rope(q_pe, cos[:, :, None, :], sin[:, :, None, :])
    k_pe = apply_rope(k_pe, cos, sin)
    q = jnp.concatenate([q_nope, q_pe], axis=-1)
    k = jnp.concatenate([k_nope, jnp.broadcast_to(k_pe[:, :, None, :], (bsz, seq, MLA_HEADS, QK_ROPE))], axis=-1)
    scale = QK_HEAD ** -0.5
    n_blocks = seq // Q_BLOCK
    q_blocks = q.reshape(bsz, n_blocks, Q_BLOCK, MLA_HEADS, QK_HEAD).transpose(1, 0, 2, 3, 4)
    starts = jnp.arange(n_blocks, dtype=jnp.int32) * Q_BLOCK
    key_pos = jnp.arange(seq, dtype=jnp.int32)

    def attend(args):
        qb, start = args
        s = jnp.einsum('bqhd,bkhd->bhqk', qb, k).astype(jnp.float32) * scale
        q_pos = start + jnp.arange(Q_BLOCK, dtype=jnp.int32)
        mask = key_pos[None, :] <= q_pos[:, None]
        s = jnp.where(mask, s, jnp.finfo(jnp.float32).min)
        p = jax.nn.softmax(s, axis=-1).astype(v.dtype)
        return jnp.einsum('bhqk,bkhd->bqhd', p, v)

    out = lax.map(attend, (q_blocks, starts))
    return out.transpose(1, 0, 2, 3, 4).reshape(bsz, seq, MLA_HEADS * V_HEAD)


def ab_mixer(x, positions, w_in, gm_ln_g, gm_ln_b, w_s, b_s, q_norm, w_uq, kv_norm, w_ukv, w_out):
    h = x @ w_in
    a_uv, c_q, c_kv, k_pe = jnp.split(h, [2 * A_WIDTH, 2 * A_WIDTH + Q_LORA, 2 * A_WIDTH + Q_LORA + KV_LORA], axis=-1)
    y_a = gmlp_spatial_gating(jax.nn.gelu(a_uv), gm_ln_g, gm_ln_b, w_s, b_s)
    y_b = mla_attention(c_q, c_kv, k_pe, positions, q_norm, w_uq, kv_norm, w_ukv)
    return jnp.concatenate([y_a, y_b], axis=-1) @ w_out


def _ssm_combine(e1, e2):
    a1r, a1i, b1r, b1i = e1
    a2r, a2i, b2r, b2i = e2
    return (a2r * a1r - a2i * a1i,
            a2r * a1i + a2i * a1r,
            a2r * b1r - a2i * b1i + b2r,
            a2r * b1i + a2i * b1r + b2i)


def s5_mixer(x, w_in, a_re, a_im, log_step, b_re, b_im, c_re, c_im, d_skip, glu_w, glu_b, w_out):
    bsz, seq, _ = x.shape
    f32 = jnp.float32
    u = (x @ w_in).astype(f32)
    ug = u.reshape(bsz, seq, S5_GROUPS, S5_GROUP_DIM)
    a_re, a_im = a_re.astype(f32), a_im.astype(f32)
    delta = jnp.exp(log_step.astype(f32))[:, None]
    mag = jnp.exp(delta * a_re)
    abar_re = mag * jnp.cos(delta * a_im)
    abar_im = mag * jnp.sin(delta * a_im)
    den = a_re * a_re + a_im * a_im
    coef_re = ((abar_re - 1.0) * a_re + abar_im * a_im) / den
    coef_im = (abar_im * a_re - (abar_re - 1.0) * a_im) / den
    br, bi = b_re.astype(f32), b_im.astype(f32)
    bb_re = coef_re[..., None] * br - coef_im[..., None] * bi
    bb_im = coef_re[..., None] * bi + coef_im[..., None] * br
    bu_re = jnp.einsum('bsgc,gpc->sbgp', ug, bb_re)
    bu_im = jnp.einsum('bsgc,gpc->sbgp', ug, bb_im)
    ar = jnp.broadcast_to(abar_re[None, None], (seq, 1, S5_GROUPS, S5_STATE))
    ai = jnp.broadcast_to(abar_im[None, None], (seq, 1, S5_GROUPS, S5_STATE))
    _, _, h_re, h_im = lax.associative_scan(_ssm_combine, (ar, ai, bu_re, bu_im), axis=0)
    y = (jnp.einsum('sbgp,gcp->bsgc', h_re, c_re.astype(f32))
         - jnp.einsum('sbgp,gcp->bsgc', h_im, c_im.astype(f32)))
    y = (y.reshape(bsz, seq, C_WIDTH) + d_skip.astype(f32) * u).astype(x.dtype)
    g = jax.nn.gelu(y)
    z = g * jax.nn.sigmoid(g @ glu_w + glu_b)
    return z @ w_out


def swiglu(x, w_gate, w_up, w_down):
    return (jax.nn.silu(x @ w_gate) * (x @ w_up)) @ w_down


def moe_swiglu(x, router, w_gate, w_up, w_down):
    bsz, seq, d = x.shape
    xt = x.reshape(bsz * seq, d)
    logits = (xt @ router).astype(jnp.float32)
    top_vals, top_idx = lax.top_k(logits, TOP_K)
    gates = jax.nn.softmax(top_vals, axis=-1)
    combine = jnp.sum(gates[..., None] * jax.nn.one_hot(top_idx, N_EXPERTS, dtype=jnp.float32), axis=1).astype(x.dtype)
    out = jnp.zeros_like(xt)
    for e in range(N_EXPERTS):
        out = out + combine[:, e:e + 1] * swiglu(xt, w_gate[e], w_up[e], w_down[e])
    return out.reshape(bsz, seq, d)


def setup_inputs(seed: int = 0) -> dict:
    key = jax.random.key(seed)
    keys = iter(jax.random.split(key, 64))
    ne = (DEPTH + 1) // 2
    no = DEPTH // 2

    def nrm(shape, scale):
        return jax.random.normal(next(keys), shape, jnp.float32) * scale

    def gain(shape):
        return 1.0 + nrm(shape, 0.02)

    x = jax.random.normal(next(keys), (BATCH, SEQ, D_MODEL), jnp.float32)
    offsets = jax.random.randint(next(keys), (BATCH, 1), 0, MAX_POS_OFFSET, dtype=jnp.int32)
    positions = (offsets + jnp.arange(SEQ, dtype=jnp.int32)[None, :]).astype(jnp.int32)
    log_lo, log_hi = math.log(S5_MIN_STEP), math.log(S5_MAX_STEP)
    a_im_init = math.pi * jnp.arange(S5_STATE, dtype=jnp.float32)
    return {
        'x': x,
        'positions': positions,
        'ab_w_in': nrm((ne, D_MODEL, AB_IN), D_MODEL ** -0.5),
        'gm_ln_g': gain((ne, A_WIDTH)),
        'gm_ln_b': nrm((ne, A_WIDTH), 0.02),
        'gm_w_s': nrm((ne, A_GROUPS, A_CHUNK, A_CHUNK), A_CHUNK ** -0.5),
        'gm_b_s': gain((ne, A_GROUPS, A_CHUNK)),
        'mla_q_norm': gain((ne, Q_LORA)),
        'mla_w_uq': nrm((ne, Q_LORA, MLA_HEADS * QK_HEAD), Q_LORA ** -0.5),
        'mla_kv_norm': gain((ne, KV_LORA)),
        'mla_w_ukv': nrm((ne, KV_LORA, MLA_HEADS * (QK_NOPE + V_HEAD)), KV_LORA ** -0.5),
        'ab_w_out': nrm((ne, AB_OUT, D_MODEL), AB_OUT ** -0.5 * DEEPNORM_BETA),
        'ab_ln_g': gain((ne, D_MODEL)),
        'ab_ln_b': nrm((ne, D_MODEL), 0.02),
        'ffd_w_gate': nrm((ne, D_MODEL, D_FF), D_MODEL ** -0.5),
        'ffd_w_up': nrm((ne, D_MODEL, D_FF), D_MODEL ** -0.5),
        'ffd_w_down': nrm((ne, D_FF, D_MODEL), D_FF ** -0.5 * DEEPNORM_BETA),
        'ffd_ln_g': gain((ne, D_MODEL)),
        'ffd_ln_b': nrm((ne, D_MODEL), 0.02),
        'c_w_in': nrm((no, D_MODEL, C_WIDTH), D_MODEL ** -0.5),
        's5_a_re': -0.5 * (1.0 + nrm((no, S5_GROUPS, S5_STATE), 0.02)),
        's5_a_im': a_im_init[None, None, :] + nrm((no, S5_GROUPS, S5_STATE), 0.01),
        's5_log_step': jax.random.uniform(next(keys), (no, S5_GROUPS), jnp.float32, log_lo, log_hi),
        's5_b_re': nrm((no, S5_GROUPS, S5_STATE, S5_GROUP_DIM), (2.0 * S5_GROUP_DIM) ** -0.5),
        's5_b_im': nrm((no, S5_GROUPS, S5_STATE, S5_GROUP_DIM), (2.0 * S5_GROUP_DIM) ** -0.5),
        's5_c_re': nrm((no, S5_GROUPS, S5_GROUP_DIM, S5_STATE), (2.0 * S5_STATE) ** -0.5),
        's5_c_im': nrm((no, S5_GROUPS, S5_GROUP_DIM, S5_STATE), (2.0 * S5_STATE) ** -0.5),
        's5_d': nrm((no, C_WIDTH), 1.0),
        'glu_w': nrm((no, C_WIDTH, C_WIDTH), C_WIDTH ** -0.5),
        'glu_b': nrm((no, C_WIDTH), 0.02),
        'c_w_out': nrm((no, C_WIDTH, D_MODEL), C_WIDTH ** -0.5 * DEEPNORM_BETA),
        'c_ln_g': gain((no, D_MODEL)),
        'c_ln_b': nrm((no, D_MODEL), 0.02),
        'moe_router': nrm((no, D_MODEL, N_EXPERTS), D_MODEL ** -0.5),
        'moe_w_gate': nrm((no, N_EXPERTS, D_MODEL, D_EXPERT), D_MODEL ** -0.5),
        'moe_w_up': nrm((no, N_EXPERTS, D_MODEL, D_EXPERT), D_MODEL ** -0.5),
        'moe_w_down': nrm((no, N_EXPERTS, D_EXPERT, D_MODEL), D_EXPERT ** -0.5 * DEEPNORM_BETA),
        'moe_ln_g': gain((no, D_MODEL)),
        'moe_ln_b': nrm((no, D_MODEL), 0.02),
    }


def reference(x, positions, ab_w_in, gm_ln_g, gm_ln_b, gm_w_s, gm_b_s, mla_q_norm, mla_w_uq, mla_kv_norm, mla_w_ukv, ab_w_out, ab_ln_g, ab_ln_b, ffd_w_gate, ffd_w_up, ffd_w_down, ffd_ln_g, ffd_ln_b, c_w_in, s5_a_re, s5_a_im, s5_log_step, s5_b_re, s5_b_im, s5_c_re, s5_c_im, s5_d, glu_w, glu_b, c_w_out, c_ln_g, c_ln_b, moe_router, moe_w_gate, moe_w_up, moe_w_down, moe_ln_g, moe_ln_b):
    for i in range(DEPTH):
        j = i // 2
        if i % 2 == 0:
            mix = ab_mixer(x, positions, ab_w_in[j], gm_ln_g[j], gm_ln_b[j], gm_w_s[j], gm_b_s[j],
                           mla_q_norm[j], mla_w_uq[j], mla_kv_norm[j], mla_w_ukv[j], ab_w_out[j])
            x = layernorm(DEEPNORM_ALPHA * x + mix, ab_ln_g[j], ab_ln_b[j])
            ffn = swiglu(x, ffd_w_gate[j], ffd_w_up[j], ffd_w_down[j])
            x = layernorm(DEEPNORM_ALPHA * x + ffn, ffd_ln_g[j], ffd_ln_b[j])
        else:
            mix = s5_mixer(x, c_w_in[j], s5_a_re[j], s5_a_im[j], s5_log_step[j], s5_b_re[j], s5_b_im[j],
                           s5_c_re[j], s5_c_im[j], s5_d[j], glu_w[j], glu_b[j], c_w_out[j])
            x = layernorm(DEEPNORM_ALPHA * x + mix, c_ln_g[j], c_ln_b[j])
            ffn = moe_swiglu(x, moe_router[j], moe_w_gate[j], moe_w_up[j], moe_w_down[j])
            x = layernorm(DEEPNORM_ALPHA * x + ffn, moe_ln_g[j], moe_ln_b[j])
    return x
```

```python
import math
import numpy as np
from contextlib import ExitStack
import concourse.bass as bass
import concourse.mybir as mybir
from concourse.bass_utils import run_bass_kernel_spmd

F32 = mybir.dt.float32
BF16 = mybir.dt.bfloat16
I32 = mybir.dt.int32
ALU = mybir.AluOpType
AF = mybir.ActivationFunctionType

N_CORES = 8
SEQ = 2048
NB = 2
TOK = NB * SEQ
D = 1024
KT = 8
ALPHA = 4.0 ** 0.25
LN_EPS = 1e-5
RMS_EPS = 1e-6
D_FF = 2816
N_EXP = 8
D_EXP = 3584
EPOCH = 12000
MAGIC = 12582912.0
TWO_PI = 2.0 * math.pi
C1 = 6.28125
C2 = TWO_PI - 6.28125
PI_SAFE = 3.141592


class Buf:
    __slots__ = ("name", "writers", "readers", "sem", "cnt", "dram", "excl", "prev")

    def __init__(self, name="", dram=False, excl=False):
        self.name = name
        self.dram = dram
        self.excl = excl
        self.prev = set()
        self.writers = []
        self.readers = []
        self.sem = None
        self.cnt = 0


class T:
    __slots__ = ("buf", "ap")

    def __init__(self, buf, ap):
        self.buf = buf
        self.ap = ap

    def __getitem__(self, idx):
        return T(self.buf, self.ap[idx])

    def v(self, ap):
        return T(self.buf, ap)

    def bc(self, shape):
        return T(self.buf, self.ap.to_broadcast(list(shape)))


class Op:
    __slots__ = ("eng", "fn", "deps", "dma", "tok", "signal", "idx", "wbuf")


class Sched:
    ENGS = ("pe", "act", "dve", "pool", "sp")

    def __init__(self, nc, stack):
        self.nc = nc
        self.stack = stack
        self.ops = []
        self.nsem = 0
        self.pending = {e: set() for e in self.ENGS}
        self.last_on = {}
        self.dma_since = {}

    def new_sem(self, name):
        self.nsem += 1
        return self.stack.enter_context(self.nc.semaphore(f"{name}_{self.nsem}"))

    def add(self, eng, fn, reads=(), writes=(), dma=False, join=False):
        op = Op()
        op.eng = eng
        op.fn = fn
        op.dma = dma
        op.idx = len(self.ops)
        op.signal = False
        op.tok = None
        op.wbuf = None
        deps = set()
        cz = self._compress
        for t in reads:
            deps.update(cz(t.buf.writers))
            if t.buf.excl:
                deps.update(cz([r for r in t.buf.readers if self.ops[r].eng != eng]))
        for t in writes:
            b = t.buf
            if not join:
                deps.update(cz(b.writers))
            else:
                deps.update(b.prev)
            deps.update(cz(b.readers))
        for t in reads:
            t.buf.readers.append(op.idx)
        for t in writes:
            b = t.buf
            if join:
                if b.readers:
                    b.prev = b.prev | cz(b.readers)
                b.writers.append(op.idx)
            else:
                b.prev = cz(b.writers) | cz(b.readers)
                b.writers = [op.idx]
            b.readers = []
        if dma:
            op.wbuf = writes[0].buf
            if writes[0].buf.dram and not reads[0].buf.dram:
                op.wbuf = reads[0].buf
            self.dma_since[id(op.wbuf)] = op.idx
        else:
            self.last_on[eng] = op.idx
        if self.pending[eng]:
            deps.update(self.pending[eng])
            self.pending[eng] = set()
        deps.discard(op.idx)
        op.deps = deps
        self.ops.append(op)
        return op

    def _compress(self, lst):
        n = len(self.ops)
        if len(lst) <= 1:
            return set(i for i in lst if i < n)
        out = set()
        last = {}
        for i in lst:
            if i >= n:
                continue
            o = self.ops[i]
            if o.dma:
                out.add(i)
            else:
                if last.get(o.eng, -1) < i:
                    last[o.eng] = i
        out.update(last.values())
        return out

    def barrier(self):
        deps = set(self.last_on.values()) | set(self.dma_since.values())
        for e in self.ENGS:
            self.pending[e] = set(deps) | self.pending[e]
        self.dma_since = {}

    def emit(self):
        nc = self.nc
        ops = self.ops
        for op in ops:
            for d in op.deps:
                p = ops[d]
                if p.dma:
                    continue
                if p.eng == "pe" and op.eng == "pe" and not op.dma:
                    continue
                p.signal = True
        eng_cnt = {e: 0 for e in self.ENGS}
        eng_sems = {e: [] for e in self.ENGS}
        for op in ops:
            if op.dma:
                b = op.wbuf
                if b.sem is None:
                    b.sem = self.new_sem("d")
                b.cnt += 16
                op.tok = (b.sem, b.cnt)
            elif op.signal:
                c = eng_cnt[op.eng]
                ep = c // EPOCH
                if ep >= len(eng_sems[op.eng]):
                    eng_sems[op.eng].append(self.new_sem("e" + op.eng))
                op.tok = (eng_sems[op.eng][ep], c % EPOCH + 1)
                eng_cnt[op.eng] = c + 1
        streams = {e: [] for e in self.ENGS}
        for op in ops:
            streams[op.eng].append(op)

        def run_stream(eng_name):
            def body(e):
                waited = {}
                for op in streams[eng_name]:
                    need = {}
                    for d in op.deps:
                        p = ops[d]
                        if (not p.dma) and p.eng == "pe" and eng_name == "pe" and not op.dma:
                            continue
                        s, v = p.tok
                        k = id(s)
                        if waited.get(k, 0) >= v:
                            continue
                        if k not in need or need[k][1] < v:
                            need[k] = (s, v)
                    for k, (s, v) in need.items():
                        e.wait_ge(s, v)
                        waited[k] = v
                    ins = op.fn(e)
                    if op.tok is not None:
                        ins.then_inc(op.tok[0], 16 if op.dma else 1)
            return body

        with nc.Block() as block:
            block.sync(run_stream("sp"))
            block.tensor(run_stream("pe"))
            block.vector(run_stream("dve"))
            block.scalar(run_stream("act"))
            block.gpsimd(run_stream("pool"))

    def dma(self, out, in_, q="sp", join=False, **kw):
        return self.add(q, lambda e: e.dma_start(out=out.ap, in_=in_.ap, **kw),
                        reads=[in_], writes=[out], dma=True, join=join)

    def mm(self, out, lhsT, rhs, start=True, stop=True, join=None, **kw):
        if join is None:
            join = not start
        return self.add("pe", lambda e: e.matmul(out.ap, lhsT.ap, rhs.ap, start=start, stop=stop, **kw),
                        reads=[lhsT, rhs], writes=[out], join=join)

    def transpose(self, out, in_, ident, join=False):
        return self.add("pe", lambda e: e.transpose(out.ap, in_.ap, ident.ap),
                        reads=[in_, ident], writes=[out], join=join)

    def act(self, out, in_, func, bias=None, scale=None, accum=None, join=False):
        reads = [in_]
        kw = {}
        if bias is not None:
            if isinstance(bias, T):
                reads.append(bias)
                kw["bias"] = bias.ap
            else:
                kw["bias"] = bias
        if scale is not None:
            if isinstance(scale, T):
                reads.append(scale)
                kw["scale"] = scale.ap
            else:
                kw["scale"] = scale
        writes = [out]
        if accum is not None:
            writes.append(accum)
            kw["accum_out"] = accum.ap
        return self.add("act", lambda e: e.activation(out.ap, in_.ap, func, **kw),
                        reads=reads, writes=writes, join=join)

    def tt(self, out, a, b, op, eng="dve", join=False):
        return self.add(eng, lambda e: e.tensor_tensor(out.ap, a.ap, b.ap, op),
                        reads=[a, b], writes=[out], join=join)

    def ts(self, out, a, s1, op0, s2=None, op1=None, eng="dve", join=False):
        reads = [a]
        v1, v2 = s1, s2
        if isinstance(s1, T):
            reads.append(s1)
            v1 = s1.ap
        if isinstance(s2, T):
            reads.append(s2)
            v2 = s2.ap
        if op1 is None:
            return self.add(eng, lambda e: e.tensor_scalar(out.ap, a.ap, v1, None, op0),
                            reads=reads, writes=[out], join=join)
        return self.add(eng, lambda e: e.tensor_scalar(out.ap, a.ap, v1, v2, op0, op1),
                        reads=reads, writes=[out], join=join)

    def stt(self, out, a, s, b, op0, op1, eng="dve", join=False):
        reads = [a, b]
        v = s
        if isinstance(s, T):
            reads.append(s)
            v = s.ap
        return self.add(eng, lambda e: e.scalar_tensor_tensor(out.ap, a.ap, v, b.ap, op0, op1),
                        reads=reads, writes=[out], join=join)

    def copy(self, out, in_, eng="dve", join=False):
        if eng == "act":
            return self.add("act", lambda e: e.copy(out.ap, in_.ap), reads=[in_], writes=[out], join=join)
        return self.add(eng, lambda e: e.tensor_copy(out.ap, in_.ap), reads=[in_], writes=[out], join=join)

    def memset(self, out, val, eng="dve", join=False):
        return self.add(eng, lambda e: e.memset(out.ap, val), reads=[], writes=[out], join=join)

    def recip(self, out, in_, join=False):
        return self.add("dve", lambda e: e.reciprocal(out.ap, in_.ap), reads=[in_], writes=[out], join=join)

    def wait_all(self, eng, bufs):
        return self.add(eng, lambda e: e.nop(), reads=bufs, writes=[])


def _dsize(dt):
    return 2 if dt == BF16 else 4


class Arena:
    def __init__(self, S, nwords):
        self.S = S
        self.nwords = nwords
        self.t = S.stack.enter_context(S.nc.sbuf_tensor("arena", [128, nwords], F32))
        self.off = 0
        self.peak = 0

    def alloc(self, name, shape, dtype=F32):
        p = shape[0]
        free = 1
        for s in shape[1:]:
            free *= s
        words = (free * _dsize(dtype) + 3) // 4
        words = (words + 7) // 8 * 8
        assert self.off + words <= self.nwords, f"arena overflow at {name}: {self.off}+{words}>{self.nwords}"
        ap = self.t[0:p, self.off:self.off + words]
        self.off += words
        self.peak = max(self.peak, self.off)
        if dtype != F32:
            ap = ap.bitcast(dtype)
        ap = ap[:, 0:free]
        if len(shape) == 3:
            ap = ap.rearrange("p (a b) -> p a b", a=shape[1])
        elif len(shape) == 4:
            ap = ap.rearrange("p (a b c) -> p a b c", a=shape[1], b=shape[2])
        return T(Buf(name), ap)

    def ring(self, name, n, shape, dtype=F32):
        return Ring([self.alloc(f"{name}{i}", shape, dtype) for i in range(n)])

    def mark(self):
        return self.off

    def reset(self, m):
        self.off = m


class Ring:
    def __init__(self, items):
        self.items = items
        self.i = 0

    def next(self):
        t = self.items[self.i % len(self.items)]
        self.i += 1
        return t


class Prog:
    def __init__(self, debug=False, upto="D"):
        self.debug = debug
        self.upto = upto
        self.nc = bass.Bass("TRN2", target_bir_lowering=False)
        self.inputs = {}

    def din(self, name, shape, dtype=F32):
        t = self.nc.dram_tensor(name, list(shape), dtype, kind="ExternalInput")
        self.inputs[name] = (tuple(shape), dtype)
        return T(Buf(name, dram=True), t.ap())

    def dscr(self, name, shape, dtype=F32):
        kind = "ExternalOutput" if self.debug else "Internal"
        t = self.nc.dram_tensor(name, list(shape), dtype, kind=kind)
        return T(Buf(name, dram=True), t.ap())

    def load_xT(self, src, tok0, ntiles, xT, xtok_ring, ps_ring, ident, keep=None, xTf_cb=None):
        S = self.S
        for i in range(ntiles):
            xt = xtok_ring.next() if keep is None else keep[i]
            S.dma(xt, src[tok0 + i * 128: tok0 + (i + 1) * 128, :], q="sp")
            for half in range(2):
                ps = ps_ring.next()
                for k4 in range(4):
                    kt = half * 4 + k4
                    S.transpose(ps[:, k4 * 128:(k4 + 1) * 128], xt[:, kt * 128:(kt + 1) * 128], ident,
                                join=(k4 > 0))
                dst = xT[:, half * 4:half * 4 + 4, i * 128:(i + 1) * 128]
                srcv = ps.v(ps.ap.rearrange("p (a b) -> p a b", a=4))
                ev = "dve" if (i + half) % 2 == 0 else "act"
                S.copy(dst, srcv, eng=ev)
                if xTf_cb is not None:
                    xTf_cb(i, half, srcv, ev)

    def ln_epilogue(self, zsrc_a, zsrc_b, xres, g_b, b_b, z, st, mv, sd, rstd, nmr, zn, xo, dst, eps_col):
        S = self.S
        for hf, zs in enumerate((zsrc_a, zsrc_b)):
            S.stt(z[:, hf * 512:(hf + 1) * 512], xres[:, hf * 512:(hf + 1) * 512], ALPHA, zs,
                  ALU.mult, ALU.add, join=(hf == 1))
        for hf in range(2):
            S.add("dve", (lambda hf: lambda e: e.bn_stats(st.ap[:, hf, :], z.ap[:, hf * 512:(hf + 1) * 512]))(hf),
                  reads=[z], writes=[st], join=(hf == 1))
        S.add("dve", lambda e: e.bn_aggr(mv.ap, st.ap.rearrange("p a b -> p (a b)")), reads=[st], writes=[mv])
        S.act(sd, mv[:, 1:2], AF.Sqrt, bias=eps_col, scale=1.0)
        S.recip(rstd, sd)
        S.stt(nmr, mv[:, 0:1], -1.0, rstd, ALU.mult, ALU.mult)
        S.act(zn, z, AF.Identity, bias=nmr, scale=rstd)
        S.tt(zn, zn, g_b, ALU.mult, eng="pool")
        S.tt(xo, zn, b_b, ALU.add, eng="pool")
        S.dma(dst, xo, q="sp", join=True)

    def build(self):
        nc = self.nc
        with ExitStack() as stack:
            S = Sched(nc, stack)
            self.S = S
            A = Arena(S, 50176)
            self.A = A
            psb = [T(Buf(f"ps{i}", excl=True), stack.enter_context(nc.psum_tensor(f"ps{i}", [128, 512], F32))[:, :])
                   for i in range(8)]
            self.psb = psb
            self.declare_io()
            self.load_consts()
            order = ["A1", "A2", "B", "Ca", "Cb", "Cc", "D"]
            stages = {"A1": self.stage_A1, "A2": self.stage_A2, "B": self.stage_B, "Ca": self.stage_Ca,
                      "Cb": self.stage_Cb, "Cc": self.stage_Cc, "D": self.stage_D}
            base = A.mark()
            if getattr(self, "only", None):
                order = [self.only]
                self.x3_d = self.x
            for name in order:
                A.reset(base)
                stages[name]()
                S.barrier()
                if name == self.upto:
                    break
            if self.upto != "D":
                pass
            finals = [self.out] + (self.scratch_list if self.debug else [])
            S.wait_all("sp", finals)
            S.emit()
        return nc

    def declare_io(self):
        din = self.din
        self.x = din("x", [TOK, D])
        self.pos = din("positions", [NB, SEQ], I32)
        self.cst = din("cst", [128, 1024])
        self.scanmask = din("scanmask", [128, 512])
        self.w_in_main = din("w_in_main", [D, 1664])
        self.w_kpe2 = din("w_kpe2", [D, 192])
        self.gm_ln_g = din("gm_ln_g_b", [128, 512])
        self.gm_ln_b = din("gm_ln_b_b", [128, 512])
        self.wsT = din("wsT", [128, 4, 128])
        self.bs_b = din("bs_b", [128, 4, 512])
        self.qnorm = din("qnorm_c", [128, 3])
        self.kvnorm = din("kvnorm_c", [128, 2])
        self.w_uq = din("w_uq", [384, 768])
        self.w_uq_sw = din("w_uq_sw", [384, 768])
        self.w_uk = din("w_uk", [256, 512])
        self.w_uv = din("w_uv", [256, 512])
        self.w_out_a = din("w_out_a", [512, D])
        self.w_out_b = din("w_out_b", [512, D])
        self.ab_ln_g = din("ab_ln_g_b", [128, D])
        self.ab_ln_b = din("ab_ln_b_b", [128, D])
        self.ffd_wg = din("ffd_w_gate", [D, D_FF])
        self.ffd_wu = din("ffd_w_up", [D, D_FF])
        self.ffd_wd = din("ffd_w_down", [D_FF, D])
        self.ffd_ln_g = din("ffd_ln_g_b", [128, D])
        self.ffd_ln_b = din("ffd_ln_b_b", [128, D])
        self.c_w_in = din("c_w_in", [D, D])
        self.s5_are = din("s5_are_sm", [128, 32])
        self.s5_aim = din("s5_aim_sm", [128, 32])
        self.s5_ls = din("s5_ls_sm", [128, 32])
        self.s5_BT = din("s5_BT", [2, 8, 128, 4, 128])
        self.s5_CT = din("s5_CT", [2, 8, 128, 4, 128])
        self.s5_d = din("s5_d_c", [128, 8])
        self.glu_w = din("glu_w", [D, D])
        self.glu_b = din("glu_b_c", [128, 8])
        self.c_w_out = din("c_w_out", [D, D])
        self.c_ln_g = din("c_ln_g_b", [128, D])
        self.c_ln_b = din("c_ln_b_b", [128, D])
        if self.upto == "D":
            self.router = din("moe_router_c", [128, KT, N_EXP])
            self.moe_wg = din("moe_w_gate", [N_EXP, D, D_EXP])
            self.moe_wu = din("moe_w_up", [N_EXP, D, D_EXP])
            self.moe_wd = din("moe_w_down", [N_EXP, D_EXP, D])
            self.moe_ln_g = din("moe_ln_g_b", [128, D])
            self.moe_ln_b = din("moe_ln_b_b", [128, D])
        self.out = T(Buf("out", dram=True), nc_out(self.nc, "out", [TOK, D], F32))
        ds = self.dscr
        self.qT_d = ds("qT_d", [8, 96, TOK], BF16)
        self.kT_d = ds("kT_d", [8, 96, TOK], BF16)
        self.V_d = ds("V_d", [TOK, 1024], BF16)
        self.yaT_d = ds("yaT_d", [512, TOK], BF16)
        self.x1_d = ds("x1_d", [TOK, D])
        self.x2_d = ds("x2_d", [TOK, D])
        self.uT_d = ds("uT_d", [D, TOK])
        self.gT_d = ds("gT_d", [D, TOK], BF16)
        self.x3_d = ds("x3_d", [TOK, D])
        self.scratch_list = [self.qT_d, self.kT_d, self.V_d, self.yaT_d, self.x1_d, self.x2_d,
                             self.uT_d, self.gT_d, self.x3_d]

    def load_consts(self):
        S, A = self.S, self.A
        self.cstS = A.alloc("cstS", [128, 1024])
        S.dma(self.cstS, self.cst, q="sp")
        c = self.cstS
        self.ident = c[:, 0:128]
        self.tril = c[:, 128:256]
        self.masknegf = c[:, 256:384]
        self.ropef = c[0:96, 384:385]
        self.ropesign = c[0:96, 385:386]
        self.halfpi = c[:, 386:387]
        self.eps_ln = c[:, 387:388]
        self.eps_rms = c[:, 388:389]
        self.jgrid = c[:, 512:640]
        self.ident_bf = A.alloc("ident_bf", [128, 128], BF16)
        S.copy(self.ident_bf, self.ident)
        self.maskneg = A.alloc("maskneg", [128, 128], BF16)
        S.copy(self.maskneg, self.masknegf)
        self.ones_f = A.alloc("ones_f", [128, 128])
        S.memset(self.ones_f, 1.0)
        self.ones_bf = A.alloc("ones_bf", [128, 128], BF16)
        S.memset(self.ones_bf, 1.0)

    def stage_A1(self):
        S, A, psb = self.S, self.A, self.psb
        w_in_b = A.alloc("w_in_b", [128, KT, 1664], BF16)
        wm = self.w_in_main.ap.rearrange("(kt p) n -> p kt n", p=128)
        for kt0 in range(0, KT, 2):
            S.dma(w_in_b[:, kt0:kt0 + 2, :], self.w_in_main.v(wm[:, kt0:kt0 + 2, :]), q="pool", join=(kt0 > 0))
        w_kpe = A.alloc("w_kpe", [128, KT, 192], BF16)
        S.dma(w_kpe, self.w_kpe2.v(self.w_kpe2.ap.rearrange("(kt p) n -> p kt n", p=128)), q="pool")
        w_uq = A.alloc("w_uq_b", [128, 3, 768], BF16)
        S.dma(w_uq, self.w_uq.v(self.w_uq.ap.rearrange("(kt p) n -> p kt n", p=128)), q="pool")
        w_uqs = A.alloc("w_uqs_b", [128, 3, 768], BF16)
        S.dma(w_uqs, self.w_uq_sw.v(self.w_uq_sw.ap.rearrange("(kt p) n -> p kt n", p=128)), q="pool")
        w_uk = A.alloc("w_uk_b", [128, 2, 512], BF16)
        S.dma(w_uk, self.w_uk.v(self.w_uk.ap.rearrange("(kt p) n -> p kt n", p=128)), q="pool")
        w_uv = A.alloc("w_uv_b", [128, 2, 512], BF16)
        S.dma(w_uv, self.w_uv.v(self.w_uv.ap.rearrange("(kt p) n -> p kt n", p=128)), q="pool")
        gmg = A.alloc("gmg", [128, 512])
        S.dma(gmg, self.gm_ln_g, q="sp")
        gmb = A.alloc("gmb", [128, 512])
        S.dma(gmb, self.gm_ln_b, q="sp")
        bsb = A.alloc("bsb", [128, 4, 512])
        S.dma(bsb, self.bs_b, q="sp")
        wsf = A.alloc("wsf", [128, 4, 128])
        S.dma(wsf, self.wsT, q="sp")
        wsb = A.alloc("wsb", [128, 4, 128], BF16)
        for h in range(4):
            S.tt(wsb[:, h, :], wsf[:, h, :], self.tril, ALU.mult, join=(h > 0))
        qn = A.alloc("qn", [128, 3])
        S.dma(qn, self.qnorm, q="sp")
        kvn = A.alloc("kvn", [128, 2])
        S.dma(kvn, self.kvnorm, q="sp")

        xtok = A.ring("xtok", 2, [128, D])
        xT = A.alloc("xT", [128, KT, 512], BF16)
        uT = A.alloc("uT", [128, 4, 512], BF16)
        scr = A.ring("scr", 8, [128, 512])
        vnr = A.ring("vn", 5, [128, 512], BF16)
        small = A.ring("small", 8, [128, 8])
        yaT = A.alloc("yaT", [128, 4, 512], BF16)
        cT = A.alloc("cT", [128, 5, 512])
        Rq = A.alloc("Rq", [128, 512])
        Rkv = A.alloc("Rkv", [128, 512])
        cqn = A.alloc("cqn", [128, 5, 512], BF16)
        posi = A.alloc("posi", [96, 512], I32)
        Ctab = A.alloc("Ctab", [96, 512])
        Stab = A.alloc("Stab", [96, 512])
        qT = A.alloc("qT", [96, 8, 512], BF16)
        kTt = A.alloc("kTt", [96, 8, 512], BF16)
        kper = A.alloc("kper", [96, 512])
        Vt = A.ring("Vt", 2, [128, 8, 128], BF16)
        for _vt in Vt.items:
            S.memset(_vt, 1.0, eng="pool")
        gen = Ring([psb[0], psb[1], psb[6], psb[7]])
        ps_ya = psb[2:6]

        for c in range(TOK // 512):
            tok0 = c * 512
            b = c // 4
            s0 = (c % 4) * 512
            self.load_xT(self.x, tok0, 4, xT, xtok, gen, self.ident)
            for h in range(4):
                ps = gen.next()
                for kt in range(KT):
                    S.mm(ps, w_in_b[:, kt, h * 128:(h + 1) * 128], xT[:, kt, :], start=(kt == 0), stop=(kt == KT - 1))
                S.act(uT[:, h, :], ps, AF.Gelu_apprx_tanh, join=(h > 0))
            vns = []
            for i in range(4):
                ps = gen.next()
                for kt in range(KT):
                    S.mm(ps, xT[:, kt, i * 128:(i + 1) * 128], w_in_b[:, kt, 512:1024], start=(kt == 0), stop=(kt == KT - 1))
                v = scr.next()
                S.act(v, ps, AF.Gelu_apprx_tanh)
                sm = small.next()
                st = sm[:, 0:6]
                S.add("dve", (lambda st, v: lambda e: e.bn_stats(st.ap, v.ap))(st, v), reads=[v], writes=[sm])
                sm2 = small.next()
                mv = sm2[:, 0:2]
                S.add("dve", (lambda mv, st: lambda e: e.bn_aggr(mv.ap, st.ap))(mv, st), reads=[sm], writes=[sm2])
                S.act(sm2[:, 2:3], sm2[:, 1:2], AF.Sqrt, bias=self.eps_ln, scale=1.0, join=True)
                S.recip(sm2[:, 3:4], sm2[:, 2:3], join=True)
                S.stt(sm2[:, 4:5], sm2[:, 0:1], -1.0, sm2[:, 3:4], ALU.mult, ALU.mult, join=True)
                vn0 = scr.next()
                S.act(vn0, v, AF.Identity, bias=sm2[:, 4:5], scale=sm2[:, 3:4])
                S.tt(vn0, vn0, gmg, ALU.mult, eng="pool")
                vn = vnr.next()
                S.tt(vn, vn0, gmb, ALU.add, eng="pool")
                vns.append(vn)
            ps_sq = ps_ya[0]
            ps_skv = ps_ya[1]
            for m in range(5):
                ps = gen.next()
                for kt in range(KT):
                    S.mm(ps, w_in_b[:, kt, 1024 + m * 128:1024 + (m + 1) * 128], xT[:, kt, :],
                         start=(kt == 0), stop=(kt == KT - 1))
                S.copy(cT[:, m, :], ps, eng="act", join=(m > 0))
                sq = scr.next()
                S.act(sq, ps, AF.Square)
                if m < 3:
                    S.mm(ps_sq, self.ones_f, sq, start=(m == 0), stop=(m == 2))
                else:
                    S.mm(ps_skv, self.ones_f, sq, start=(m == 3), stop=(m == 4))
            t = scr.next()
            S.act(t, ps_sq, AF.Sqrt, bias=self.eps_rms, scale=1.0 / 384.0)
            S.recip(Rq, t)
            t = scr.next()
            S.act(t, ps_skv, AF.Sqrt, bias=self.eps_rms, scale=1.0 / 256.0)
            S.recip(Rkv, t)
            for m in range(5):
                g = qn[:, m:m + 1] if m < 3 else kvn[:, m - 3:m - 2]
                S.stt(cqn[:, m, :], cT[:, m, :], g, Rq if m < 3 else Rkv, ALU.mult, ALU.mult, join=(m > 0))
            S.dma(posi, self.pos.v(self.pos.ap[b:b + 1, s0:s0 + 512].to_broadcast([96, 512])), q="sp")
            ang = scr.next()[0:96, :]
            S.copy(ang, posi)
            S.ts(ang, ang, self.ropef, ALU.mult)
            kk = scr.next()[0:96, :]
            S.ts(kk, ang, 1.0 / TWO_PI, ALU.mult, MAGIC, ALU.add)
            S.ts(kk, kk, MAGIC, ALU.subtract)
            r1 = scr.next()[0:96, :]
            S.stt(r1, kk, -C1, ang, ALU.mult, ALU.add)
            S.stt(r1, kk, -C2, r1, ALU.mult, ALU.add)
            S.ts(r1, r1, -PI_SAFE, ALU.max, PI_SAFE, ALU.min)
            S.act(Stab, r1, AF.Sin, scale=self.ropesign)
            S.stt(r1, r1, -1.0, r1, ALU.mult, ALU.max)
            S.act(Ctab, r1, AF.Sin, bias=self.halfpi[0:96, :], scale=-1.0)
            for h in range(8):
                ps = gen.next()
                ps2 = gen.next()
                for kt in range(3):
                    S.mm(ps[0:96, :], w_uq[:, kt, h * 96:(h + 1) * 96], cqn[:, kt, :], start=(kt == 0), stop=(kt == 2))
                for kt in range(3):
                    S.mm(ps2[0:96, :], w_uqs[:, kt, h * 96:(h + 1) * 96], cqn[:, kt, :], start=(kt == 0), stop=(kt == 2))
                t1 = scr.next()[0:96, :]
                S.tt(t1, ps[0:96, :], Ctab, ALU.mult)
                t2 = scr.next()[0:96, :]
                S.tt(t2, ps2[0:96, :], Stab, ALU.mult)
                S.tt(qT[:, h, :], t1, t2, ALU.add, eng="pool", join=(h > 0))
            S.dma(self.qT_d.v(self.qT_d.ap.rearrange("h d t -> d h t")[:, :, tok0:tok0 + 512]), qT, q="sp", join=True)
            ps = gen.next()
            ps2 = gen.next()
            for kt in range(KT):
                S.mm(ps[0:96, :], w_kpe[:, kt, 0:96], xT[:, kt, :], start=(kt == 0), stop=(kt == KT - 1))
            for kt in range(KT):
                S.mm(ps2[0:96, :], w_kpe[:, kt, 96:192], xT[:, kt, :], start=(kt == 0), stop=(kt == KT - 1))
            t1 = scr.next()[0:96, :]
            S.tt(t1, ps[0:96, :], Ctab, ALU.mult)
            t2 = scr.next()[0:96, :]
            S.tt(t2, ps2[0:96, :], Stab, ALU.mult)
            S.tt(kper, t1, t2, ALU.add, eng="pool")
            for h in range(8):
                ps = gen.next()
                for kt in range(2):
                    S.mm(ps[0:64, :], w_uk[:, kt, h * 64:(h + 1) * 64], cqn[:, 3 + kt, :], start=(kt == 0), stop=(kt == 1))
                S.copy(kTt[0:64, h, :], ps[0:64, :], eng="act", join=(h > 0))
                S.copy(kTt[64:96, h, :], kper[64:96, :], eng="pool", join=True)
            S.dma(self.kT_d.v(self.kT_d.ap.rearrange("h d t -> d h t")[:, :, tok0:tok0 + 512]), kTt, q="sp", join=True)
            for i in range(4):
                for h in range(4):
                    S.mm(ps_ya[h][:, i * 128:(i + 1) * 128], vns[i][:, h * 128:(h + 1) * 128], wsb[:, h, :],
                         start=True, stop=True, join=(i > 0))
            for h in range(4):
                t = scr.next()
                S.tt(t, ps_ya[h], bsb[:, h, :], ALU.add)
                S.tt(yaT[:, h, :], t, uT[:, h, :], ALU.mult, eng="pool", join=(h > 0))
            S.dma(self.yaT_d.v(self.yaT_d.ap.rearrange("(h p) t -> p h t", p=128)[:, :, tok0:tok0 + 512]), yaT,
                  q="sp", join=True)
            for i in range(4):
                ps = gen.next()
                for kt in range(2):
                    S.mm(ps, cqn[:, 3 + kt, i * 128:(i + 1) * 128], w_uv[:, kt, :], start=(kt == 0), stop=(kt == 1))
                vt = Vt.next()
                S.copy(vt[:, :, 0:64], ps.v(ps.ap.rearrange("p (h d) -> p h d", h=8)), eng="act", join=True)
                S.dma(self.V_d.v(self.V_d.ap[tok0 + i * 128:tok0 + (i + 1) * 128, :].rearrange("p (h d) -> p h d", h=8)),
                      vt, q="sp", join=True)

    def stage_A2(self):
        S, A, psb = self.S, self.A, self.psb
        w_oa = A.alloc("w_oa", [128, 4, D], BF16)
        S.dma(w_oa, self.w_out_a.v(self.w_out_a.ap.rearrange("(kt p) n -> p kt n", p=128)), q="pool")
        w_ob = A.alloc("w_ob", [128, 4, D], BF16)
        S.dma(w_ob, self.w_out_b.v(self.w_out_b.ap.rearrange("(kt p) n -> p kt n", p=128)), q="pool")
        lng = A.alloc("lng", [128, D])
        S.dma(lng, self.ab_ln_g, q="sp")
        lnb = A.alloc("lnb", [128, D])
        S.dma(lnb, self.ab_ln_b, q="sp")
        KTs = A.alloc("KTs", [96, 8, SEQ], BF16)
        Vs = A.alloc("Vs", [128, 16, 1024], BF16)
        qTc = A.ring("qTc", 2, [96, 8, 512], BF16)
        yaTc = A.ring("yaTc", 2, [128, 4, 512], BF16)
        ybT = A.alloc("ybT", [128, 4, 512], BF16)
        PT = A.ring("PT", 3, [128, 512], BF16)
        rec = A.ring("rec", 2, [64, 512])
        xres = A.ring("xres", 2, [128, D])
        z = A.ring("z", 2, [128, D])
        zn = A.ring("zn", 2, [128, D])
        xo = A.ring("xo", 2, [128, D])
        small = A.ring("smallA2", 4, [128, 24])
        ps_s = Ring([psb[0], psb[1], psb[2]])
        ps_nd = Ring([psb[3], psb[4], psb[5], psb[6]])
        ps_o = Ring([psb[7], psb[0]])
        scale = 96.0 ** -0.5
        for b in range(NB):
            for h in range(8):
                S.dma(KTs[:, h, :], self.kT_d[h, :, b * SEQ:(b + 1) * SEQ], q="sp", join=(h > 0))
            S.dma(Vs, self.V_d.v(self.V_d.ap[b * SEQ:(b + 1) * SEQ, :].rearrange("(n p) c -> p n c", p=128)), q="sp")
            for cs in range(4):
                tok0 = b * SEQ + cs * 512
                q = qTc.next()
                S.dma(q, self.qT_d.v(self.qT_d.ap.rearrange("h d t -> d h t")[:, :, tok0:tok0 + 512]), q="sp")
                ya = yaTc.next()
                S.dma(ya, self.yaT_d.v(self.yaT_d.ap.rearrange("(h p) t -> p h t", p=128)[:, :, tok0:tok0 + 512]), q="sp")
                nkt = 4 * (cs + 1)
                for h in range(8):
                    pnum = ps_nd.next()

                    def qk(kt, h=h):
                        j = kt - 4 * cs
                        c0 = 0 if j < 0 else j * 128
                        ps = ps_s.next()
                        S.mm(ps[:, c0:512], KTs[:, h, kt * 128:(kt + 1) * 128], q[:, h, c0:512],
                             start=True, stop=(j < 0))
                        if j >= 0:
                            S.mm(ps[:, c0:c0 + 128], self.ident_bf, self.maskneg, start=False, stop=True)
                        pt = PT.next()
                        S.act(pt[:, c0:512], ps[:, c0:512], AF.Exp, scale=scale)
                        return pt, c0

                    def pv(kt, pt, c0, h=h, pnum=pnum):
                        S.mm(pnum[:, c0:512], Vs[:, kt, h * 128:(h + 1) * 128], pt[:, c0:512],
                             start=(kt == 0), stop=(kt == nkt - 1))

                    pend = [qk(0)]
                    for kt in range(nkt):
                        if kt + 1 < nkt:
                            pend.append(qk(kt + 1))
                        pt, c0 = pend.pop(0)
                        pv(kt, pt, c0)
                    r = rec.next()
                    S.recip(r, pnum[64:128, :])
                    po = (h % 2) * 64
                    S.tt(ybT[po:po + 64, h // 2, :], pnum[0:64, :], r, ALU.mult, join=(h > 0))
                for i in range(4):
                    xr = xres.next()
                    S.dma(xr, self.x[tok0 + i * 128:tok0 + (i + 1) * 128, :], q="sp")
                    pso = [ps_o.next(), ps_o.next()]
                    for hf in range(2):
                        for kk in range(4):
                            S.mm(pso[hf], ya[:, kk, i * 128:(i + 1) * 128], w_oa[:, kk, hf * 512:(hf + 1) * 512],
                                 start=(kk == 0), stop=False)
                        for hh in range(4):
                            S.mm(pso[hf], ybT[:, hh, i * 128:(i + 1) * 128], w_ob[:, hh, hf * 512:(hf + 1) * 512],
                                 start=False, stop=(hh == 3))
                    sm = small.next()
                    self.ln_epilogue(pso[0], pso[1], xr, lng, lnb, z.next(),
                                     sm.v(sm.ap[:, 0:12].rearrange("p (a b) -> p a b", a=2)), sm[:, 12:14], sm[:, 14:15],
                                     sm[:, 15:16], sm[:, 16:17], zn.next(), xo.next(),
                                     self.x1_d[tok0 + i * 128:tok0 + (i + 1) * 128, :], self.eps_ln)

    def ffn_stage(self, src_d, dst_d, units, lng_d, lnb_d, router=None):
        S, A, psb = self.S, self.A, self.psb
        NT = 8
        lng = A.alloc("lng", [128, D])
        S.dma(lng, lng_d, q="sp")
        lnb = A.alloc("lnb", [128, D])
        S.dma(lnb, lnb_d, q="sp")
        maxh = max(u[3] for u in units)
        xT = A.alloc("xT", [128, KT, NT * 128], BF16)
        hT = A.alloc("hT", [128, maxh, NT * 128], BF16)
        acc = A.alloc("acc", [128, NT, D])
        xtok = A.ring("xtok", 2, [128, D])
        wgr = A.ring("wg", 4, [128, KT, 256], BF16)
        wur = A.ring("wu", 4, [128, KT, 256], BF16)
        wd = A.alloc("wd", [128, maxh, D], BF16)
        sil = A.ring("sil", 3, [128, 512])
        z = A.ring("z", 2, [128, D])
        zn = A.ring("zn", 2, [128, D])
        small = A.ring("smallF", 4, [128, 24])
        ps_gu = Ring([psb[0], psb[1], psb[2], psb[3]])
        ps_dn = Ring([psb[4], psb[5], psb[6], psb[7]])
        if router is not None:
            rt = A.alloc("rt", [128, KT, N_EXP])
            S.dma(rt, router, q="sp")
            xTf = A.ring("xTf", 2, [128, KT, 128])
            comb = A.alloc("comb", [128, NT, N_EXP])
            rsm = A.ring("rsm", 4, [128, 32])
        for c in range(TOK // (NT * 128)):
            tok0 = c * NT * 128
            if router is None:
                self.load_xT(src_d, tok0, NT, xT, xtok, ps_dn, self.ident)
            else:
                cur = {}

                def cb(i, half, srcv, ev, cur=cur):
                    if half == 0:
                        cur["t"] = xTf.next()
                    S.copy(cur["t"][:, half * 4:half * 4 + 4, :], srcv, eng=ev, join=(half == 1))
                    lvl = int(getattr(self, "rlevel", 4))
                    if half == 1 and lvl == 0:
                        S.memset(comb[:, i, :], 0.125, join=True)
                        return
                    if half == 1:
                        xf = cur["t"]
                        ps = ps_gu.next()
                        for kt in range(KT):
                            S.mm(ps[:, 0:N_EXP], xf[:, kt, :], rt[:, kt, :], start=(kt == 0), stop=(kt == KT - 1))
                        r = rsm.next()
                        lg = r[:, 0:8]
                        S.copy(lg, ps[:, 0:N_EXP])
                        lvl = int(getattr(self, "rlevel", 4))
                        if lvl < 4:
                            S.memset(comb[:, i, :], 0.125, join=True)
                        if lvl < 2:
                            return
                        mx = r[:, 8:16]
                        S.add("dve", (lambda mx, lg: lambda e: e.max(out=mx.ap, in_=lg.ap))(mx, lg), reads=[r], writes=[r], join=True)
                        if lvl < 3:
                            return
                        dd = r[:, 16:17]
                        S.tt(dd, r[:, 9:10], r[:, 8:9], ALU.subtract, join=True)
                        S.act(r[:, 17:18], dd, AF.Exp, join=True)
                        S.ts(r[:, 18:19], r[:, 17:18], 1.0, ALU.add, join=True)
                        S.recip(r[:, 19:20], r[:, 18:19], join=True)
                        S.ts(r[:, 20:21], r[:, 19:20], -1.0, ALU.mult, 1.0, ALU.add, join=True)
                        if lvl < 4:
                            return
                        r2 = rsm.next()
                        S.ts(r2[:, 0:8], lg, r[:, 8:9], ALU.is_equal)
                        S.ts(r2[:, 8:16], lg, r[:, 9:10], ALU.is_equal, join=True)
                        S.ts(r2[:, 16:24], lg, r[:, 8:9], ALU.is_lt, join=True)
                        S.tt(r2[:, 8:16], r2[:, 8:16], r2[:, 16:24], ALU.mult, join=True)
                        S.ts(r2[:, 0:8], r2[:, 0:8], r[:, 19:20], ALU.mult, join=True)
                        S.stt(comb[:, i, :], r2[:, 8:16], r[:, 20:21], r2[:, 0:8], ALU.mult, ALU.add, join=True)

                self.load_xT(src_d, tok0, NT, xT, xtok, ps_dn, self.ident, xTf_cb=cb)
            for ui, (wg_ap, wu_ap, wd_ap, nh, ex) in enumerate(units):
                wdv = wd_ap.rearrange("(j p) n -> p j n", p=128)

                def load_wd(sidx, wdv=wdv, nh=nh):
                    nsplit = 2
                    per = (nh + nsplit - 1) // nsplit
                    ja, jb = sidx * per, min(nh, (sidx + 1) * per)
                    S.dma(wd[:, ja:jb, :], T(self.wsrc, wdv[:, ja:jb, :]), q="pool", join=(sidx > 0))
                wgv = wg_ap.rearrange("(kt p) n -> p kt n", p=128)
                wuv = wu_ap.rearrange("(kt p) n -> p kt n", p=128)
                for j0 in range(0, nh, 2):
                    nj = min(2, nh - j0)
                    wgb = wgr.next()
                    wub = wur.next()
                    S.dma(wgb[:, :, 0:nj * 128], T(self.wsrc, wgv[:, :, j0 * 128:(j0 + nj) * 128]), q="pool")
                    S.dma(wub[:, :, 0:nj * 128], T(self.wsrc, wuv[:, :, j0 * 128:(j0 + nj) * 128]), q="pool")
                    if j0 == 6:
                        load_wd(0)
                    if j0 == 10:
                        load_wd(1)
                    for jj in range(nj):
                        j = j0 + jj
                        for blk in range(NT // 4):
                            pg = ps_gu.next()
                            pu = ps_gu.next()
                            for kt in range(KT):
                                S.mm(pg, wgb[:, kt, jj * 128:(jj + 1) * 128], xT[:, kt, blk * 512:(blk + 1) * 512],
                                     start=(kt == 0), stop=(kt == KT - 1))
                            for kt in range(KT):
                                S.mm(pu, wub[:, kt, jj * 128:(jj + 1) * 128], xT[:, kt, blk * 512:(blk + 1) * 512],
                                     start=(kt == 0), stop=(kt == KT - 1))
                            sl = sil.next()
                            S.act(sl, pg, AF.Silu)
                            S.tt(hT[:, j, blk * 512:(blk + 1) * 512], pu, sl, ALU.mult, join=not (j == 0 and blk == 0))
                for i in range(NT):
                    for hf in range(2):
                        pd = ps_dn.next()
                        for j in range(nh):
                            S.mm(pd, hT[:, j, i * 128:(i + 1) * 128], wd[:, j, hf * 512:(hf + 1) * 512],
                                 start=(j == 0), stop=(j == nh - 1))
                        dst = acc[:, i, hf * 512:(hf + 1) * 512]
                        first = (ui == 0)
                        if ex is None or getattr(self, "noscale", False):
                            if first:
                                S.copy(dst, pd, eng="act", join=not (i == 0 and hf == 0))
                            else:
                                S.tt(dst, pd, dst, ALU.add, join=True)
                        else:
                            cw = comb[:, i, ex:ex + 1]
                            if first:
                                S.act(dst, pd, AF.Identity, scale=cw, join=not (i == 0 and hf == 0))
                            else:
                                S.stt(dst, pd, cw, dst, ALU.mult, ALU.add, join=True)
            for i in range(NT):
                xr = xtok.next()
                S.dma(xr, src_d[tok0 + i * 128:tok0 + (i + 1) * 128, :], q="sp")
                sm = small.next()
                znt = zn.next()
                self.ln_epilogue(acc[:, i, 0:512], acc[:, i, 512:1024], xr, lng, lnb, z.next(),
                                 sm.v(sm.ap[:, 0:12].rearrange("p (a b) -> p a b", a=2)), sm[:, 12:14], sm[:, 14:15],
                                 sm[:, 15:16], sm[:, 16:17], znt, znt,
                                 dst_d[tok0 + i * 128:tok0 + (i + 1) * 128, :], self.eps_ln)

    def stage_B(self):
        self.wsrc = Buf("wsrc", dram=True)
        units = []
        half = D_FF // 2
        for u in range(2):
            units.append((self.ffd_wg.ap[:, u * half:(u + 1) * half], self.ffd_wu.ap[:, u * half:(u + 1) * half],
                          self.ffd_wd.ap[u * half:(u + 1) * half, :], half // 128, None))
        self.ffn_stage(self.x1_d, self.x2_d, units, self.ffd_ln_g, self.ffd_ln_b)

    def stage_D(self):
        self.wsrc = Buf("wsrc", dram=True)
        units = []
        half = D_EXP // 2
        for e in range(N_EXP):
            for u in range(2):
                units.append((self.moe_wg.ap[e, :, u * half:(u + 1) * half], self.moe_wu.ap[e, :, u * half:(u + 1) * half],
                              self.moe_wd.ap[e, u * half:(u + 1) * half, :], half // 128, e))
        mode = getattr(self, "mode", "full")
        if mode == "nr1":
            units = [(a, b, c, n, None) for (a, b, c, n, e) in units[:2]]
            self.ffn_stage(self.x3_d, self.out, units, self.moe_ln_g, self.moe_ln_b)
            return
        if mode == "norouter":
            units = [(a, b, c, n, None) for (a, b, c, n, e) in units]
            self.ffn_stage(self.x3_d, self.out, units, self.moe_ln_g, self.moe_ln_b)
            return
        if mode == "two":
            units = units[:4]
        self.ffn_stage(self.x3_d, self.out, units, self.moe_ln_g, self.moe_ln_b, router=self.router)

    def stage_Ca(self):
        S, A, psb = self.S, self.A, self.psb
        w = A.alloc("cwin", [128, KT, D], BF16)
        S.dma(w, self.c_w_in.v(self.c_w_in.ap.rearrange("(kt p) n -> p kt n", p=128)), q="pool")
        xtok = A.ring("xtok", 2, [128, D])
        xT = A.alloc("xT", [128, KT, 512], BF16)
        uT = A.ring("uTo", 2, [128, KT, 512])
        gen = Ring(psb)
        for c in range(TOK // 512):
            tok0 = c * 512
            self.load_xT(self.x2_d, tok0, 4, xT, xtok, gen, self.ident)
            u = uT.next()
            for m in range(KT):
                ps = gen.next()
                for kt in range(KT):
                    S.mm(ps, w[:, kt, m * 128:(m + 1) * 128], xT[:, kt, :], start=(kt == 0), stop=(kt == KT - 1))
                S.copy(u[:, m, :], ps, eng="act" if m % 2 else "dve", join=(m > 0))
            S.dma(self.uT_d.v(self.uT_d.ap.rearrange("(m p) t -> p m t", p=128)[:, :, tok0:tok0 + 512]), u, q="sp", join=True)

    s5e = ("dve", "dve", "dve", "dve", "dve", "dve", "pool")

    def stage_Cb(self):
        S, A, psb = self.S, self.A, self.psb
        NM = 32
        are = A.alloc("are", [128, NM]); S.dma(are, self.s5_are, q="sp")
        aim = A.alloc("aim", [128, NM]); S.dma(aim, self.s5_aim, q="sp")
        lst = A.alloc("lst", [128, NM]); S.dma(lst, self.s5_ls, q="sp")
        dsk = A.alloc("dsk", [128, 8]); S.dma(dsk, self.s5_d, q="sp")
        BTr = A.alloc("BTr", [128, 8, 4, 128], BF16)
        BTi = A.alloc("BTi", [128, 8, 4, 128], BF16)
        CTr = A.alloc("CTr", [128, 8, 4, 128], BF16)
        CTi = A.alloc("CTi", [128, 8, 4, 128], BF16)
        for (dst, src, k) in ((BTr, self.s5_BT, 0), (BTi, self.s5_BT, 1), (CTr, self.s5_CT, 0), (CTi, self.s5_CT, 1)):
            S.dma(dst, src.v(src.ap[k].rearrange("kt p m s -> p kt m s")), q="pool")
        S.ts(CTi, CTi, -1.0, ALU.mult, eng="pool")
        CTrn = A.alloc("CTrn", [128, 8, 4, 128], BF16)
        S.ts(CTrn, CTr, -1.0, ALU.mult, eng="pool")
        mask = A.alloc("smask", [128, 512]); S.dma(mask, self.scanmask, q="sp")
        sc = A.alloc("s5sc", [128, 16, NM])
        dl = sc[:, 0, :]
        S.act(dl, lst, AF.Exp)
        rho = sc[:, 1, :]; S.tt(rho, dl, are, ALU.mult)
        th = sc[:, 2, :]; S.tt(th, dl, aim, ALU.mult)
        Einr = A.alloc("Einr", [128, NM, 128]); Eini = A.alloc("Eini", [128, NM, 128])
        Eoutr = A.alloc("Eoutr", [128, NM, 128]); Eouti = A.alloc("Eouti", [128, NM, 128])
        tm = A.mark()
        jg = self.jgrid
        jg3 = jg.v(jg.ap[:, None, :].to_broadcast([128, NM, 128]))

        def bc(t):
            return t.v(t.ap[:, :, None].to_broadcast([128, NM, 128]))
        ang = A.alloc("ang", [128, NM, 128])
        S.tt(ang, jg3, bc(th), ALU.mult)
        kk = A.alloc("kk", [128, NM, 128])
        S.ts(kk, ang, 1.0 / TWO_PI, ALU.mult, MAGIC, ALU.add)
        S.ts(kk, kk, MAGIC, ALU.subtract)
        r1 = A.alloc("r1", [128, NM, 128])
        S.stt(r1, kk, -C1, ang, ALU.mult, ALU.add)
        S.stt(r1, kk, -C2, r1, ALU.mult, ALU.add)
        S.ts(r1, r1, -PI_SAFE, ALU.max, PI_SAFE, ALU.min)
        sn = ang
        S.act(sn, r1, AF.Sin)
        S.stt(r1, r1, -1.0, r1, ALU.mult, ALU.max)
        cs_ = kk
        S.act(cs_, r1, AF.Sin, bias=self.halfpi, scale=-1.0)
        lm = r1
        S.tt(lm, jg3, bc(rho), ALU.mult)
        mo = A.alloc("mo", [128, NM, 128])
        S.act(mo, lm, AF.Exp)
        S.tt(Eoutr, mo, cs_, ALU.mult)
        S.tt(Eouti, mo, sn, ALU.mult)
        S.act(mo, lm, AF.Exp, scale=-1.0)
        er = lm
        S.tt(er, mo, cs_, ALU.mult)
        ei = A.alloc("ei", [128, NM, 128])
        S.tt(ei, mo, sn, ALU.mult)
        S.ts(ei, ei, -1.0, ALU.mult)
        abr = sc[:, 3, :]; S.copy(abr, Eoutr[:, :, 1])
        abi = sc[:, 4, :]; S.copy(abi, Eouti[:, :, 1])
        am1 = sc[:, 5, :]; S.ts(am1, abr, -1.0, ALU.add)
        den = sc[:, 6, :]; S.tt(den, are, are, ALU.mult)
        t0 = sc[:, 7, :]; S.tt(t0, aim, aim, ALU.mult)
        S.tt(den, den, t0, ALU.add)
        rden = sc[:, 8, :]; S.recip(rden, den)
        cre = sc[:, 9, :]; S.tt(cre, am1, are, ALU.mult)
        S.tt(t0, abi, aim, ALU.mult); S.tt(cre, cre, t0, ALU.add); S.tt(cre, cre, rden, ALU.mult)
        cim = sc[:, 10, :]; S.tt(cim, abi, are, ALU.mult)
        S.tt(t0, am1, aim, ALU.mult); S.tt(cim, cim, t0, ALU.subtract); S.tt(cim, cim, rden, ALU.mult)
        S.tt(Einr, er, bc(cre), ALU.mult)
        S.tt(mo, ei, bc(cim), ALU.mult)
        S.tt(Einr, Einr, mo, ALU.subtract)
        S.tt(Eini, er, bc(cim), ALU.mult)
        S.tt(mo, ei, bc(cre), ALU.mult)
        S.tt(Eini, Eini, mo, ALU.add)
        Gr = sc[:, 11, :]; Gi = sc[:, 12, :]
        t1 = sc[:, 13, :]
        S.tt(Gr, Eoutr[:, :, 127], abr, ALU.mult)
        S.tt(t1, Eouti[:, :, 127], abi, ALU.mult)
        S.tt(Gr, Gr, t1, ALU.subtract)
        S.tt(Gi, Eoutr[:, :, 127], abi, ALU.mult)
        S.tt(t1, Eouti[:, :, 127], abr, ALU.mult)
        S.tt(Gi, Gi, t1, ALU.add)
        S.barrier()
        A.reset(tm)
        uld = A.ring("uld", 2, [128, 512])
        uf2 = A.ring("uf2", 2, [128, 512])
        ub = A.ring("ub", 8, [128, 512], BF16)
        ringB = A.ring("rB", 24, [128, 4, 128], BF16)
        ringF = A.ring("rF", 12, [128, 4, 128])
        Hc = [[A.alloc(f"Hc{b}_{kt}", [128, 2, 4]) for kt in range(KT)] for b in range(NB)]
        cin = A.ring("cin", 8, [128, 4, 4])
        yv = A.ring("yv", 2, [128, 512])
        go = A.ring("go", 2, [128, 512], BF16)
        ps_bu = Ring([psb[0], psb[1], psb[2], psb[3]])
        ps_y = Ring([psb[4], psb[5], psb[6], psb[7]])
        uTv = self.uT_d.ap.rearrange("(m p) t -> p m t", p=128)
        gTv = self.gT_d.ap.rearrange("(m p) t -> p m t", p=128)
        for b in range(NB):
            for kt in range(KT):
                S.memset(Hc[b][kt], 0.0, eng="pool")

        def st1(it):
            ch = it["ch"]; kt = ch["kt"]; k = it["k"]; m0 = kt * 4
            if k == 0:
                ul = uld.next()
                S.dma(ul, self.uT_d.v(uTv[:, kt, ch["tok0"]:ch["tok0"] + 512]), q="sp")
                ch["ub"] = ub.next()
                S.copy(ch["ub"], ul, eng="act")
            u_b = ch["ub"]
            pr = ps_bu.next(); pi = ps_bu.next()
            for ml in range(4):
                S.mm(pr[:, ml * 128:(ml + 1) * 128], BTr[:, kt, ml, :], u_b[:, k * 128:(k + 1) * 128],
                     start=True, stop=True, join=(ml > 0))
            for ml in range(4):
                S.mm(pi[:, ml * 128:(ml + 1) * 128], BTi[:, kt, ml, :], u_b[:, k * 128:(k + 1) * 128],
                     start=True, stop=True, join=(ml > 0))
            pr3 = pr.v(pr.ap.rearrange("p (a b) -> p a b", a=4))
            pi3 = pi.v(pi.ap.rearrange("p (a b) -> p a b", a=4))
            eir = Einr[:, m0:m0 + 4, :]; eii = Eini[:, m0:m0 + 4, :]
            a = [ringB.next() for _ in range(4)]
            S.tt(a[0], pr3, eir, ALU.mult)
            S.tt(a[1], pi3, eii, ALU.mult)
            S.tt(a[2], pi3, eir, ALU.mult)
            S.tt(a[3], pr3, eii, ALU.mult)
            it["a"] = a

        def st2(it):
            a = it["a"]
            kr = ringF.next(); ki = ringF.next()
            S.tt(kr, a[0], a[1], ALU.subtract, eng=self.s5e[0])
            S.tt(ki, a[2], a[3], ALU.add, eng=self.s5e[1])
            it["kr"] = kr; it["ki"] = ki

        def st3(it):
            ch = it["ch"]; kt = ch["kt"]; m0 = kt * 4
            kr = it["kr"]; ki = it["ki"]
            H = Hc[ch["b"]][kt]
            Hr = H[:, 0, :]; Hi = H[:, 1, :]
            g_r = Gr[:, m0:m0 + 4]; g_i = Gi[:, m0:m0 + 4]
            ci = cin.next()
            te = self.s5e[6]
            S.tt(ci[:, 0, :], Hr, g_r, ALU.mult, eng=te)
            S.tt(ci[:, 1, :], Hi, g_i, ALU.mult, join=True, eng=te)
            S.tt(ci[:, 2, :], Hr, g_i, ALU.mult, join=True, eng=te)
            S.tt(ci[:, 3, :], Hi, g_r, ALU.mult, join=True, eng=te)
            S.tt(ci[:, 0, :], ci[:, 0, :], ci[:, 1, :], ALU.subtract, join=True, eng=te)
            S.tt(ci[:, 2, :], ci[:, 2, :], ci[:, 3, :], ALU.add, join=True, eng=te)
            S.tt(kr[:, :, 0], kr[:, :, 0], ci[:, 0, :], ALU.add, join=True, eng=te)
            S.tt(ki[:, :, 0], ki[:, :, 0], ci[:, 2, :], ALU.add, join=True, eng=te)
            hr_t = ringF.next(); hi_t = ringF.next()
            for (src, dst) in ((kr, hr_t), (ki, hi_t)):
                sf = src.ap.rearrange("p a b -> p (a b)")
                df = dst.ap.rearrange("p a b -> p (a b)")
                S.add("dve", (lambda df, sf: lambda e: e.tensor_tensor_scan(df, mask.ap, sf, 0.0, ALU.mult, ALU.add))(df, sf),
                      reads=[mask, src], writes=[dst])
            S.copy(H[:, 0, :], hr_t[:, :, 127], eng="act")
            S.copy(H[:, 1, :], hi_t[:, :, 127], eng="act", join=True)
            it["hr_t"] = hr_t; it["hi_t"] = hi_t

        def st4(it):
            kt = it["ch"]["kt"]; m0 = kt * 4
            eor = Eoutr[:, m0:m0 + 4, :]; eoi = Eouti[:, m0:m0 + 4, :]
            hr_t = it["hr_t"]; hi_t = it["hi_t"]
            bb = [ringB.next() for _ in range(4)]
            S.tt(bb[0], hr_t, eor, ALU.mult, eng=self.s5e[2])
            S.tt(bb[1], hi_t, eoi, ALU.mult, eng=self.s5e[3])
            S.tt(bb[2], hr_t, eoi, ALU.mult, eng=self.s5e[4])
            S.tt(bb[3], hi_t, eor, ALU.mult, eng=self.s5e[5])
            it["bb"] = bb

        def st5(it):
            ch = it["ch"]; kt = ch["kt"]; k = it["k"]
            bb = it["bb"]
            if k == 0:
                ch["py"] = ps_y.next()
            py = ch["py"]
            for wi, (W, P_) in enumerate(((CTr, bb[0]), (CTrn, bb[1]), (CTi, bb[2]), (CTi, bb[3]))):
                for ml in range(4):
                    S.mm(py[:, k * 128:(k + 1) * 128], W[:, kt, ml, :], P_[:, ml, :],
                         start=(wi == 0 and ml == 0), stop=(wi == 3 and ml == 3),
                         join=not (k == 0 and wi == 0 and ml == 0))
            if k == 3:
                u_f = uf2.next()
                S.dma(u_f, self.uT_d.v(uTv[:, kt, ch["tok0"]:ch["tok0"] + 512]), q="sp")
                y = yv.next()
                S.stt(y, u_f, dsk[:, kt:kt + 1], py, ALU.mult, ALU.add)
                g = go.next()
                S.act(g, y, AF.Gelu_apprx_tanh)
                S.dma(self.gT_d.v(gTv[:, kt, ch["tok0"]:ch["tok0"] + 512]), g, q="sp", join=True)

        its = []
        for b in range(NB):
            for blk in range(SEQ // 512):
                tok0 = b * SEQ + blk * 512
                for kq in range(0, KT, 4):
                    chains = [{"kt": kt, "b": b, "tok0": tok0} for kt in range(kq, kq + 4)]
                    for k in range(4):
                        for ch in chains:
                            its.append({"ch": ch, "k": k})
        stages = (st1, st2, st3, st4, st5)
        for t in range(len(its) + 4):
            for si in range(5):
                n = t - si
                if 0 <= n < len(its):
                    stages[si](its[n])

    def stage_Cc(self):
        S, A, psb = self.S, self.A, self.psb
        wg = A.alloc("gluw", [128, KT, D], BF16)
        S.dma(wg, self.glu_w.v(self.glu_w.ap.rearrange("(kt p) n -> p kt n", p=128)), q="pool")
        wo = A.alloc("cwout", [128, KT, D], BF16)
        S.dma(wo, self.c_w_out.v(self.c_w_out.ap.rearrange("(kt p) n -> p kt n", p=128)), q="pool")
        gb = A.alloc("glub", [128, 8]); S.dma(gb, self.glu_b, q="sp")
        lng = A.alloc("lng", [128, D]); S.dma(lng, self.c_ln_g, q="sp")
        lnb = A.alloc("lnb", [128, D]); S.dma(lnb, self.c_ln_b, q="sp")
        gTr = A.ring("gTc", 2, [128, KT, 512], BF16)
        zT = A.alloc("zT", [128, KT, 512], BF16)
        sg = A.ring("sg", 2, [128, 512])
        xres = A.ring("xres", 2, [128, D])
        z = A.ring("z", 2, [128, D]); zn = A.ring("zn", 2, [128, D])
        small = A.ring("smallC", 4, [128, 24])
        gen = Ring([psb[0], psb[1], psb[2], psb[3]])
        ps_o = Ring([psb[4], psb[5], psb[6], psb[7]])
        gTv = self.gT_d.ap.rearrange("(m p) t -> p m t", p=128)
        for c in range(TOK // 512):
            tok0 = c * 512
            g = gTr.next()
            S.dma(g, self.gT_d.v(gTv[:, :, tok0:tok0 + 512]), q="sp")
            for n in range(KT):
                ps = gen.next()
                for kt in range(KT):
                    S.mm(ps, wg[:, kt, n * 128:(n + 1) * 128], g[:, kt, :], start=(kt == 0), stop=(kt == KT - 1))
                s = sg.next()
                S.act(s, ps, AF.Sigmoid, bias=gb[:, n:n + 1], scale=1.0)
                S.tt(zT[:, n, :], g[:, n, :], s, ALU.mult, join=(n > 0))
            for i in range(4):
                xr = xres.next()
                S.dma(xr, self.x2_d[tok0 + i * 128:tok0 + (i + 1) * 128, :], q="sp")
                pso = [ps_o.next(), ps_o.next()]
                for hf in range(2):
                    for kt in range(KT):
                        S.mm(pso[hf], zT[:, kt, i * 128:(i + 1) * 128], wo[:, kt, hf * 512:(hf + 1) * 512],
                             start=(kt == 0), stop=(kt == KT - 1))
                sm = small.next()
                znt = zn.next()
                self.ln_epilogue(pso[0], pso[1], xr, lng, lnb, z.next(),
                                 sm.v(sm.ap[:, 0:12].rearrange("p (a b) -> p a b", a=2)), sm[:, 12:14], sm[:, 14:15],
                                 sm[:, 15:16], sm[:, 16:17], znt, znt,
                                 self.x3_d[tok0 + i * 128:tok0 + (i + 1) * 128, :], self.eps_ln)


def nc_out(nc, name, shape, dtype):
    return nc.dram_tensor(name, list(shape), dtype, kind="ExternalOutput").ap()


def _consts():
    c = np.zeros((128, 1024), np.float32)
    c[:, 0:128] = np.eye(128, dtype=np.float32)
    s = np.arange(128)[:, None]
    t = np.arange(128)[None, :]
    c[:, 128:256] = (s <= t).astype(np.float32)
    c[:, 256:384] = np.where(s <= t, 0.0, -30000.0).astype(np.float32)
    inv_freq = (1.0 / (np.float32(10000.0) ** (np.arange(0, 32, 2, dtype=np.float32) / np.float32(32)))).astype(np.float32)
    c[64:80, 384] = inv_freq
    c[80:96, 384] = inv_freq
    c[64:80, 385] = -1.0
    c[80:96, 385] = 1.0
    c[:, 386] = np.float32(math.pi / 2)
    c[:, 387] = LN_EPS
    c[:, 388] = RMS_EPS
    c[:, 512:640] = np.arange(128, dtype=np.float32)[None, :]
    m = np.ones((128, 512), np.float32)
    m[:, 0::128] = 0.0
    return c, m


def _bcast(v, n=128):
    return np.ascontiguousarray(np.broadcast_to(np.asarray(v, np.float32)[None, :], (n, v.shape[0])))


def _cols(v, k):
    return np.ascontiguousarray(np.asarray(v, np.float32).reshape(k, 128).T)


def prepare_shared(inp):
    f = lambda a: np.ascontiguousarray(np.asarray(a, np.float32))
    sh = {}
    cst, smask = _consts()
    sh["cst"] = cst
    sh["scanmask"] = smask
    w_in = f(inp["ab_w_in"][0])
    sh["w_in_main"] = np.ascontiguousarray(w_in[:, :1664])
    kp = w_in[:, 1664:1696]
    k2 = np.zeros((D, 192), np.float32)
    k2[:, 64:96] = kp
    k2[:, 96 + 64:96 + 80] = kp[:, 16:32]
    k2[:, 96 + 80:96 + 96] = kp[:, 0:16]
    sh["w_kpe2"] = k2
    sh["gm_ln_g_b"] = _bcast(inp["gm_ln_g"][0])
    sh["gm_ln_b_b"] = _bcast(inp["gm_ln_b"][0])
    ws = f(inp["gm_w_s"][0])
    sh["wsT"] = np.ascontiguousarray(ws.transpose(2, 0, 1))
    bs = f(inp["gm_b_s"][0])
    sh["bs_b"] = np.ascontiguousarray(np.broadcast_to(np.tile(bs, (1, 4))[None], (128, 4, 512)))
    sh["qnorm_c"] = _cols(inp["mla_q_norm"][0], 3)
    sh["kvnorm_c"] = _cols(inp["mla_kv_norm"][0], 2)
    wuq = f(inp["mla_w_uq"][0])
    sh["w_uq"] = wuq
    w3 = wuq.reshape(384, 8, 96)
    sw = np.zeros_like(w3)
    sw[:, :, 64:80] = w3[:, :, 80:96]
    sw[:, :, 80:96] = w3[:, :, 64:80]
    sh["w_uq_sw"] = np.ascontiguousarray(sw.reshape(384, 768))
    wukv = f(inp["mla_w_ukv"][0]).reshape(256, 8, 128)
    sh["w_uk"] = np.ascontiguousarray(wukv[:, :, 0:64].reshape(256, 512))
    sh["w_uv"] = np.ascontiguousarray(wukv[:, :, 64:128].reshape(256, 512))
    wout = f(inp["ab_w_out"][0])
    sh["w_out_a"] = np.ascontiguousarray(wout[0:512])
    sh["w_out_b"] = np.ascontiguousarray(wout[512:1024])
    sh["ab_ln_g_b"] = _bcast(inp["ab_ln_g"][0])
    sh["ab_ln_b_b"] = _bcast(inp["ab_ln_b"][0])
    sh["ffd_w_gate"] = f(inp["ffd_w_gate"][0])
    sh["ffd_w_up"] = f(inp["ffd_w_up"][0])
    sh["ffd_w_down"] = f(inp["ffd_w_down"][0])
    sh["ffd_ln_g_b"] = _bcast(inp["ffd_ln_g"][0])
    sh["ffd_ln_b_b"] = _bcast(inp["ffd_ln_b"][0])
    sh["c_w_in"] = f(inp["c_w_in"][0])

    def sm(a):
        a = f(a).reshape(32, 2, 64)
        return np.ascontiguousarray(a.transpose(1, 2, 0).reshape(128, 32))
    sh["s5_are_sm"] = sm(inp["s5_a_re"][0])
    sh["s5_aim_sm"] = sm(inp["s5_a_im"][0])
    ls = f(inp["s5_log_step"][0])
    sh["s5_ls_sm"] = sm(np.broadcast_to(ls[:, None], (64, 64)))
    BT = np.zeros((2, 8, 128, 4, 128), np.float32)
    CT = np.zeros((2, 8, 128, 4, 128), np.float32)
    for k, (Bk, Ck) in enumerate(((inp["s5_b_re"][0], inp["s5_c_re"][0]), (inp["s5_b_im"][0], inp["s5_c_im"][0]))):
        Bk = f(Bk)
        Ck = f(Ck)
        for g in range(64):
            kt, gl = divmod(g, 8)
            ml, two = divmod(gl, 2)
            BT[k, kt, gl * 16:(gl + 1) * 16, ml, two * 64:(two + 1) * 64] = Bk[g].T
            CT[k, kt, two * 64:(two + 1) * 64, ml, gl * 16:(gl + 1) * 16] = Ck[g].T
    sh["s5_BT"] = BT
    sh["s5_CT"] = CT
    sh["s5_d_c"] = _cols(inp["s5_d"][0], 8)
    sh["glu_w"] = f(inp["glu_w"][0])
    sh["glu_b_c"] = _cols(inp["glu_b"][0], 8)
    sh["c_w_out"] = f(inp["c_w_out"][0])
    sh["c_ln_g_b"] = _bcast(inp["c_ln_g"][0])
    sh["c_ln_b_b"] = _bcast(inp["c_ln_b"][0])
    sh["moe_router_c"] = np.ascontiguousarray(f(inp["moe_router"][0]).reshape(KT, 128, N_EXP).transpose(1, 0, 2))
    sh["moe_w_gate"] = f(inp["moe_w_gate"][0])
    sh["moe_w_up"] = f(inp["moe_w_up"][0])
    sh["moe_w_down"] = f(inp["moe_w_down"][0])
    sh["moe_ln_g_b"] = _bcast(inp["moe_ln_g"][0])
    sh["moe_ln_b_b"] = _bcast(inp["moe_ln_b"][0])
    return sh


_PROG_CACHE = {}


def get_prog(debug=False, upto="D"):
    key = (debug, upto)
    if key not in _PROG_CACHE:
        p = Prog(debug=debug, upto=upto)
        p.build()
        _PROG_CACHE[key] = p
    return _PROG_CACHE[key]


def run(inputs, debug=False, upto="D", cores=N_CORES):
    p = get_prog(debug, upto)
    sh = prepare_shared(inputs)
    x = np.asarray(inputs["x"], np.float32)
    pos = np.asarray(inputs["positions"], np.int32)
    in_maps = []
    for c in range(cores):
        m = {k: v for k, v in sh.items() if k in p.inputs}
        m["x"] = np.ascontiguousarray(x[c * NB:(c + 1) * NB].reshape(TOK, D))
        m["positions"] = np.ascontiguousarray(pos[c * NB:(c + 1) * NB])
        in_maps.append(m)
    res = run_bass_kernel_spmd(p.nc, in_maps, core_ids=list(range(cores)))
    return res


def kernel(**inputs):
    res = run(inputs)
    out = np.stack([np.asarray(r["out"], np.float32).reshape(NB, SEQ, D) for r in res.results], axis=0)
    return out.reshape(N_CORES * NB, SEQ, D)
```

```python
import math
import numpy as np
from contextlib import ExitStack
import concourse.bass as bass
import concourse.mybir as mybir
from concourse.bass_utils import run_bass_kernel_spmd

F32 = mybir.dt.float32
BF16 = mybir.dt.bfloat16
I32 = mybir.dt.int32
ALU = mybir.AluOpType
AF = mybir.ActivationFunctionType

N_CORES = 8
SEQ = 2048
NB = 2
TOK = NB * SEQ
D = 1024
KT = 8
ALPHA = 4.0 ** 0.25
LN_EPS = 1e-5
RMS_EPS = 1e-6
D_FF = 2816
N_EXP = 8
D_EXP = 3584
EPOCH = 12000
MAGIC = 12582912.0
TWO_PI = 2.0 * math.pi
C1 = 6.28125
C2 = TWO_PI - 6.28125
PI_SAFE = 3.141592


class Buf:
    __slots__ = ("name", "writers", "readers", "sem", "cnt", "dram", "excl", "prev")

    def __init__(self, name="", dram=False, excl=False):
        self.name = name
        self.dram = dram
        self.excl = excl
        self.prev = set()
        self.writers = []
        self.readers = []
        self.sem = None
        self.cnt = 0


class T:
    __slots__ = ("buf", "ap")

    def __init__(self, buf, ap):
        self.buf = buf
        self.ap = ap

    def __getitem__(self, idx):
        return T(self.buf, self.ap[idx])

    def v(self, ap):
        return T(self.buf, ap)

    def bc(self, shape):
        return T(self.buf, self.ap.to_broadcast(list(shape)))


class Op:
    __slots__ = ("eng", "fn", "deps", "dma", "tok", "signal", "idx", "wbuf")


class Sched:
    ENGS = ("pe", "act", "dve", "pool", "sp")

    def __init__(self, nc, stack):
        self.nc = nc
        self.stack = stack
        self.ops = []
        self.nsem = 0
        self.pending = {e: set() for e in self.ENGS}
        self.last_on = {}
        self.dma_since = {}

    def new_sem(self, name):
        self.nsem += 1
        return self.stack.enter_context(self.nc.semaphore(f"{name}_{self.nsem}"))

    def add(self, eng, fn, reads=(), writes=(), dma=False, join=False):
        op = Op()
        op.eng = eng
        op.fn = fn
        op.dma = dma
        op.idx = len(self.ops)
        op.signal = False
        op.tok = None
        op.wbuf = None
        deps = set()
        cz = self._compress
        for t in reads:
            deps.update(cz(t.buf.writers))
            if t.buf.excl:
                deps.update(cz([r for r in t.buf.readers if self.ops[r].eng != eng]))
        for t in writes:
            b = t.buf
            if not join:
                deps.update(cz(b.writers))
            else:
                deps.update(b.prev)
            deps.update(cz(b.readers))
        for t in reads:
            t.buf.readers.append(op.idx)
        for t in writes:
            b = t.buf
            if join:
                if b.readers:
                    b.prev = b.prev | cz(b.readers)
                b.writers.append(op.idx)
            else:
                b.prev = cz(b.writers) | cz(b.readers)
                b.writers = [op.idx]
            b.readers = []
        if dma:
            op.wbuf = writes[0].buf
            if writes[0].buf.dram and not reads[0].buf.dram:
                op.wbuf = reads[0].buf
            self.dma_since[id(op.wbuf)] = op.idx
        else:
            self.last_on[eng] = op.idx
        if self.pending[eng]:
            deps.update(self.pending[eng])
            self.pending[eng] = set()
        deps.discard(op.idx)
        op.deps = deps
        self.ops.append(op)
        return op

    def _compress(self, lst):
        n = len(self.ops)
        if len(lst) <= 1:
            return set(i for i in lst if i < n)
        out = set()
        last = {}
        for i in lst:
            if i >= n:
                continue
            o = self.ops[i]
            if o.dma:
                out.add(i)
            else:
                if last.get(o.eng, -1) < i:
                    last[o.eng] = i
        out.update(last.values())
        return out

    def barrier(self):
        deps = set(self.last_on.values()) | set(self.dma_since.values())
        for e in self.ENGS:
            self.pending[e] = set(deps) | self.pending[e]
        self.dma_since = {}

    def emit(self):
        nc = self.nc
        ops = self.ops
        for op in ops:
            for d in op.deps:
                p = ops[d]
                if p.dma:
                    continue
                if p.eng == "pe" and op.eng == "pe" and not op.dma:
                    continue
                p.signal = True
        eng_cnt = {e: 0 for e in self.ENGS}
        eng_sems = {e: [] for e in self.ENGS}
        for op in ops:
            if op.dma:
                b = op.wbuf
                if b.sem is None:
                    b.sem = self.new_sem("d")
                b.cnt += 16
                op.tok = (b.sem, b.cnt)
            elif op.signal:
                c = eng_cnt[op.eng]
                ep = c // EPOCH
                if ep >= len(eng_sems[op.eng]):
                    eng_sems[op.eng].append(self.new_sem("e" + op.eng))
                op.tok = (eng_sems[op.eng][ep], c % EPOCH + 1)
                eng_cnt[op.eng] = c + 1
        streams = {e: [] for e in self.ENGS}
        for op in ops:
            streams[op.eng].append(op)

        def run_stream(eng_name):
            def body(e):
                waited = {}
                for op in streams[eng_name]:
                    need = {}
                    for d in op.deps:
                        p = ops[d]
                        if (not p.dma) and p.eng == "pe" and eng_name == "pe" and not op.dma:
                            continue
                        s, v = p.tok
                        k = id(s)
                        if waited.get(k, 0) >= v:
                            continue
                        if k not in need or need[k][1] < v:
                            need[k] = (s, v)
                    for k, (s, v) in need.items():
                        e.wait_ge(s, v)
                        waited[k] = v
                    ins = op.fn(e)
                    if op.tok is not None:
                        ins.then_inc(op.tok[0], 16 if op.dma else 1)
            return body

        with nc.Block() as block:
            block.sync(run_stream("sp"))
            block.tensor(run_stream("pe"))
            block.vector(run_stream("dve"))
            block.scalar(run_stream("act"))
            block.gpsimd(run_stream("pool"))

    def dma(self, out, in_, q="sp", join=False, **kw):
        return self.add(q, lambda e: e.dma_start(out=out.ap, in_=in_.ap, **kw),
                        reads=[in_], writes=[out], dma=True, join=join)

    def mm(self, out, lhsT, rhs, start=True, stop=True, join=None, **kw):
        if join is None:
            join = not start
        return self.add("pe", lambda e: e.matmul(out.ap, lhsT.ap, rhs.ap, start=start, stop=stop, **kw),
                        reads=[lhsT, rhs], writes=[out], join=join)

    def transpose(self, out, in_, ident, join=False):
        return self.add("pe", lambda e: e.transpose(out.ap, in_.ap, ident.ap),
                        reads=[in_, ident], writes=[out], join=join)

    def act(self, out, in_, func, bias=None, scale=None, accum=None, join=False):
        reads = [in_]
        kw = {}
        if bias is not None:
            if isinstance(bias, T):
                reads.append(bias)
                kw["bias"] = bias.ap
            else:
                kw["bias"] = bias
        if scale is not None:
            if isinstance(scale, T):
                reads.append(scale)
                kw["scale"] = scale.ap
            else:
                kw["scale"] = scale
        writes = [out]
        if accum is not None:
            writes.append(accum)
            kw["accum_out"] = accum.ap
        return self.add("act", lambda e: e.activation(out.ap, in_.ap, func, **kw),
                        reads=reads, writes=writes, join=join)

    def tt(self, out, a, b, op, eng="dve", join=False):
        return self.add(eng, lambda e: e.tensor_tensor(out.ap, a.ap, b.ap, op),
                        reads=[a, b], writes=[out], join=join)

    def ts(self, out, a, s1, op0, s2=None, op1=None, eng="dve", join=False):
        reads = [a]
        v1, v2 = s1, s2
        if isinstance(s1, T):
            reads.append(s1)
            v1 = s1.ap
        if isinstance(s2, T):
            reads.append(s2)
            v2 = s2.ap
        if op1 is None:
            return self.add(eng, lambda e: e.tensor_scalar(out.ap, a.ap, v1, None, op0),
                            reads=reads, writes=[out], join=join)
        return self.add(eng, lambda e: e.tensor_scalar(out.ap, a.ap, v1, v2, op0, op1),
                        reads=reads, writes=[out], join=join)

    def stt(self, out, a, s, b, op0, op1, eng="dve", join=False):
        reads = [a, b]
        v = s
        if isinstance(s, T):
            reads.append(s)
            v = s.ap
        return self.add(eng, lambda e: e.scalar_tensor_tensor(out.ap, a.ap, v, b.ap, op0, op1),
                        reads=reads, writes=[out], join=join)

    def copy(self, out, in_, eng="dve", join=False):
        if eng == "act":
            return self.add("act", lambda e: e.copy(out.ap, in_.ap), reads=[in_], writes=[out], join=join)
        return self.add(eng, lambda e: e.tensor_copy(out.ap, in_.ap), reads=[in_], writes=[out], join=join)

    def memset(self, out, val, eng="dve", join=False):
        return self.add(eng, lambda e: e.memset(out.ap, val), reads=[], writes=[out], join=join)

    def recip(self, out, in_, join=False):
        return self.add("dve", lambda e: e.reciprocal(out.ap, in_.ap), reads=[in_], writes=[out], join=join)

    def wait_all(self, eng, bufs):
        return self.add(eng, lambda e: e.nop(), reads=bufs, writes=[])


def _dsize(dt):
    return 2 if dt == BF16 else 4


class Arena:
    def __init__(self, S, nwords):
        self.S = S
        self.nwords = nwords
        self.t = S.stack.enter_context(S.nc.sbuf_tensor("arena", [128, nwords], F32))
        self.off = 0
        self.peak = 0

    def alloc(self, name, shape, dtype=F32):
        p = shape[0]
        free = 1
        for s in shape[1:]:
            free *= s
        words = (free * _dsize(dtype) + 3) // 4
        words = (words + 7) // 8 * 8
        assert self.off + words <= self.nwords, f"arena overflow at {name}: {self.off}+{words}>{self.nwords}"
        ap = self.t[0:p, self.off:self.off + words]
        self.off += words
        self.peak = max(self.peak, self.off)
        if dtype != F32:
            ap = ap.bitcast(dtype)
        ap = ap[:, 0:free]
        if len(shape) == 3:
            ap = ap.rearrange("p (a b) -> p a b", a=shape[1])
        elif len(shape) == 4:
            ap = ap.rearrange("p (a b c) -> p a b c", a=shape[1], b=shape[2])
        return T(Buf(name), ap)

    def ring(self, name, n, shape, dtype=F32):
        return Ring([self.alloc(f"{name}{i}", shape, dtype) for i in range(n)])

    def mark(self):
        return self.off

    def reset(self, m):
        self.off = m


class Ring:
    def __init__(self, items):
        self.items = items
        self.i = 0

    def next(self):
        t = self.items[self.i % len(self.items)]
        self.i += 1
        return t


class Prog:
    def __init__(self, debug=False, upto="D"):
        self.debug = debug
        self.upto = upto
        self.nc = bass.Bass("TRN2", target_bir_lowering=False)
        self.inputs = {}

    def din(self, name, shape, dtype=F32):
        t = self.nc.dram_tensor(name, list(shape), dtype, kind="ExternalInput")
        self.inputs[name] = (tuple(shape), dtype)
        return T(Buf(name, dram=True), t.ap())

    def dscr(self, name, shape, dtype=F32):
        kind = "ExternalOutput" if self.debug else "Internal"
        t = self.nc.dram_tensor(name, list(shape), dtype, kind=kind)
        return T(Buf(name, dram=True), t.ap())

    def load_xT(self, src, tok0, ntiles, xT, xtok_ring, ps_ring, ident, keep=None, xTf_cb=None):
        S = self.S
        for i in range(ntiles):
            xt = xtok_ring.next() if keep is None else keep[i]
            S.dma(xt, src[tok0 + i * 128: tok0 + (i + 1) * 128, :], q="sp")
            for half in range(2):
                ps = ps_ring.next()
                for k4 in range(4):
                    kt = half * 4 + k4
                    S.transpose(ps[:, k4 * 128:(k4 + 1) * 128], xt[:, kt * 128:(kt + 1) * 128], ident,
                                join=(k4 > 0))
                dst = xT[:, half * 4:half * 4 + 4, i * 128:(i + 1) * 128]
                srcv = ps.v(ps.ap.rearrange("p (a b) -> p a b", a=4))
                ev = "dve" if (i + half) % 2 == 0 else "act"
                S.copy(dst, srcv, eng=ev)
                if xTf_cb is not None:
                    xTf_cb(i, half, srcv, ev)

    def ln_epilogue(self, zsrc_a, zsrc_b, xres, g_b, b_b, z, st, mv, sd, rstd, nmr, zn, xo, dst, eps_col):
        S = self.S
        for hf, zs in enumerate((zsrc_a, zsrc_b)):
            S.stt(z[:, hf * 512:(hf + 1) * 512], xres[:, hf * 512:(hf + 1) * 512], ALPHA, zs,
                  ALU.mult, ALU.add, join=(hf == 1))
        for hf in range(2):
            S.add("dve", (lambda hf: lambda e: e.bn_stats(st.ap[:, hf, :], z.ap[:, hf * 512:(hf + 1) * 512]))(hf),
                  reads=[z], writes=[st], join=(hf == 1))
        S.add("dve", lambda e: e.bn_aggr(mv.ap, st.ap.rearrange("p a b -> p (a b)")), reads=[st], writes=[mv])
        S.act(sd, mv[:, 1:2], AF.Sqrt, bias=eps_col, scale=1.0)
        S.recip(rstd, sd)
        S.stt(nmr, mv[:, 0:1], -1.0, rstd, ALU.mult, ALU.mult)
        S.act(zn, z, AF.Identity, bias=nmr, scale=rstd)
        S.tt(zn, zn, g_b, ALU.mult, eng="pool")
        S.tt(xo, zn, b_b, ALU.add, eng="pool")
        S.dma(dst, xo, q="sp", join=True)

    def build(self):
        nc = self.nc
        with ExitStack() as stack:
            S = Sched(nc, stack)
            self.S = S
            A = Arena(S, 50176)
            self.A = A
            psb = [T(Buf(f"ps{i}", excl=True), stack.enter_context(nc.psum_tensor(f"ps{i}", [128, 512], F32))[:, :])
                   for i in range(8)]
            self.psb = psb
            self.declare_io()
            self.load_consts()
            order = ["A1", "A2", "B", "Ca", "Cb", "Cc", "D"]
            stages = {"A1": self.stage_A1, "A2": self.stage_A2, "B": self.stage_B, "Ca": self.stage_Ca,
                      "Cb": self.stage_Cb, "Cc": self.stage_Cc, "D": self.stage_D}
            base = A.mark()
            if getattr(self, "only", None):
                order = [self.only]
                self.x3_d = self.x
            for name in order:
                A.reset(base)
                stages[name]()
                S.barrier()
                if name == self.upto:
                    break
            if self.upto != "D":
                pass
            finals = [self.out] + (self.scratch_list if self.debug else [])
            S.wait_all("sp", finals)
            S.emit()
        return nc

    def declare_io(self):
        din = self.din
        self.x = din("x", [TOK, D])
        self.pos = din("positions", [NB, SEQ], I32)
        self.cst = din("cst", [128, 1024])
        self.scanmask = din("scanmask", [128, 512])
        self.w_in_main = din("w_in_main", [D, 1664])
        self.w_kpe2 = din("w_kpe2", [D, 192])
        self.gm_ln_g = din("gm_ln_g_b", [128, 512])
        self.gm_ln_b = din("gm_ln_b_b", [128, 512])
        self.wsT = din("wsT", [128, 4, 128])
        self.bs_b = din("bs_b", [128, 4, 512])
        self.qnorm = din("qnorm_c", [128, 3])
        self.kvnorm = din("kvnorm_c", [128, 2])
        self.w_uq = din("w_uq", [384, 768])
        self.w_uq_sw = din("w_uq_sw", [384, 768])
        self.w_uk = din("w_uk", [256, 512])
        self.w_uv = din("w_uv", [256, 512])
        self.w_out_a = din("w_out_a", [512, D])
        self.w_out_b = din("w_out_b", [512, D])
        self.ab_ln_g = din("ab_ln_g_b", [128, D])
        self.ab_ln_b = din("ab_ln_b_b", [128, D])
        self.ffd_wg = din("ffd_w_gate", [D, D_FF])
        self.ffd_wu = din("ffd_w_up", [D, D_FF])
        self.ffd_wd = din("ffd_w_down", [D_FF, D])
        self.ffd_ln_g = din("ffd_ln_g_b", [128, D])
        self.ffd_ln_b = din("ffd_ln_b_b", [128, D])
        self.c_w_in = din("c_w_in", [D, D])
        self.s5_are = din("s5_are_sm", [128, 32])
        self.s5_aim = din("s5_aim_sm", [128, 32])
        self.s5_ls = din("s5_ls_sm", [128, 32])
        self.s5_BT = din("s5_BT", [2, 8, 128, 4, 128])
        self.s5_CT = din("s5_CT", [2, 8, 128, 4, 128])
        self.s5_d = din("s5_d_c", [128, 8])
        self.glu_w = din("glu_w", [D, D])
        self.glu_b = din("glu_b_c", [128, 8])
        self.c_w_out = din("c_w_out", [D, D])
        self.c_ln_g = din("c_ln_g_b", [128, D])
        self.c_ln_b = din("c_ln_b_b", [128, D])
        if self.upto == "D":
            self.router = din("moe_router_c", [128, KT, N_EXP])
            self.moe_wg = din("moe_w_gate", [N_EXP, D, D_EXP])
            self.moe_wu = din("moe_w_up", [N_EXP, D, D_EXP])
            self.moe_wd = din("moe_w_down", [N_EXP, D_EXP, D])
            self.moe_ln_g = din("moe_ln_g_b", [128, D])
            self.moe_ln_b = din("moe_ln_b_b", [128, D])
        self.out = T(Buf("out", dram=True), nc_out(self.nc, "out", [TOK, D], F32))
        ds = self.dscr
        self.qT_d = ds("qT_d", [8, 96, TOK], BF16)
        self.kT_d = ds("kT_d", [8, 96, TOK], BF16)
        self.V_d = ds("V_d", [TOK, 1024], BF16)
        self.yaT_d = ds("yaT_d", [512, TOK], BF16)
        self.x1_d = ds("x1_d", [TOK, D])
        self.x2_d = ds("x2_d", [TOK, D])
        self.uT_d = ds("uT_d", [D, TOK])
        self.gT_d = ds("gT_d", [D, TOK], BF16)
        self.x3_d = ds("x3_d", [TOK, D])
        self.scratch_list = [self.qT_d, self.kT_d, self.V_d, self.yaT_d, self.x1_d, self.x2_d,
                             self.uT_d, self.gT_d, self.x3_d]

    def load_consts(self):
        S, A = self.S, self.A
        self.cstS = A.alloc("cstS", [128, 1024])
        S.dma(self.cstS, self.cst, q="sp")
        c = self.cstS
        self.ident = c[:, 0:128]
        self.tril = c[:, 128:256]
        self.masknegf = c[:, 256:384]
        self.ropef = c[0:96, 384:385]
        self.ropesign = c[0:96, 385:386]
        self.halfpi = c[:, 386:387]
        self.eps_ln = c[:, 387:388]
        self.eps_rms = c[:, 388:389]
        self.jgrid = c[:, 512:640]
        self.ident_bf = A.alloc("ident_bf", [128, 128], BF16)
        S.copy(self.ident_bf, self.ident)
        self.maskneg = A.alloc("maskneg", [128, 128], BF16)
        S.copy(self.maskneg, self.masknegf)
        self.ones_f = A.alloc("ones_f", [128, 128])
        S.memset(self.ones_f, 1.0)
        self.ones_bf = A.alloc("ones_bf", [128, 128], BF16)
        S.memset(self.ones_bf, 1.0)

    def stage_A1(self):
        S, A, psb = self.S, self.A, self.psb
        w_in_b = A.alloc("w_in_b", [128, KT, 1664], BF16)
        wm = self.w_in_main.ap.rearrange("(kt p) n -> p kt n", p=128)
        for kt0 in range(0, KT, 2):
            S.dma(w_in_b[:, kt0:kt0 + 2, :], self.w_in_main.v(wm[:, kt0:kt0 + 2, :]), q="pool", join=(kt0 > 0))
        w_kpe = A.alloc("w_kpe", [128, KT, 192], BF16)
        S.dma(w_kpe, self.w_kpe2.v(self.w_kpe2.ap.rearrange("(kt p) n -> p kt n", p=128)), q="pool")
        w_uq = A.alloc("w_uq_b", [128, 3, 768], BF16)
        S.dma(w_uq, self.w_uq.v(self.w_uq.ap.rearrange("(kt p) n -> p kt n", p=128)), q="pool")
        w_uqs = A.alloc("w_uqs_b", [128, 3, 768], BF16)
        S.dma(w_uqs, self.w_uq_sw.v(self.w_uq_sw.ap.rearrange("(kt p) n -> p kt n", p=128)), q="pool")
        w_uk = A.alloc("w_uk_b", [128, 2, 512], BF16)
        S.dma(w_uk, self.w_uk.v(self.w_uk.ap.rearrange("(kt p) n -> p kt n", p=128)), q="pool")
        w_uv = A.alloc("w_uv_b", [128, 2, 512], BF16)
        S.dma(w_uv, self.w_uv.v(self.w_uv.ap.rearrange("(kt p) n -> p kt n", p=128)), q="pool")
        gmg = A.alloc("gmg", [128, 512])
        S.dma(gmg, self.gm_ln_g, q="sp")
        gmb = A.alloc("gmb", [128, 512])
        S.dma(gmb, self.gm_ln_b, q="sp")
        bsb = A.alloc("bsb", [128, 4, 512])
        S.dma(bsb, self.bs_b, q="sp")
        wsf = A.alloc("wsf", [128, 4, 128])
        S.dma(wsf, self.wsT, q="sp")
        wsb = A.alloc("wsb", [128, 4, 128], BF16)
        for h in range(4):
            S.tt(wsb[:, h, :], wsf[:, h, :], self.tril, ALU.mult, join=(h > 0))
        qn = A.alloc("qn", [128, 3])
        S.dma(qn, self.qnorm, q="sp")
        kvn = A.alloc("kvn", [128, 2])
        S.dma(kvn, self.kvnorm, q="sp")

        xtok = A.ring("xtok", 2, [128, D])
        xT = A.alloc("xT", [128, KT, 512], BF16)
        uT = A.alloc("uT", [128, 4, 512], BF16)
        scr = A.ring("scr", 8, [128, 512])
        vnr = A.ring("vn", 5, [128, 512], BF16)
        small = A.ring("small", 8, [128, 8])
        yaT = A.alloc("yaT", [128, 4, 512], BF16)
        cT = A.alloc("cT", [128, 5, 512])
        Rq = A.alloc("Rq", [128, 512])
        Rkv = A.alloc("Rkv", [128, 512])
        cqn = A.alloc("cqn", [128, 5, 512], BF16)
        posi = A.alloc("posi", [96, 512], I32)
        Ctab = A.alloc("Ctab", [96, 512])
        Stab = A.alloc("Stab", [96, 512])
        qT = A.alloc("qT", [96, 8, 512], BF16)
        kTt = A.alloc("kTt", [96, 8, 512], BF16)
        kper = A.alloc("kper", [96, 512])
        Vt = A.ring("Vt", 2, [128, 8, 128], BF16)
        for _vt in Vt.items:
            S.memset(_vt, 1.0, eng="pool")
        gen = Ring([psb[0], psb[1], psb[6], psb[7]])
        ps_ya = psb[2:6]

        for c in range(TOK // 512):
            tok0 = c * 512
            b = c // 4
            s0 = (c % 4) * 512
            self.load_xT(self.x, tok0, 4, xT, xtok, gen, self.ident)
            for h in range(4):
                ps = gen.next()
                for kt in range(KT):
                    S.mm(ps, w_in_b[:, kt, h * 128:(h + 1) * 128], xT[:, kt, :], start=(kt == 0), stop=(kt == KT - 1))
                S.act(uT[:, h, :], ps, AF.Gelu_apprx_tanh, join=(h > 0))
            vns = []
            for i in range(4):
                ps = gen.next()
                for kt in range(KT):
                    S.mm(ps, xT[:, kt, i * 128:(i + 1) * 128], w_in_b[:, kt, 512:1024], start=(kt == 0), stop=(kt == KT - 1))
                v = scr.next()
                S.act(v, ps, AF.Gelu_apprx_tanh)
                sm = small.next()
                st = sm[:, 0:6]
                S.add("dve", (lambda st, v: lambda e: e.bn_stats(st.ap, v.ap))(st, v), reads=[v], writes=[sm])
                sm2 = small.next()
                mv = sm2[:, 0:2]
                S.add("dve", (lambda mv, st: lambda e: e.bn_aggr(mv.ap, st.ap))(mv, st), reads=[sm], writes=[sm2])
                S.act(sm2[:, 2:3], sm2[:, 1:2], AF.Sqrt, bias=self.eps_ln, scale=1.0, join=True)
                S.recip(sm2[:, 3:4], sm2[:, 2:3], join=True)
                S.stt(sm2[:, 4:5], sm2[:, 0:1], -1.0, sm2[:, 3:4], ALU.mult, ALU.mult, join=True)
                vn0 = scr.next()
                S.act(vn0, v, AF.Identity, bias=sm2[:, 4:5], scale=sm2[:, 3:4])
                S.tt(vn0, vn0, gmg, ALU.mult, eng="pool")
                vn = vnr.next()
                S.tt(vn, vn0, gmb, ALU.add, eng="pool")
                vns.append(vn)
            ps_sq = ps_ya[0]
            ps_skv = ps_ya[1]
            for m in range(5):
                ps = gen.next()
                for kt in range(KT):
                    S.mm(ps, w_in_b[:, kt, 1024 + m * 128:1024 + (m + 1) * 128], xT[:, kt, :],
                         start=(kt == 0), stop=(kt == KT - 1))
                S.copy(cT[:, m, :], ps, eng="act", join=(m > 0))
                sq = scr.next()
                S.act(sq, ps, AF.Square)
                if m < 3:
                    S.mm(ps_sq, self.ones_f, sq, start=(m == 0), stop=(m == 2))
                else:
                    S.mm(ps_skv, self.ones_f, sq, start=(m == 3), stop=(m == 4))
            t = scr.next()
            S.act(t, ps_sq, AF.Sqrt, bias=self.eps_rms, scale=1.0 / 384.0)
            S.recip(Rq, t)
            t = scr.next()
            S.act(t, ps_skv, AF.Sqrt, bias=self.eps_rms, scale=1.0 / 256.0)
            S.recip(Rkv, t)
            for m in range(5):
                g = qn[:, m:m + 1] if m < 3 else kvn[:, m - 3:m - 2]
                S.stt(cqn[:, m, :], cT[:, m, :], g, Rq if m < 3 else Rkv, ALU.mult, ALU.mult, join=(m > 0))
            S.dma(posi, self.pos.v(self.pos.ap[b:b + 1, s0:s0 + 512].to_broadcast([96, 512])), q="sp")
            ang = scr.next()[0:96, :]
            S.copy(ang, posi)
            S.ts(ang, ang, self.ropef, ALU.mult)
            kk = scr.next()[0:96, :]
            S.ts(kk, ang, 1.0 / TWO_PI, ALU.mult, MAGIC, ALU.add)
            S.ts(kk, kk, MAGIC, ALU.subtract)
            r1 = scr.next()[0:96, :]
            S.stt(r1, kk, -C1, ang, ALU.mult, ALU.add)
            S.stt(r1, kk, -C2, r1, ALU.mult, ALU.add)
            S.ts(r1, r1, -PI_SAFE, ALU.max, PI_SAFE, ALU.min)
            S.act(Stab, r1, AF.Sin, scale=self.ropesign)
            S.stt(r1, r1, -1.0, r1, ALU.mult, ALU.max)
            S.act(Ctab, r1, AF.Sin, bias=self.halfpi[0:96, :], scale=-1.0)
            for h in range(8):
                ps = gen.next()
                ps2 = gen.next()
                for kt in range(3):
                    S.mm(ps[0:96, :], w_uq[:, kt, h * 96:(h + 1) * 96], cqn[:, kt, :], start=(kt == 0), stop=(kt == 2))
                for kt in range(3):
                    S.mm(ps2[0:96, :], w_uqs[:, kt, h * 96:(h + 1) * 96], cqn[:, kt, :], start=(kt == 0), stop=(kt == 2))
                t1 = scr.next()[0:96, :]
                S.tt(t1, ps[0:96, :], Ctab, ALU.mult)
                t2 = scr.next()[0:96, :]
                S.tt(t2, ps2[0:96, :], Stab, ALU.mult)
                S.tt(qT[:, h, :], t1, t2, ALU.add, eng="pool", join=(h > 0))
            S.dma(self.qT_d.v(self.qT_d.ap.rearrange("h d t -> d h t")[:, :, tok0:tok0 + 512]), qT, q="sp", join=True)
            ps = gen.next()
            ps2 = gen.next()
            for kt in range(KT):
                S.mm(ps[0:96, :], w_kpe[:, kt, 0:96], xT[:, kt, :], start=(kt == 0), stop=(kt == KT - 1))
            for kt in range(KT):
                S.mm(ps2[0:96, :], w_kpe[:, kt, 96:192], xT[:, kt, :], start=(kt == 0), stop=(kt == KT - 1))
            t1 = scr.next()[0:96, :]
            S.tt(t1, ps[0:96, :], Ctab, ALU.mult)
            t2 = scr.next()[0:96, :]
            S.tt(t2, ps2[0:96, :], Stab, ALU.mult)
            S.tt(kper, t1, t2, ALU.add, eng="pool")
            for h in range(8):
                ps = gen.next()
                for kt in range(2):
                    S.mm(ps[0:64, :], w_uk[:, kt, h * 64:(h + 1) * 64], cqn[:, 3 + kt, :], start=(kt == 0), stop=(kt == 1))
                S.copy(kTt[0:64, h, :], ps[0:64, :], eng="act", join=(h > 0))
                S.copy(kTt[64:96, h, :], kper[64:96, :], eng="pool", join=True)
            S.dma(self.kT_d.v(self.kT_d.ap.rearrange("h d t -> d h t")[:, :, tok0:tok0 + 512]), kTt, q="sp", join=True)
            for i in range(4):
                for h in range(4):
                    S.mm(ps_ya[h][:, i * 128:(i + 1) * 128], vns[i][:, h * 128:(h + 1) * 128], wsb[:, h, :],
                         start=True, stop=True, join=(i > 0))
            for h in range(4):
                t = scr.next()
                S.tt(t, ps_ya[h], bsb[:, h, :], ALU.add)
                S.tt(yaT[:, h, :], t, uT[:, h, :], ALU.mult, eng="pool", join=(h > 0))
            S.dma(self.yaT_d.v(self.yaT_d.ap.rearrange("(h p) t -> p h t", p=128)[:, :, tok0:tok0 + 512]), yaT,
                  q="sp", join=True)
            for i in range(4):
                ps = gen.next()
                for kt in range(2):
                    S.mm(ps, cqn[:, 3 + kt, i * 128:(i + 1) * 128], w_uv[:, kt, :], start=(kt == 0), stop=(kt == 1))
                vt = Vt.next()
                S.copy(vt[:, :, 0:64], ps.v(ps.ap.rearrange("p (h d) -> p h d", h=8)), eng="act", join=True)
                S.dma(self.V_d.v(self.V_d.ap[tok0 + i * 128:tok0 + (i + 1) * 128, :].rearrange("p (h d) -> p h d", h=8)),
                      vt, q="sp", join=True)

    def stage_A2(self):
        S, A, psb = self.S, self.A, self.psb
        w_oa = A.alloc("w_oa", [128, 4, D], BF16)
        S.dma(w_oa, self.w_out_a.v(self.w_out_a.ap.rearrange("(kt p) n -> p kt n", p=128)), q="pool")
        w_ob = A.alloc("w_ob", [128, 4, D], BF16)
        S.dma(w_ob, self.w_out_b.v(self.w_out_b.ap.rearrange("(kt p) n -> p kt n", p=128)), q="pool")
        lng = A.alloc("lng", [128, D])
        S.dma(lng, self.ab_ln_g, q="sp")
        lnb = A.alloc("lnb", [128, D])
        S.dma(lnb, self.ab_ln_b, q="sp")
        KTs = A.alloc("KTs", [96, 8, SEQ], BF16)
        Vs = A.alloc("Vs", [128, 16, 1024], BF16)
        qTc = A.ring("qTc", 2, [96, 8, 512], BF16)
        yaTc = A.ring("yaTc", 2, [128, 4, 512], BF16)
        ybT = A.alloc("ybT", [128, 4, 512], BF16)
        PT = A.ring("PT", 3, [128, 512], BF16)
        rec = A.ring("rec", 2, [64, 512])
        xres = A.ring("xres", 2, [128, D])
        z = A.ring("z", 2, [128, D])
        zn = A.ring("zn", 2, [128, D])
        xo = A.ring("xo", 2, [128, D])
        small = A.ring("smallA2", 4, [128, 24])
        ps_s = Ring([psb[0], psb[1], psb[2]])
        ps_nd = Ring([psb[3], psb[4], psb[5]])
        ps_o = Ring([psb[6], psb[7]])
        scale = 96.0 ** -0.5
        for b in range(NB):
            for h in range(8):
                S.dma(KTs[:, h, :], self.kT_d[h, :, b * SEQ:(b + 1) * SEQ], q="sp", join=(h > 0))
            S.dma(Vs, self.V_d.v(self.V_d.ap[b * SEQ:(b + 1) * SEQ, :].rearrange("(n p) c -> p n c", p=128)), q="sp")
            for cs in range(4):
                tok0 = b * SEQ + cs * 512
                q = qTc.next()
                S.dma(q, self.qT_d.v(self.qT_d.ap.rearrange("h d t -> d h t")[:, :, tok0:tok0 + 512]), q="sp")
                ya = yaTc.next()
                S.dma(ya, self.yaT_d.v(self.yaT_d.ap.rearrange("(h p) t -> p h t", p=128)[:, :, tok0:tok0 + 512]), q="sp")
                nkt = 4 * (cs + 1)
                for h in range(8):
                    pnum = ps_nd.next()

                    def qk(kt, h=h):
                        j = kt - 4 * cs
                        c0 = 0 if j < 0 else j * 128
                        ps = ps_s.next()
                        S.mm(ps[:, c0:512], KTs[:, h, kt * 128:(kt + 1) * 128], q[:, h, c0:512],
                             start=True, stop=(j < 0))
                        if j >= 0:
                            S.mm(ps[:, c0:c0 + 128], self.ident_bf, self.maskneg, start=False, stop=True)
                        pt = PT.next()
                        S.act(pt[:, c0:512], ps[:, c0:512], AF.Exp, scale=scale)
                        return pt, c0

                    def pv(kt, pt, c0, h=h, pnum=pnum):
                        S.mm(pnum[:, c0:512], Vs[:, kt, h * 128:(h + 1) * 128], pt[:, c0:512],
                             start=(kt == 0), stop=(kt == nkt - 1))

                    pend = [qk(0)]
                    for kt in range(nkt):
                        if kt + 1 < nkt:
                            pend.append(qk(kt + 1))
                        pt, c0 = pend.pop(0)
                        pv(kt, pt, c0)
                    r = rec.next()
                    S.recip(r, pnum[64:128, :])
                    po = (h % 2) * 64
                    S.tt(ybT[po:po + 64, h // 2, :], pnum[0:64, :], r, ALU.mult, join=(h > 0))
                for i in range(4):
                    xr = xres.next()
                    S.dma(xr, self.x[tok0 + i * 128:tok0 + (i + 1) * 128, :], q="sp")
                    pso = [ps_o.next(), ps_o.next()]
                    for hf in range(2):
                        for kk in range(4):
                            S.mm(pso[hf], ya[:, kk, i * 128:(i + 1) * 128], w_oa[:, kk, hf * 512:(hf + 1) * 512],
                                 start=(kk == 0), stop=False)
                        for hh in range(4):
                            S.mm(pso[hf], ybT[:, hh, i * 128:(i + 1) * 128], w_ob[:, hh, hf * 512:(hf + 1) * 512],
                                 start=False, stop=(hh == 3))
                    sm = small.next()
                    self.ln_epilogue(pso[0], pso[1], xr, lng, lnb, z.next(),
                                     sm.v(sm.ap[:, 0:12].rearrange("p (a b) -> p a b", a=2)), sm[:, 12:14], sm[:, 14:15],
                                     sm[:, 15:16], sm[:, 16:17], zn.next(), xo.next(),
                                     self.x1_d[tok0 + i * 128:tok0 + (i + 1) * 128, :], self.eps_ln)

    def ffn_stage(self, src_d, dst_d, units, lng_d, lnb_d, router=None):
        S, A, psb = self.S, self.A, self.psb
        NT = 8
        lng = A.alloc("lng", [128, D])
        S.dma(lng, lng_d, q="sp")
        lnb = A.alloc("lnb", [128, D])
        S.dma(lnb, lnb_d, q="sp")
        maxh = max(u[3] for u in units)
        xT = A.alloc("xT", [128, KT, NT * 128], BF16)
        hT = A.alloc("hT", [128, maxh, NT * 128], BF16)
        acc = A.alloc("acc", [128, NT, D])
        xtok = A.ring("xtok", 2, [128, D])
        wgr = A.ring("wg", 3, [128, KT, 256], BF16)
        wur = A.ring("wu", 3, [128, KT, 256], BF16)
        wd = A.alloc("wd", [128, maxh, D], BF16)
        sil = A.ring("sil", 3, [128, 512])
        z = A.ring("z", 2, [128, D])
        zn = A.ring("zn", 2, [128, D])
        small = A.ring("smallF", 4, [128, 24])
        ps_gu = Ring([psb[0], psb[1], psb[2], psb[3]])
        ps_dn = Ring([psb[4], psb[5], psb[6], psb[7]])
        if router is not None:
            rt = A.alloc("rt", [128, KT, N_EXP])
            S.dma(rt, router, q="sp")
            xTf = A.ring("xTf", 2, [128, KT, 128])
            comb = A.alloc("comb", [128, NT, N_EXP])
            rsm = A.ring("rsm", 4, [128, 32])
        for c in range(TOK // (NT * 128)):
            tok0 = c * NT * 128
            if router is None:
                self.load_xT(src_d, tok0, NT, xT, xtok, ps_dn, self.ident)
            else:
                cur = {}

                def cb(i, half, srcv, ev, cur=cur):
                    if half == 0:
                        cur["t"] = xTf.next()
                    S.copy(cur["t"][:, half * 4:half * 4 + 4, :], srcv, eng=ev, join=(half == 1))
                    lvl = int(getattr(self, "rlevel", 4))
                    if half == 1 and lvl == 0:
                        S.memset(comb[:, i, :], 0.125, join=True)
                        return
                    if half == 1:
                        xf = cur["t"]
                        ps = ps_gu.next()
                        for kt in range(KT):
                            S.mm(ps[:, 0:N_EXP], xf[:, kt, :], rt[:, kt, :], start=(kt == 0), stop=(kt == KT - 1))
                        r = rsm.next()
                        lg = r[:, 0:8]
                        S.copy(lg, ps[:, 0:N_EXP])
                        lvl = int(getattr(self, "rlevel", 4))
                        if lvl < 4:
                            S.memset(comb[:, i, :], 0.125, join=True)
                        if lvl < 2:
                            return
                        mx = r[:, 8:16]
                        S.add("dve", (lambda mx, lg: lambda e: e.max(out=mx.ap, in_=lg.ap))(mx, lg), reads=[r], writes=[r], join=True)
                        if lvl < 3:
                            return
                        dd = r[:, 16:17]
                        S.tt(dd, r[:, 9:10], r[:, 8:9], ALU.subtract, join=True)
                        S.act(r[:, 17:18], dd, AF.Exp, join=True)
                        S.ts(r[:, 18:19], r[:, 17:18], 1.0, ALU.add, join=True)
                        S.recip(r[:, 19:20], r[:, 18:19], join=True)
                        S.ts(r[:, 20:21], r[:, 19:20], -1.0, ALU.mult, 1.0, ALU.add, join=True)
                        if lvl < 4:
                            return
                        r2 = rsm.next()
                        S.ts(r2[:, 0:8], lg, r[:, 8:9], ALU.is_equal)
                        S.ts(r2[:, 8:16], lg, r[:, 9:10], ALU.is_equal, join=True)
                        S.ts(r2[:, 16:24], lg, r[:, 8:9], ALU.is_lt, join=True)
                        S.tt(r2[:, 8:16], r2[:, 8:16], r2[:, 16:24], ALU.mult, join=True)
                        S.ts(r2[:, 0:8], r2[:, 0:8], r[:, 19:20], ALU.mult, join=True)
                        S.stt(comb[:, i, :], r2[:, 8:16], r[:, 20:21], r2[:, 0:8], ALU.mult, ALU.add, join=True)

                self.load_xT(src_d, tok0, NT, xT, xtok, ps_dn, self.ident, xTf_cb=cb)
            for ui, (wg_ap, wu_ap, wd_ap, nh, ex) in enumerate(units):
                wdv = wd_ap.rearrange("(j p) n -> p j n", p=128)

                def load_wd(wdv=wdv, nh=nh):
                    nsplit = 2
                    per = (nh + nsplit - 1) // nsplit
                    for sidx in range(nsplit):
                        ja, jb = sidx * per, min(nh, (sidx + 1) * per)
                        S.dma(wd[:, ja:jb, :], T(self.wsrc, wdv[:, ja:jb, :]), q="pool", join=(sidx > 0))
                wgv = wg_ap.rearrange("(kt p) n -> p kt n", p=128)
                wuv = wu_ap.rearrange("(kt p) n -> p kt n", p=128)
                for j0 in range(0, nh, 2):
                    nj = min(2, nh - j0)
                    wgb = wgr.next()
                    wub = wur.next()
                    S.dma(wgb[:, :, 0:nj * 128], T(self.wsrc, wgv[:, :, j0 * 128:(j0 + nj) * 128]), q="pool")
                    S.dma(wub[:, :, 0:nj * 128], T(self.wsrc, wuv[:, :, j0 * 128:(j0 + nj) * 128]), q="pool")
                    if j0 == 6:
                        load_wd()
                    for jj in range(nj):
                        j = j0 + jj
                        for blk in range(NT // 4):
                            pg = ps_gu.next()
                            pu = ps_gu.next()
                            for kt in range(KT):
                                S.mm(pg, wgb[:, kt, jj * 128:(jj + 1) * 128], xT[:, kt, blk * 512:(blk + 1) * 512],
                                     start=(kt == 0), stop=(kt == KT - 1))
                            for kt in range(KT):
                                S.mm(pu, wub[:, kt, jj * 128:(jj + 1) * 128], xT[:, kt, blk * 512:(blk + 1) * 512],
                                     start=(kt == 0), stop=(kt == KT - 1))
                            sl = sil.next()
                            S.act(sl, pg, AF.Silu)
                            S.tt(hT[:, j, blk * 512:(blk + 1) * 512], pu, sl, ALU.mult, join=not (j == 0 and blk == 0))
                for i in range(NT):
                    for hf in range(2):
                        pd = ps_dn.next()
                        for j in range(nh):
                            S.mm(pd, hT[:, j, i * 128:(i + 1) * 128], wd[:, j, hf * 512:(hf + 1) * 512],
                                 start=(j == 0), stop=(j == nh - 1))
                        dst = acc[:, i, hf * 512:(hf + 1) * 512]
                        first = (ui == 0)
                        if ex is None or getattr(self, "noscale", False):
                            if first:
                                S.copy(dst, pd, eng="act", join=not (i == 0 and hf == 0))
                            else:
                                S.tt(dst, pd, dst, ALU.add, join=True)
                        else:
                            cw = comb[:, i, ex:ex + 1]
                            if first:
                                S.act(dst, pd, AF.Identity, scale=cw, join=not (i == 0 and hf == 0))
                            else:
                                S.stt(dst, pd, cw, dst, ALU.mult, ALU.add, join=True)
            for i in range(NT):
                xr = xtok.next()
                S.dma(xr, src_d[tok0 + i * 128:tok0 + (i + 1) * 128, :], q="sp")
                sm = small.next()
                znt = zn.next()
                self.ln_epilogue(acc[:, i, 0:512], acc[:, i, 512:1024], xr, lng, lnb, z.next(),
                                 sm.v(sm.ap[:, 0:12].rearrange("p (a b) -> p a b", a=2)), sm[:, 12:14], sm[:, 14:15],
                                 sm[:, 15:16], sm[:, 16:17], znt, znt,
                                 dst_d[tok0 + i * 128:tok0 + (i + 1) * 128, :], self.eps_ln)

    def stage_B(self):
        self.wsrc = Buf("wsrc", dram=True)
        units = []
        half = D_FF // 2
        for u in range(2):
            units.append((self.ffd_wg.ap[:, u * half:(u + 1) * half], self.ffd_wu.ap[:, u * half:(u + 1) * half],
                          self.ffd_wd.ap[u * half:(u + 1) * half, :], half // 128, None))
        self.ffn_stage(self.x1_d, self.x2_d, units, self.ffd_ln_g, self.ffd_ln_b)

    def stage_D(self):
        self.wsrc = Buf("wsrc", dram=True)
        units = []
        half = D_EXP // 2
        for e in range(N_EXP):
            for u in range(2):
                units.append((self.moe_wg.ap[e, :, u * half:(u + 1) * half], self.moe_wu.ap[e, :, u * half:(u + 1) * half],
                              self.moe_wd.ap[e, u * half:(u + 1) * half, :], half // 128, e))
        mode = getattr(self, "mode", "full")
        if mode == "nr1":
            units = [(a, b, c, n, None) for (a, b, c, n, e) in units[:2]]
            self.ffn_stage(self.x3_d, self.out, units, self.moe_ln_g, self.moe_ln_b)
            return
        if mode == "norouter":
            units = [(a, b, c, n, None) for (a, b, c, n, e) in units]
            self.ffn_stage(self.x3_d, self.out, units, self.moe_ln_g, self.moe_ln_b)
            return
        if mode == "two":
            units = units[:4]
        self.ffn_stage(self.x3_d, self.out, units, self.moe_ln_g, self.moe_ln_b, router=self.router)

    def stage_Ca(self):
        S, A, psb = self.S, self.A, self.psb
        w = A.alloc("cwin", [128, KT, D], BF16)
        S.dma(w, self.c_w_in.v(self.c_w_in.ap.rearrange("(kt p) n -> p kt n", p=128)), q="pool")
        xtok = A.ring("xtok", 2, [128, D])
        xT = A.alloc("xT", [128, KT, 512], BF16)
        uT = A.ring("uTo", 2, [128, KT, 512])
        gen = Ring(psb)
        for c in range(TOK // 512):
            tok0 = c * 512
            self.load_xT(self.x2_d, tok0, 4, xT, xtok, gen, self.ident)
            u = uT.next()
            for m in range(KT):
                ps = gen.next()
                for kt in range(KT):
                    S.mm(ps, w[:, kt, m * 128:(m + 1) * 128], xT[:, kt, :], start=(kt == 0), stop=(kt == KT - 1))
                S.copy(u[:, m, :], ps, eng="act" if m % 2 else "dve", join=(m > 0))
            S.dma(self.uT_d.v(self.uT_d.ap.rearrange("(m p) t -> p m t", p=128)[:, :, tok0:tok0 + 512]), u, q="sp", join=True)

    s5e = ("dve", "dve", "dve", "dve", "dve", "dve", "pool")

    def stage_Cb(self):
        S, A, psb = self.S, self.A, self.psb
        NM = 32
        are = A.alloc("are", [128, NM]); S.dma(are, self.s5_are, q="sp")
        aim = A.alloc("aim", [128, NM]); S.dma(aim, self.s5_aim, q="sp")
        lst = A.alloc("lst", [128, NM]); S.dma(lst, self.s5_ls, q="sp")
        dsk = A.alloc("dsk", [128, 8]); S.dma(dsk, self.s5_d, q="sp")
        BTr = A.alloc("BTr", [128, 8, 4, 128], BF16)
        BTi = A.alloc("BTi", [128, 8, 4, 128], BF16)
        CTr = A.alloc("CTr", [128, 8, 4, 128], BF16)
        CTi = A.alloc("CTi", [128, 8, 4, 128], BF16)
        for (dst, src, k) in ((BTr, self.s5_BT, 0), (BTi, self.s5_BT, 1), (CTr, self.s5_CT, 0), (CTi, self.s5_CT, 1)):
            S.dma(dst, src.v(src.ap[k].rearrange("kt p m s -> p kt m s")), q="pool")
        S.ts(CTi, CTi, -1.0, ALU.mult, eng="pool")
        CTrn = A.alloc("CTrn", [128, 8, 4, 128], BF16)
        S.ts(CTrn, CTr, -1.0, ALU.mult, eng="pool")
        mask = A.alloc("smask", [128, 512]); S.dma(mask, self.scanmask, q="sp")
        sc = A.alloc("s5sc", [128, 16, NM])
        dl = sc[:, 0, :]
        S.act(dl, lst, AF.Exp)
        rho = sc[:, 1, :]; S.tt(rho, dl, are, ALU.mult)
        th = sc[:, 2, :]; S.tt(th, dl, aim, ALU.mult)
        Einr = A.alloc("Einr", [128, NM, 128]); Eini = A.alloc("Eini", [128, NM, 128])
        Eoutr = A.alloc("Eoutr", [128, NM, 128]); Eouti = A.alloc("Eouti", [128, NM, 128])
        tm = A.mark()
        jg = self.jgrid
        jg3 = jg.v(jg.ap[:, None, :].to_broadcast([128, NM, 128]))

        def bc(t):
            return t.v(t.ap[:, :, None].to_broadcast([128, NM, 128]))
        ang = A.alloc("ang", [128, NM, 128])
        S.tt(ang, jg3, bc(th), ALU.mult)
        kk = A.alloc("kk", [128, NM, 128])
        S.ts(kk, ang, 1.0 / TWO_PI, ALU.mult, MAGIC, ALU.add)
        S.ts(kk, kk, MAGIC, ALU.subtract)
        r1 = A.alloc("r1", [128, NM, 128])
        S.stt(r1, kk, -C1, ang, ALU.mult, ALU.add)
        S.stt(r1, kk, -C2, r1, ALU.mult, ALU.add)
        S.ts(r1, r1, -PI_SAFE, ALU.max, PI_SAFE, ALU.min)
        sn = ang
        S.act(sn, r1, AF.Sin)
        S.stt(r1, r1, -1.0, r1, ALU.mult, ALU.max)
        cs_ = kk
        S.act(cs_, r1, AF.Sin, bias=self.halfpi, scale=-1.0)
        lm = r1
        S.tt(lm, jg3, bc(rho), ALU.mult)
        mo = A.alloc("mo", [128, NM, 128])
        S.act(mo, lm, AF.Exp)
        S.tt(Eoutr, mo, cs_, ALU.mult)
        S.tt(Eouti, mo, sn, ALU.mult)
        S.act(mo, lm, AF.Exp, scale=-1.0)
        er = lm
        S.tt(er, mo, cs_, ALU.mult)
        ei = A.alloc("ei", [128, NM, 128])
        S.tt(ei, mo, sn, ALU.mult)
        S.ts(ei, ei, -1.0, ALU.mult)
        abr = sc[:, 3, :]; S.copy(abr, Eoutr[:, :, 1])
        abi = sc[:, 4, :]; S.copy(abi, Eouti[:, :, 1])
        am1 = sc[:, 5, :]; S.ts(am1, abr, -1.0, ALU.add)
        den = sc[:, 6, :]; S.tt(den, are, are, ALU.mult)
        t0 = sc[:, 7, :]; S.tt(t0, aim, aim, ALU.mult)
        S.tt(den, den, t0, ALU.add)
        rden = sc[:, 8, :]; S.recip(rden, den)
        cre = sc[:, 9, :]; S.tt(cre, am1, are, ALU.mult)
        S.tt(t0, abi, aim, ALU.mult); S.tt(cre, cre, t0, ALU.add); S.tt(cre, cre, rden, ALU.mult)
        cim = sc[:, 10, :]; S.tt(cim, abi, are, ALU.mult)
        S.tt(t0, am1, aim, ALU.mult); S.tt(cim, cim, t0, ALU.subtract); S.tt(cim, cim, rden, ALU.mult)
        S.tt(Einr, er, bc(cre), ALU.mult)
        S.tt(mo, ei, bc(cim), ALU.mult)
        S.tt(Einr, Einr, mo, ALU.subtract)
        S.tt(Eini, er, bc(cim), ALU.mult)
        S.tt(mo, ei, bc(cre), ALU.mult)
        S.tt(Eini, Eini, mo, ALU.add)
        Gr = sc[:, 11, :]; Gi = sc[:, 12, :]
        t1 = sc[:, 13, :]
        S.tt(Gr, Eoutr[:, :, 127], abr, ALU.mult)
        S.tt(t1, Eouti[:, :, 127], abi, ALU.mult)
        S.tt(Gr, Gr, t1, ALU.subtract)
        S.tt(Gi, Eoutr[:, :, 127], abi, ALU.mult)
        S.tt(t1, Eouti[:, :, 127], abr, ALU.mult)
        S.tt(Gi, Gi, t1, ALU.add)
        S.barrier()
        A.reset(tm)
        uld = A.ring("uld", 2, [128, 512])
        uf2 = A.ring("uf2", 2, [128, 512])
        ub = A.ring("ub", 8, [128, 512], BF16)
        ringB = A.ring("rB", 24, [128, 4, 128], BF16)
        ringF = A.ring("rF", 12, [128, 4, 128])
        Hc = [[A.alloc(f"Hc{b}_{kt}", [128, 2, 4]) for kt in range(KT)] for b in range(NB)]
        cin = A.ring("cin", 8, [128, 4, 4])
        yv = A.ring("yv", 2, [128, 512])
        go = A.ring("go", 2, [128, 512], BF16)
        ps_bu = Ring([psb[0], psb[1], psb[2], psb[3]])
        ps_y = Ring([psb[4], psb[5], psb[6], psb[7]])
        uTv = self.uT_d.ap.rearrange("(m p) t -> p m t", p=128)
        gTv = self.gT_d.ap.rearrange("(m p) t -> p m t", p=128)
        for b in range(NB):
            for kt in range(KT):
                S.memset(Hc[b][kt], 0.0, eng="pool")

        def st1(it):
            ch = it["ch"]; kt = ch["kt"]; k = it["k"]; m0 = kt * 4
            if k == 0:
                ul = uld.next()
                S.dma(ul, self.uT_d.v(uTv[:, kt, ch["tok0"]:ch["tok0"] + 512]), q="sp")
                ch["ub"] = ub.next()
                S.copy(ch["ub"], ul, eng="act")
            u_b = ch["ub"]
            pr = ps_bu.next(); pi = ps_bu.next()
            for ml in range(4):
                S.mm(pr[:, ml * 128:(ml + 1) * 128], BTr[:, kt, ml, :], u_b[:, k * 128:(k + 1) * 128],
                     start=True, stop=True, join=(ml > 0))
            for ml in range(4):
                S.mm(pi[:, ml * 128:(ml + 1) * 128], BTi[:, kt, ml, :], u_b[:, k * 128:(k + 1) * 128],
                     start=True, stop=True, join=(ml > 0))
            pr3 = pr.v(pr.ap.rearrange("p (a b) -> p a b", a=4))
            pi3 = pi.v(pi.ap.rearrange("p (a b) -> p a b", a=4))
            eir = Einr[:, m0:m0 + 4, :]; eii = Eini[:, m0:m0 + 4, :]
            a = [ringB.next() for _ in range(4)]
            S.tt(a[0], pr3, eir, ALU.mult)
            S.tt(a[1], pi3, eii, ALU.mult)
            S.tt(a[2], pi3, eir, ALU.mult)
            S.tt(a[3], pr3, eii, ALU.mult)
            it["a"] = a

        def st2(it):
            a = it["a"]
            kr = ringF.next(); ki = ringF.next()
            S.tt(kr, a[0], a[1], ALU.subtract, eng=self.s5e[0])
            S.tt(ki, a[2], a[3], ALU.add, eng=self.s5e[1])
            it["kr"] = kr; it["ki"] = ki

        def st3(it):
            ch = it["ch"]; kt = ch["kt"]; m0 = kt * 4
            kr = it["kr"]; ki = it["ki"]
            H = Hc[ch["b"]][kt]
            Hr = H[:, 0, :]; Hi = H[:, 1, :]
            g_r = Gr[:, m0:m0 + 4]; g_i = Gi[:, m0:m0 + 4]
            ci = cin.next()
            te = self.s5e[6]
            S.tt(ci[:, 0, :], Hr, g_r, ALU.mult, eng=te)
            S.tt(ci[:, 1, :], Hi, g_i, ALU.mult, join=True, eng=te)
            S.tt(ci[:, 2, :], Hr, g_i, ALU.mult, join=True, eng=te)
            S.tt(ci[:, 3, :], Hi, g_r, ALU.mult, join=True, eng=te)
            S.tt(ci[:, 0, :], ci[:, 0, :], ci[:, 1, :], ALU.subtract, join=True, eng=te)
            S.tt(ci[:, 2, :], ci[:, 2, :], ci[:, 3, :], ALU.add, join=True, eng=te)
            S.tt(kr[:, :, 0], kr[:, :, 0], ci[:, 0, :], ALU.add, join=True, eng=te)
            S.tt(ki[:, :, 0], ki[:, :, 0], ci[:, 2, :], ALU.add, join=True, eng=te)
            hr_t = ringF.next(); hi_t = ringF.next()
            for (src, dst) in ((kr, hr_t), (ki, hi_t)):
                sf = src.ap.rearrange("p a b -> p (a b)")
                df = dst.ap.rearrange("p a b -> p (a b)")
                S.add("dve", (lambda df, sf: lambda e: e.tensor_tensor_scan(df, mask.ap, sf, 0.0, ALU.mult, ALU.add))(df, sf),
                      reads=[mask, src], writes=[dst])
            S.copy(H[:, 0, :], hr_t[:, :, 127], eng="act")
            S.copy(H[:, 1, :], hi_t[:, :, 127], eng="act", join=True)
            it["hr_t"] = hr_t; it["hi_t"] = hi_t

        def st4(it):
            kt = it["ch"]["kt"]; m0 = kt * 4
            eor = Eoutr[:, m0:m0 + 4, :]; eoi = Eouti[:, m0:m0 + 4, :]
            hr_t = it["hr_t"]; hi_t = it["hi_t"]
            bb = [ringB.next() for _ in range(4)]
            S.tt(bb[0], hr_t, eor, ALU.mult, eng=self.s5e[2])
            S.tt(bb[1], hi_t, eoi, ALU.mult, eng=self.s5e[3])
            S.tt(bb[2], hr_t, eoi, ALU.mult, eng=self.s5e[4])
            S.tt(bb[3], hi_t, eor, ALU.mult, eng=self.s5e[5])
            it["bb"] = bb

        def st5(it):
            ch = it["ch"]; kt = ch["kt"]; k = it["k"]
            bb = it["bb"]
            if k == 0:
                ch["py"] = ps_y.next()
            py = ch["py"]
            for wi, (W, P_) in enumerate(((CTr, bb[0]), (CTrn, bb[1]), (CTi, bb[2]), (CTi, bb[3]))):
                for ml in range(4):
                    S.mm(py[:, k * 128:(k + 1) * 128], W[:, kt, ml, :], P_[:, ml, :],
                         start=(wi == 0 and ml == 0), stop=(wi == 3 and ml == 3),
                         join=not (k == 0 and wi == 0 and ml == 0))
            if k == 3:
                u_f = uf2.next()
                S.dma(u_f, self.uT_d.v(uTv[:, kt, ch["tok0"]:ch["tok0"] + 512]), q="sp")
                y = yv.next()
                S.stt(y, u_f, dsk[:, kt:kt + 1], py, ALU.mult, ALU.add)
                g = go.next()
                S.act(g, y, AF.Gelu_apprx_tanh)
                S.dma(self.gT_d.v(gTv[:, kt, ch["tok0"]:ch["tok0"] + 512]), g, q="sp", join=True)

        its = []
        for b in range(NB):
            for blk in range(SEQ // 512):
                tok0 = b * SEQ + blk * 512
                for kq in range(0, KT, 4):
                    chains = [{"kt": kt, "b": b, "tok0": tok0} for kt in range(kq, kq + 4)]
                    for k in range(4):
                        for ch in chains:
                            its.append({"ch": ch, "k": k})
        stages = (st1, st2, st3, st4, st5)
        for t in range(len(its) + 4):
            for si in range(5):
                n = t - si
                if 0 <= n < len(its):
                    stages[si](its[n])

    def stage_Cc(self):
        S, A, psb = self.S, self.A, self.psb
        wg = A.alloc("gluw", [128, KT, D], BF16)
        S.dma(wg, self.glu_w.v(self.glu_w.ap.rearrange("(kt p) n -> p kt n", p=128)), q="pool")
        wo = A.alloc("cwout", [128, KT, D], BF16)
        S.dma(wo, self.c_w_out.v(self.c_w_out.ap.rearrange("(kt p) n -> p kt n", p=128)), q="pool")
        gb = A.alloc("glub", [128, 8]); S.dma(gb, self.glu_b, q="sp")
        lng = A.alloc("lng", [128, D]); S.dma(lng, self.c_ln_g, q="sp")
        lnb = A.alloc("lnb", [128, D]); S.dma(lnb, self.c_ln_b, q="sp")
        gTr = A.ring("gTc", 2, [128, KT, 512], BF16)
        zT = A.alloc("zT", [128, KT, 512], BF16)
        sg = A.ring("sg", 2, [128, 512])
        xres = A.ring("xres", 2, [128, D])
        z = A.ring("z", 2, [128, D]); zn = A.ring("zn", 2, [128, D])
        small = A.ring("smallC", 4, [128, 24])
        gen = Ring([psb[0], psb[1], psb[2], psb[3]])
        ps_o = Ring([psb[4], psb[5], psb[6], psb[7]])
        gTv = self.gT_d.ap.rearrange("(m p) t -> p m t", p=128)
        for c in range(TOK // 512):
            tok0 = c * 512
            g = gTr.next()
            S.dma(g, self.gT_d.v(gTv[:, :, tok0:tok0 + 512]), q="sp")
            for n in range(KT):
                ps = gen.next()
                for kt in range(KT):
                    S.mm(ps, wg[:, kt, n * 128:(n + 1) * 128], g[:, kt, :], start=(kt == 0), stop=(kt == KT - 1))
                s = sg.next()
                S.act(s, ps, AF.Sigmoid, bias=gb[:, n:n + 1], scale=1.0)
                S.tt(zT[:, n, :], g[:, n, :], s, ALU.mult, join=(n > 0))
            for i in range(4):
                xr = xres.next()
                S.dma(xr, self.x2_d[tok0 + i * 128:tok0 + (i + 1) * 128, :], q="sp")
                pso = [ps_o.next(), ps_o.next()]
                for hf in range(2):
                    for kt in range(KT):
                        S.mm(pso[hf], zT[:, kt, i * 128:(i + 1) * 128], wo[:, kt, hf * 512:(hf + 1) * 512],
                             start=(kt == 0), stop=(kt == KT - 1))
                sm = small.next()
                znt = zn.next()
                self.ln_epilogue(pso[0], pso[1], xr, lng, lnb, z.next(),
                                 sm.v(sm.ap[:, 0:12].rearrange("p (a b) -> p a b", a=2)), sm[:, 12:14], sm[:, 14:15],
                                 sm[:, 15:16], sm[:, 16:17], znt, znt,
                                 self.x3_d[tok0 + i * 128:tok0 + (i + 1) * 128, :], self.eps_ln)


def nc_out(nc, name, shape, dtype):
    return nc.dram_tensor(name, list(shape), dtype, kind="ExternalOutput").ap()


def _consts():
    c = np.zeros((128, 1024), np.float32)
    c[:, 0:128] = np.eye(128, dtype=np.float32)
    s = np.arange(128)[:, None]
    t = np.arange(128)[None, :]
    c[:, 128:256] = (s <= t).astype(np.float32)
    c[:, 256:384] = np.where(s <= t, 0.0, -30000.0).astype(np.float32)
    inv_freq = (1.0 / (np.float32(10000.0) ** (np.arange(0, 32, 2, dtype=np.float32) / np.float32(32)))).astype(np.float32)
    c[64:80, 384] = inv_freq
    c[80:96, 384] = inv_freq
    c[64:80, 385] = -1.0
    c[80:96, 385] = 1.0
    c[:, 386] = np.float32(math.pi / 2)
    c[:, 387] = LN_EPS
    c[:, 388] = RMS_EPS
    c[:, 512:640] = np.arange(128, dtype=np.float32)[None, :]
    m = np.ones((128, 512), np.float32)
    m[:, 0::128] = 0.0
    return c, m


def _bcast(v, n=128):
    return np.ascontiguousarray(np.broadcast_to(np.asarray(v, np.float32)[None, :], (n, v.shape[0])))


def _cols(v, k):
    return np.ascontiguousarray(np.asarray(v, np.float32).reshape(k, 128).T)


def prepare_shared(inp):
    f = lambda a: np.ascontiguousarray(np.asarray(a, np.float32))
    sh = {}
    cst, smask = _consts()
    sh["cst"] = cst
    sh["scanmask"] = smask
    w_in = f(inp["ab_w_in"][0])
    sh["w_in_main"] = np.ascontiguousarray(w_in[:, :1664])
    kp = w_in[:, 1664:1696]
    k2 = np.zeros((D, 192), np.float32)
    k2[:, 64:96] = kp
    k2[:, 96 + 64:96 + 80] = kp[:, 16:32]
    k2[:, 96 + 80:96 + 96] = kp[:, 0:16]
    sh["w_kpe2"] = k2
    sh["gm_ln_g_b"] = _bcast(inp["gm_ln_g"][0])
    sh["gm_ln_b_b"] = _bcast(inp["gm_ln_b"][0])
    ws = f(inp["gm_w_s"][0])
    sh["wsT"] = np.ascontiguousarray(ws.transpose(2, 0, 1))
    bs = f(inp["gm_b_s"][0])
    sh["bs_b"] = np.ascontiguousarray(np.broadcast_to(np.tile(bs, (1, 4))[None], (128, 4, 512)))
    sh["qnorm_c"] = _cols(inp["mla_q_norm"][0], 3)
    sh["kvnorm_c"] = _cols(inp["mla_kv_norm"][0], 2)
    wuq = f(inp["mla_w_uq"][0])
    sh["w_uq"] = wuq
    w3 = wuq.reshape(384, 8, 96)
    sw = np.zeros_like(w3)
    sw[:, :, 64:80] = w3[:, :, 80:96]
    sw[:, :, 80:96] = w3[:, :, 64:80]
    sh["w_uq_sw"] = np.ascontiguousarray(sw.reshape(384, 768))
    wukv = f(inp["mla_w_ukv"][0]).reshape(256, 8, 128)
    sh["w_uk"] = np.ascontiguousarray(wukv[:, :, 0:64].reshape(256, 512))
    sh["w_uv"] = np.ascontiguousarray(wukv[:, :, 64:128].reshape(256, 512))
    wout = f(inp["ab_w_out"][0])
    sh["w_out_a"] = np.ascontiguousarray(wout[0:512])
    sh["w_out_b"] = np.ascontiguousarray(wout[512:1024])
    sh["ab_ln_g_b"] = _bcast(inp["ab_ln_g"][0])
    sh["ab_ln_b_b"] = _bcast(inp["ab_ln_b"][0])
    sh["ffd_w_gate"] = f(inp["ffd_w_gate"][0])
    sh["ffd_w_up"] = f(inp["ffd_w_up"][0])
    sh["ffd_w_down"] = f(inp["ffd_w_down"][0])
    sh["ffd_ln_g_b"] = _bcast(inp["ffd_ln_g"][0])
    sh["ffd_ln_b_b"] = _bcast(inp["ffd_ln_b"][0])
    sh["c_w_in"] = f(inp["c_w_in"][0])

    def sm(a):
        a = f(a).reshape(32, 2, 64)
        return np.ascontiguousarray(a.transpose(1, 2, 0).reshape(128, 32))
    sh["s5_are_sm"] = sm(inp["s5_a_re"][0])
    sh["s5_aim_sm"] = sm(inp["s5_a_im"][0])
    ls = f(inp["s5_log_step"][0])
    sh["s5_ls_sm"] = sm(np.broadcast_to(ls[:, None], (64, 64)))
    BT = np.zeros((2, 8, 128, 4, 128), np.float32)
    CT = np.zeros((2, 8, 128, 4, 128), np.float32)
    for k, (Bk, Ck) in enumerate(((inp["s5_b_re"][0], inp["s5_c_re"][0]), (inp["s5_b_im"][0], inp["s5_c_im"][0]))):
        Bk = f(Bk)
        Ck = f(Ck)
        for g in range(64):
            kt, gl = divmod(g, 8)
            ml, two = divmod(gl, 2)
            BT[k, kt, gl * 16:(gl + 1) * 16, ml, two * 64:(two + 1) * 64] = Bk[g].T
            CT[k, kt, two * 64:(two + 1) * 64, ml, gl * 16:(gl + 1) * 16] = Ck[g].T
    sh["s5_BT"] = BT
    sh["s5_CT"] = CT
    sh["s5_d_c"] = _cols(inp["s5_d"][0], 8)
    sh["glu_w"] = f(inp["glu_w"][0])
    sh["glu_b_c"] = _cols(inp["glu_b"][0], 8)
    sh["c_w_out"] = f(inp["c_w_out"][0])
    sh["c_ln_g_b"] = _bcast(inp["c_ln_g"][0])
    sh["c_ln_b_b"] = _bcast(inp["c_ln_b"][0])
    sh["moe_router_c"] = np.ascontiguousarray(f(inp["moe_router"][0]).reshape(KT, 128, N_EXP).transpose(1, 0, 2))
    sh["moe_w_gate"] = f(inp["moe_w_gate"][0])
    sh["moe_w_up"] = f(inp["moe_w_up"][0])
    sh["moe_w_down"] = f(inp["moe_w_down"][0])
    sh["moe_ln_g_b"] = _bcast(inp["moe_ln_g"][0])
    sh["moe_ln_b_b"] = _bcast(inp["moe_ln_b"][0])
    return sh


_PROG_CACHE = {}


def get_prog(debug=False, upto="D"):
    key = (debug, upto)
    if key not in _PROG_CACHE:
        p = Prog(debug=debug, upto=upto)
        p.build()
        _PROG_CACHE[key] = p
    return _PROG_CACHE[key]


def run(inputs, debug=False, upto="D", cores=N_CORES):
    p = get_prog(debug, upto)
    sh = prepare_shared(inputs)
    x = np.asarray(inputs["x"], np.float32)
    pos = np.asarray(inputs["positions"], np.int32)
    in_maps = []
    for c in range(cores):
        m = {k: v for k, v in sh.items() if k in p.inputs}
        m["x"] = np.ascontiguousarray(x[c * NB:(c + 1) * NB].reshape(TOK, D))
        m["positions"] = np.ascontiguousarray(pos[c * NB:(c + 1) * NB])
        in_maps.append(m)
    res = run_bass_kernel_spmd(p.nc, in_maps, core_ids=list(range(cores)))
    return res


def kernel(**inputs):
    res = run(inputs)
    out = np.stack([np.asarray(r["out"], np.float32).reshape(NB, SEQ, D) for r in res.results], axis=0)
    return out.reshape(N_CORES * NB, SEQ, D)
```

```python
import math
import numpy as np
from contextlib import ExitStack
import concourse.bass as bass
import concourse.mybir as mybir
from concourse.bass_utils import run_bass_kernel_spmd

F32 = mybir.dt.float32
BF16 = mybir.dt.bfloat16
I32 = mybir.dt.int32
ALU = mybir.AluOpType
AF = mybir.ActivationFunctionType

N_CORES = 8
SEQ = 2048
NB = 2
TOK = NB * SEQ
D = 1024
KT = 8
ALPHA = 4.0 ** 0.25
LN_EPS = 1e-5
RMS_EPS = 1e-6
D_FF = 2816
N_EXP = 8
D_EXP = 3584
EPOCH = 12000
MAGIC = 12582912.0
TWO_PI = 2.0 * math.pi
C1 = 6.28125
C2 = TWO_PI - 6.28125
PI_SAFE = 3.141592


class Buf:
    __slots__ = ("name", "writers", "readers", "sem", "cnt", "dram", "excl", "prev")

    def __init__(self, name="", dram=False, excl=False):
        self.name = name
        self.dram = dram
        self.excl = excl
        self.prev = set()
        self.writers = []
        self.readers = []
        self.sem = None
        self.cnt = 0


class T:
    __slots__ = ("buf", "ap")

    def __init__(self, buf, ap):
        self.buf = buf
        self.ap = ap

    def __getitem__(self, idx):
        return T(self.buf, self.ap[idx])

    def v(self, ap):
        return T(self.buf, ap)

    def bc(self, shape):
        return T(self.buf, self.ap.to_broadcast(list(shape)))


class Op:
    __slots__ = ("eng", "fn", "deps", "dma", "tok", "signal", "idx", "wbuf")


class Sched:
    ENGS = ("pe", "act", "dve", "pool", "sp")

    def __init__(self, nc, stack):
        self.nc = nc
        self.stack = stack
        self.ops = []
        self.nsem = 0
        self.pending = {e: set() for e in self.ENGS}
        self.last_on = {}
        self.dma_since = {}

    def new_sem(self, name):
        self.nsem += 1
        return self.stack.enter_context(self.nc.semaphore(f"{name}_{self.nsem}"))

    def add(self, eng, fn, reads=(), writes=(), dma=False, join=False):
        op = Op()
        op.eng = eng
        op.fn = fn
        op.dma = dma
        op.idx = len(self.ops)
        op.signal = False
        op.tok = None
        op.wbuf = None
        deps = set()
        cz = self._compress
        for t in reads:
            deps.update(cz(t.buf.writers))
            if t.buf.excl:
                deps.update(cz([r for r in t.buf.readers if self.ops[r].eng != eng]))
        for t in writes:
            b = t.buf
            if not join:
                deps.update(cz(b.writers))
            else:
                deps.update(b.prev)
            deps.update(cz(b.readers))
        for t in reads:
            t.buf.readers.append(op.idx)
        for t in writes:
            b = t.buf
            if join:
                if b.readers:
                    b.prev = b.prev | cz(b.readers)
                b.writers.append(op.idx)
            else:
                b.prev = cz(b.writers) | cz(b.readers)
                b.writers = [op.idx]
            b.readers = []
        if dma:
            op.wbuf = writes[0].buf
            if writes[0].buf.dram and not reads[0].buf.dram:
                op.wbuf = reads[0].buf
            self.dma_since[id(op.wbuf)] = op.idx
        else:
            self.last_on[eng] = op.idx
        if self.pending[eng]:
            deps.update(self.pending[eng])
            self.pending[eng] = set()
        deps.discard(op.idx)
        op.deps = deps
        self.ops.append(op)
        return op

    def _compress(self, lst):
        n = len(self.ops)
        if len(lst) <= 1:
            return set(i for i in lst if i < n)
        out = set()
        last = {}
        for i in lst:
            if i >= n:
                continue
            o = self.ops[i]
            if o.dma:
                out.add(i)
            else:
                if last.get(o.eng, -1) < i:
                    last[o.eng] = i
        out.update(last.values())
        return out

    def barrier(self):
        deps = set(self.last_on.values()) | set(self.dma_since.values())
        for e in self.ENGS:
            self.pending[e] = set(deps) | self.pending[e]
        self.dma_since = {}

    def emit(self):
        nc = self.nc
        ops = self.ops
        for op in ops:
            for d in op.deps:
                p = ops[d]
                if p.dma:
                    continue
                if p.eng == "pe" and op.eng == "pe" and not op.dma:
                    continue
                p.signal = True
        eng_cnt = {e: 0 for e in self.ENGS}
        eng_sems = {e: [] for e in self.ENGS}
        for op in ops:
            if op.dma:
                b = op.wbuf
                if b.sem is None:
                    b.sem = self.new_sem("d")
                b.cnt += 16
                op.tok = (b.sem, b.cnt)
            elif op.signal:
                c = eng_cnt[op.eng]
                ep = c // EPOCH
                if ep >= len(eng_sems[op.eng]):
                    eng_sems[op.eng].append(self.new_sem("e" + op.eng))
                op.tok = (eng_sems[op.eng][ep], c % EPOCH + 1)
                eng_cnt[op.eng] = c + 1
        streams = {e: [] for e in self.ENGS}
        for op in ops:
            streams[op.eng].append(op)

        def run_stream(eng_name):
            def body(e):
                waited = {}
                for op in streams[eng_name]:
                    need = {}
                    for d in op.deps:
                        p = ops[d]
                        if (not p.dma) and p.eng == "pe" and eng_name == "pe" and not op.dma:
                            continue
                        s, v = p.tok
                        k = id(s)
                        if waited.get(k, 0) >= v:
                            continue
                        if k not in need or need[k][1] < v:
                            need[k] = (s, v)
                    for k, (s, v) in need.items():
                        e.wait_ge(s, v)
                        waited[k] = v
                    ins = op.fn(e)
                    if op.tok is not None:
                        ins.then_inc(op.tok[0], 16 if op.dma else 1)
            return body

        with nc.Block() as block:
            block.sync(run_stream("sp"))
            block.tensor(run_stream("pe"))
            block.vector(run_stream("dve"))
            block.scalar(run_stream("act"))
            block.gpsimd(run_stream("pool"))

    def dma(self, out, in_, q="sp", join=False, **kw):
        return self.add(q, lambda e: e.dma_start(out=out.ap, in_=in_.ap, **kw),
                        reads=[in_], writes=[out], dma=True, join=join)

    def mm(self, out, lhsT, rhs, start=True, stop=True, join=None, **kw):
        if join is None:
            join = not start
        return self.add("pe", lambda e: e.matmul(out.ap, lhsT.ap, rhs.ap, start=start, stop=stop, **kw),
                        reads=[lhsT, rhs], writes=[out], join=join)

    def transpose(self, out, in_, ident, join=False):
        return self.add("pe", lambda e: e.transpose(out.ap, in_.ap, ident.ap),
                        reads=[in_, ident], writes=[out], join=join)

    def act(self, out, in_, func, bias=None, scale=None, accum=None, join=False):
        reads = [in_]
        kw = {}
        if bias is not None:
            if isinstance(bias, T):
                reads.append(bias)
                kw["bias"] = bias.ap
            else:
                kw["bias"] = bias
        if scale is not None:
            if isinstance(scale, T):
                reads.append(scale)
                kw["scale"] = scale.ap
            else:
                kw["scale"] = scale
        writes = [out]
        if accum is not None:
            writes.append(accum)
            kw["accum_out"] = accum.ap
        return self.add("act", lambda e: e.activation(out.ap, in_.ap, func, **kw),
                        reads=reads, writes=writes, join=join)

    def tt(self, out, a, b, op, eng="dve", join=False):
        return self.add(eng, lambda e: e.tensor_tensor(out.ap, a.ap, b.ap, op),
                        reads=[a, b], writes=[out], join=join)

    def ts(self, out, a, s1, op0, s2=None, op1=None, eng="dve", join=False):
        reads = [a]
        v1, v2 = s1, s2
        if isinstance(s1, T):
            reads.append(s1)
            v1 = s1.ap
        if isinstance(s2, T):
            reads.append(s2)
            v2 = s2.ap
        if op1 is None:
            return self.add(eng, lambda e: e.tensor_scalar(out.ap, a.ap, v1, None, op0),
                            reads=reads, writes=[out], join=join)
        return self.add(eng, lambda e: e.tensor_scalar(out.ap, a.ap, v1, v2, op0, op1),
                        reads=reads, writes=[out], join=join)

    def stt(self, out, a, s, b, op0, op1, eng="dve", join=False):
        reads = [a, b]
        v = s
        if isinstance(s, T):
            reads.append(s)
            v = s.ap
        return self.add(eng, lambda e: e.scalar_tensor_tensor(out.ap, a.ap, v, b.ap, op0, op1),
                        reads=reads, writes=[out], join=join)

    def copy(self, out, in_, eng="dve", join=False):
        if eng == "act":
            return self.add("act", lambda e: e.copy(out.ap, in_.ap), reads=[in_], writes=[out], join=join)
        return self.add(eng, lambda e: e.tensor_copy(out.ap, in_.ap), reads=[in_], writes=[out], join=join)

    def memset(self, out, val, eng="dve", join=False):
        return self.add(eng, lambda e: e.memset(out.ap, val), reads=[], writes=[out], join=join)

    def recip(self, out, in_, join=False):
        return self.add("dve", lambda e: e.reciprocal(out.ap, in_.ap), reads=[in_], writes=[out], join=join)

    def wait_all(self, eng, bufs):
        return self.add(eng, lambda e: e.nop(), reads=bufs, writes=[])


def _dsize(dt):
    return 2 if dt == BF16 else 4


class Arena:
    def __init__(self, S, nwords):
        self.S = S
        self.nwords = nwords
        self.t = S.stack.enter_context(S.nc.sbuf_tensor("arena", [128, nwords], F32))
        self.off = 0
        self.peak = 0

    def alloc(self, name, shape, dtype=F32):
        p = shape[0]
        free = 1
        for s in shape[1:]:
            free *= s
        words = (free * _dsize(dtype) + 3) // 4
        words = (words + 7) // 8 * 8
        assert self.off + words <= self.nwords, f"arena overflow at {name}: {self.off}+{words}>{self.nwords}"
        ap = self.t[0:p, self.off:self.off + words]
        self.off += words
        self.peak = max(self.peak, self.off)
        if dtype != F32:
            ap = ap.bitcast(dtype)
        ap = ap[:, 0:free]
        if len(shape) == 3:
            ap = ap.rearrange("p (a b) -> p a b", a=shape[1])
        elif len(shape) == 4:
            ap = ap.rearrange("p (a b c) -> p a b c", a=shape[1], b=shape[2])
        return T(Buf(name), ap)

    def ring(self, name, n, shape, dtype=F32):
        return Ring([self.alloc(f"{name}{i}", shape, dtype) for i in range(n)])

    def mark(self):
        return self.off

    def reset(self, m):
        self.off = m


class Ring:
    def __init__(self, items):
        self.items = items
        self.i = 0

    def next(self):
        t = self.items[self.i % len(self.items)]
        self.i += 1
        return t


class Prog:
    def __init__(self, debug=False, upto="D"):
        self.debug = debug
        self.upto = upto
        self.nc = bass.Bass("TRN2", target_bir_lowering=False)
        self.inputs = {}

    def din(self, name, shape, dtype=F32):
        t = self.nc.dram_tensor(name, list(shape), dtype, kind="ExternalInput")
        self.inputs[name] = (tuple(shape), dtype)
        return T(Buf(name, dram=True), t.ap())

    def dscr(self, name, shape, dtype=F32):
        kind = "ExternalOutput" if self.debug else "Internal"
        t = self.nc.dram_tensor(name, list(shape), dtype, kind=kind)
        return T(Buf(name, dram=True), t.ap())

    def load_xT(self, src, tok0, ntiles, xT, xtok_ring, ps_ring, ident, keep=None, xTf_cb=None):
        S = self.S
        for i in range(ntiles):
            xt = xtok_ring.next() if keep is None else keep[i]
            S.dma(xt, src[tok0 + i * 128: tok0 + (i + 1) * 128, :], q="sp")
            for half in range(2):
                ps = ps_ring.next()
                for k4 in range(4):
                    kt = half * 4 + k4
                    S.transpose(ps[:, k4 * 128:(k4 + 1) * 128], xt[:, kt * 128:(kt + 1) * 128], ident,
                                join=(k4 > 0))
                dst = xT[:, half * 4:half * 4 + 4, i * 128:(i + 1) * 128]
                srcv = ps.v(ps.ap.rearrange("p (a b) -> p a b", a=4))
                ev = "dve" if (i + half) % 2 == 0 else "act"
                S.copy(dst, srcv, eng=ev)
                if xTf_cb is not None:
                    xTf_cb(i, half, srcv, ev)

    def ln_epilogue(self, zsrc_a, zsrc_b, xres, g_b, b_b, z, st, mv, sd, rstd, nmr, zn, xo, dst, eps_col):
        S = self.S
        for hf, zs in enumerate((zsrc_a, zsrc_b)):
            S.stt(z[:, hf * 512:(hf + 1) * 512], xres[:, hf * 512:(hf + 1) * 512], ALPHA, zs,
                  ALU.mult, ALU.add, join=(hf == 1))
        for hf in range(2):
            S.add("dve", (lambda hf: lambda e: e.bn_stats(st.ap[:, hf, :], z.ap[:, hf * 512:(hf + 1) * 512]))(hf),
                  reads=[z], writes=[st], join=(hf == 1))
        S.add("dve", lambda e: e.bn_aggr(mv.ap, st.ap.rearrange("p a b -> p (a b)")), reads=[st], writes=[mv])
        S.act(sd, mv[:, 1:2], AF.Sqrt, bias=eps_col, scale=1.0)
        S.recip(rstd, sd)
        S.stt(nmr, mv[:, 0:1], -1.0, rstd, ALU.mult, ALU.mult)
        S.act(zn, z, AF.Identity, bias=nmr, scale=rstd)
        S.tt(zn, zn, g_b, ALU.mult, eng="pool")
        S.tt(xo, zn, b_b, ALU.add, eng="pool")
        S.dma(dst, xo, q="sp", join=True)

    def build(self):
        nc = self.nc
        with ExitStack() as stack:
            S = Sched(nc, stack)
            self.S = S
            A = Arena(S, 50176)
            self.A = A
            psb = [T(Buf(f"ps{i}", excl=True), stack.enter_context(nc.psum_tensor(f"ps{i}", [128, 512], F32))[:, :])
                   for i in range(8)]
            self.psb = psb
            self.declare_io()
            self.load_consts()
            order = ["A1", "A2", "B", "Ca", "Cb", "Cc", "D"]
            stages = {"A1": self.stage_A1, "A2": self.stage_A2, "B": self.stage_B, "Ca": self.stage_Ca,
                      "Cb": self.stage_Cb, "Cc": self.stage_Cc, "D": self.stage_D}
            base = A.mark()
            if getattr(self, "only", None):
                order = [self.only]
                self.x3_d = self.x
            for name in order:
                A.reset(base)
                stages[name]()
                S.barrier()
                if name == self.upto:
                    break
            if self.upto != "D":
                pass
            finals = [self.out] + (self.scratch_list if self.debug else [])
            S.wait_all("sp", finals)
            S.emit()
        return nc

    def declare_io(self):
        din = self.din
        self.x = din("x", [TOK, D])
        self.pos = din("positions", [NB, SEQ], I32)
        self.cst = din("cst", [128, 1024])
        self.scanmask = din("scanmask", [128, 512])
        self.w_in_main = din("w_in_main", [D, 1664])
        self.w_kpe2 = din("w_kpe2", [D, 192])
        self.gm_ln_g = din("gm_ln_g_b", [128, 512])
        self.gm_ln_b = din("gm_ln_b_b", [128, 512])
        self.wsT = din("wsT", [128, 4, 128])
        self.bs_b = din("bs_b", [128, 4, 512])
        self.qnorm = din("qnorm_c", [128, 3])
        self.kvnorm = din("kvnorm_c", [128, 2])
        self.w_uq = din("w_uq", [384, 768])
        self.w_uq_sw = din("w_uq_sw", [384, 768])
        self.w_uk = din("w_uk", [256, 512])
        self.w_uv = din("w_uv", [256, 512])
        self.w_out_a = din("w_out_a", [512, D])
        self.w_out_b = din("w_out_b", [512, D])
        self.ab_ln_g = din("ab_ln_g_b", [128, D])
        self.ab_ln_b = din("ab_ln_b_b", [128, D])
        self.ffd_wg = din("ffd_w_gate", [D, D_FF])
        self.ffd_wu = din("ffd_w_up", [D, D_FF])
        self.ffd_wd = din("ffd_w_down", [D_FF, D])
        self.ffd_ln_g = din("ffd_ln_g_b", [128, D])
        self.ffd_ln_b = din("ffd_ln_b_b", [128, D])
        self.c_w_in = din("c_w_in", [D, D])
        self.s5_are = din("s5_are_sm", [128, 32])
        self.s5_aim = din("s5_aim_sm", [128, 32])
        self.s5_ls = din("s5_ls_sm", [128, 32])
        self.s5_BT = din("s5_BT", [2, 8, 128, 4, 128])
        self.s5_CT = din("s5_CT", [2, 8, 128, 4, 128])
        self.s5_d = din("s5_d_c", [128, 8])
        self.glu_w = din("glu_w", [D, D])
        self.glu_b = din("glu_b_c", [128, 8])
        self.c_w_out = din("c_w_out", [D, D])
        self.c_ln_g = din("c_ln_g_b", [128, D])
        self.c_ln_b = din("c_ln_b_b", [128, D])
        if self.upto == "D":
            self.router = din("moe_router_c", [128, KT, N_EXP])
            self.moe_wg = din("moe_w_gate", [N_EXP, D, D_EXP])
            self.moe_wu = din("moe_w_up", [N_EXP, D, D_EXP])
            self.moe_wd = din("moe_w_down", [N_EXP, D_EXP, D])
            self.moe_ln_g = din("moe_ln_g_b", [128, D])
            self.moe_ln_b = din("moe_ln_b_b", [128, D])
        self.out = T(Buf("out", dram=True), nc_out(self.nc, "out", [TOK, D], F32))
        ds = self.dscr
        self.qT_d = ds("qT_d", [8, 96, TOK], BF16)
        self.kT_d = ds("kT_d", [8, 96, TOK], BF16)
        self.V_d = ds("V_d", [TOK, 1024], BF16)
        self.yaT_d = ds("yaT_d", [512, TOK], BF16)
        self.x1_d = ds("x1_d", [TOK, D])
        self.x2_d = ds("x2_d", [TOK, D])
        self.uT_d = ds("uT_d", [D, TOK])
        self.gT_d = ds("gT_d", [D, TOK], BF16)
        self.x3_d = ds("x3_d", [TOK, D])
        self.scratch_list = [self.qT_d, self.kT_d, self.V_d, self.yaT_d, self.x1_d, self.x2_d,
                             self.uT_d, self.gT_d, self.x3_d]

    def load_consts(self):
        S, A = self.S, self.A
        self.cstS = A.alloc("cstS", [128, 1024])
        S.dma(self.cstS, self.cst, q="sp")
        c = self.cstS
        self.ident = c[:, 0:128]
        self.tril = c[:, 128:256]
        self.masknegf = c[:, 256:384]
        self.ropef = c[0:96, 384:385]
        self.ropesign = c[0:96, 385:386]
        self.halfpi = c[:, 386:387]
        self.eps_ln = c[:, 387:388]
        self.eps_rms = c[:, 388:389]
        self.jgrid = c[:, 512:640]
        self.ident_bf = A.alloc("ident_bf", [128, 128], BF16)
        S.copy(self.ident_bf, self.ident)
        self.maskneg = A.alloc("maskneg", [128, 128], BF16)
        S.copy(self.maskneg, self.masknegf)
        self.ones_f = A.alloc("ones_f", [128, 128])
        S.memset(self.ones_f, 1.0)
        self.ones_bf = A.alloc("ones_bf", [128, 128], BF16)
        S.memset(self.ones_bf, 1.0)

    def stage_A1(self):
        S, A, psb = self.S, self.A, self.psb
        w_in_b = A.alloc("w_in_b", [128, KT, 1664], BF16)
        wm = self.w_in_main.ap.rearrange("(kt p) n -> p kt n", p=128)
        for kt0 in range(0, KT, 2):
            S.dma(w_in_b[:, kt0:kt0 + 2, :], self.w_in_main.v(wm[:, kt0:kt0 + 2, :]), q="pool", join=(kt0 > 0))
        w_kpe = A.alloc("w_kpe", [128, KT, 192], BF16)
        S.dma(w_kpe, self.w_kpe2.v(self.w_kpe2.ap.rearrange("(kt p) n -> p kt n", p=128)), q="pool")
        w_uq = A.alloc("w_uq_b", [128, 3, 768], BF16)
        S.dma(w_uq, self.w_uq.v(self.w_uq.ap.rearrange("(kt p) n -> p kt n", p=128)), q="pool")
        w_uqs = A.alloc("w_uqs_b", [128, 3, 768], BF16)
        S.dma(w_uqs, self.w_uq_sw.v(self.w_uq_sw.ap.rearrange("(kt p) n -> p kt n", p=128)), q="pool")
        w_uk = A.alloc("w_uk_b", [128, 2, 512], BF16)
        S.dma(w_uk, self.w_uk.v(self.w_uk.ap.rearrange("(kt p) n -> p kt n", p=128)), q="pool")
        w_uv = A.alloc("w_uv_b", [128, 2, 512], BF16)
        S.dma(w_uv, self.w_uv.v(self.w_uv.ap.rearrange("(kt p) n -> p kt n", p=128)), q="pool")
        gmg = A.alloc("gmg", [128, 512])
        S.dma(gmg, self.gm_ln_g, q="sp")
        gmb = A.alloc("gmb", [128, 512])
        S.dma(gmb, self.gm_ln_b, q="sp")
        bsb = A.alloc("bsb", [128, 4, 512])
        S.dma(bsb, self.bs_b, q="sp")
        wsf = A.alloc("wsf", [128, 4, 128])
        S.dma(wsf, self.wsT, q="sp")
        wsb = A.alloc("wsb", [128, 4, 128], BF16)
        for h in range(4):
            S.tt(wsb[:, h, :], wsf[:, h, :], self.tril, ALU.mult, join=(h > 0))
        qn = A.alloc("qn", [128, 3])
        S.dma(qn, self.qnorm, q="sp")
        kvn = A.alloc("kvn", [128, 2])
        S.dma(kvn, self.kvnorm, q="sp")

        xtok = A.ring("xtok", 2, [128, D])
        xT = A.alloc("xT", [128, KT, 512], BF16)
        uT = A.alloc("uT", [128, 4, 512], BF16)
        scr = A.ring("scr", 8, [128, 512])
        vnr = A.ring("vn", 5, [128, 512], BF16)
        small = A.ring("small", 8, [128, 8])
        yaT = A.alloc("yaT", [128, 4, 512], BF16)
        cT = A.alloc("cT", [128, 5, 512])
        Rq = A.alloc("Rq", [128, 512])
        Rkv = A.alloc("Rkv", [128, 512])
        cqn = A.alloc("cqn", [128, 5, 512], BF16)
        posi = A.alloc("posi", [96, 512], I32)
        Ctab = A.alloc("Ctab", [96, 512])
        Stab = A.alloc("Stab", [96, 512])
        qT = A.alloc("qT", [96, 8, 512], BF16)
        kTt = A.alloc("kTt", [96, 8, 512], BF16)
        kper = A.alloc("kper", [96, 512])
        Vt = A.ring("Vt", 2, [128, 8, 128], BF16)
        for _vt in Vt.items:
            S.memset(_vt, 1.0, eng="pool")
        gen = Ring([psb[0], psb[1], psb[6], psb[7]])
        ps_ya = psb[2:6]

        for c in range(TOK // 512):
            tok0 = c * 512
            b = c // 4
            s0 = (c % 4) * 512
            self.load_xT(self.x, tok0, 4, xT, xtok, gen, self.ident)
            for h in range(4):
                ps = gen.next()
                for kt in range(KT):
                    S.mm(ps, w_in_b[:, kt, h * 128:(h + 1) * 128], xT[:, kt, :], start=(kt == 0), stop=(kt == KT - 1))
                S.act(uT[:, h, :], ps, AF.Gelu_apprx_tanh, join=(h > 0))
            vns = []
            for i in range(4):
                ps = gen.next()
                for kt in range(KT):
                    S.mm(ps, xT[:, kt, i * 128:(i + 1) * 128], w_in_b[:, kt, 512:1024], start=(kt == 0), stop=(kt == KT - 1))
                v = scr.next()
                S.act(v, ps, AF.Gelu_apprx_tanh)
                sm = small.next()
                st = sm[:, 0:6]
                S.add("dve", (lambda st, v: lambda e: e.bn_stats(st.ap, v.ap))(st, v), reads=[v], writes=[sm])
                sm2 = small.next()
                mv = sm2[:, 0:2]
                S.add("dve", (lambda mv, st: lambda e: e.bn_aggr(mv.ap, st.ap))(mv, st), reads=[sm], writes=[sm2])
                S.act(sm2[:, 2:3], sm2[:, 1:2], AF.Sqrt, bias=self.eps_ln, scale=1.0, join=True)
                S.recip(sm2[:, 3:4], sm2[:, 2:3], join=True)
                S.stt(sm2[:, 4:5], sm2[:, 0:1], -1.0, sm2[:, 3:4], ALU.mult, ALU.mult, join=True)
                vn0 = scr.next()
                S.act(vn0, v, AF.Identity, bias=sm2[:, 4:5], scale=sm2[:, 3:4])
                S.tt(vn0, vn0, gmg, ALU.mult, eng="pool")
                vn = vnr.next()
                S.tt(vn, vn0, gmb, ALU.add, eng="pool")
                vns.append(vn)
            ps_sq = ps_ya[0]
            ps_skv = ps_ya[1]
            for m in range(5):
                ps = gen.next()
                for kt in range(KT):
                    S.mm(ps, w_in_b[:, kt, 1024 + m * 128:1024 + (m + 1) * 128], xT[:, kt, :],
                         start=(kt == 0), stop=(kt == KT - 1))
                S.copy(cT[:, m, :], ps, eng="act", join=(m > 0))
                sq = scr.next()
                S.act(sq, ps, AF.Square)
                if m < 3:
                    S.mm(ps_sq, self.ones_f, sq, start=(m == 0), stop=(m == 2))
                else:
                    S.mm(ps_skv, self.ones_f, sq, start=(m == 3), stop=(m == 4))
            t = scr.next()
            S.act(t, ps_sq, AF.Sqrt, bias=self.eps_rms, scale=1.0 / 384.0)
            S.recip(Rq, t)
            t = scr.next()
            S.act(t, ps_skv, AF.Sqrt, bias=self.eps_rms, scale=1.0 / 256.0)
            S.recip(Rkv, t)
            for m in range(5):
                g = qn[:, m:m + 1] if m < 3 else kvn[:, m - 3:m - 2]
                S.stt(cqn[:, m, :], cT[:, m, :], g, Rq if m < 3 else Rkv, ALU.mult, ALU.mult, join=(m > 0))
            S.dma(posi, self.pos.v(self.pos.ap[b:b + 1, s0:s0 + 512].to_broadcast([96, 512])), q="sp")
            ang = scr.next()[0:96, :]
            S.copy(ang, posi)
            S.ts(ang, ang, self.ropef, ALU.mult)
            kk = scr.next()[0:96, :]
            S.ts(kk, ang, 1.0 / TWO_PI, ALU.mult, MAGIC, ALU.add)
            S.ts(kk, kk, MAGIC, ALU.subtract)
            r1 = scr.next()[0:96, :]
            S.stt(r1, kk, -C1, ang, ALU.mult, ALU.add)
            S.stt(r1, kk, -C2, r1, ALU.mult, ALU.add)
            S.ts(r1, r1, -PI_SAFE, ALU.max, PI_SAFE, ALU.min)
            S.act(Stab, r1, AF.Sin, scale=self.ropesign)
            S.stt(r1, r1, -1.0, r1, ALU.mult, ALU.max)
            S.act(Ctab, r1, AF.Sin, bias=self.halfpi[0:96, :], scale=-1.0)
            for h in range(8):
                ps = gen.next()
                ps2 = gen.next()
                for kt in range(3):
                    S.mm(ps[0:96, :], w_uq[:, kt, h * 96:(h + 1) * 96], cqn[:, kt, :], start=(kt == 0), stop=(kt == 2))
                for kt in range(3):
                    S.mm(ps2[0:96, :], w_uqs[:, kt, h * 96:(h + 1) * 96], cqn[:, kt, :], start=(kt == 0), stop=(kt == 2))
                t1 = scr.next()[0:96, :]
                S.tt(t1, ps[0:96, :], Ctab, ALU.mult)
                t2 = scr.next()[0:96, :]
                S.tt(t2, ps2[0:96, :], Stab, ALU.mult)
                S.tt(qT[:, h, :], t1, t2, ALU.add, eng="pool", join=(h > 0))
            S.dma(self.qT_d.v(self.qT_d.ap.rearrange("h d t -> d h t")[:, :, tok0:tok0 + 512]), qT, q="sp", join=True)
            ps = gen.next()
            ps2 = gen.next()
            for kt in range(KT):
                S.mm(ps[0:96, :], w_kpe[:, kt, 0:96], xT[:, kt, :], start=(kt == 0), stop=(kt == KT - 1))
            for kt in range(KT):
                S.mm(ps2[0:96, :], w_kpe[:, kt, 96:192], xT[:, kt, :], start=(kt == 0), stop=(kt == KT - 1))
            t1 = scr.next()[0:96, :]
            S.tt(t1, ps[0:96, :], Ctab, ALU.mult)
            t2 = scr.next()[0:96, :]
            S.tt(t2, ps2[0:96, :], Stab, ALU.mult)
            S.tt(kper, t1, t2, ALU.add, eng="pool")
            for h in range(8):
                ps = gen.next()
                for kt in range(2):
                    S.mm(ps[0:64, :], w_uk[:, kt, h * 64:(h + 1) * 64], cqn[:, 3 + kt, :], start=(kt == 0), stop=(kt == 1))
                S.copy(kTt[0:64, h, :], ps[0:64, :], eng="act", join=(h > 0))
                S.copy(kTt[64:96, h, :], kper[64:96, :], eng="pool", join=True)
            S.dma(self.kT_d.v(self.kT_d.ap.rearrange("h d t -> d h t")[:, :, tok0:tok0 + 512]), kTt, q="sp", join=True)
            for i in range(4):
                for h in range(4):
                    S.mm(ps_ya[h][:, i * 128:(i + 1) * 128], vns[i][:, h * 128:(h + 1) * 128], wsb[:, h, :],
                         start=True, stop=True, join=(i > 0))
            for h in range(4):
                t = scr.next()
                S.tt(t, ps_ya[h], bsb[:, h, :], ALU.add)
                S.tt(yaT[:, h, :], t, uT[:, h, :], ALU.mult, eng="pool", join=(h > 0))
            S.dma(self.yaT_d.v(self.yaT_d.ap.rearrange("(h p) t -> p h t", p=128)[:, :, tok0:tok0 + 512]), yaT,
                  q="sp", join=True)
            for i in range(4):
                ps = gen.next()
                for kt in range(2):
                    S.mm(ps, cqn[:, 3 + kt, i * 128:(i + 1) * 128], w_uv[:, kt, :], start=(kt == 0), stop=(kt == 1))
                vt = Vt.next()
                S.copy(vt[:, :, 0:64], ps.v(ps.ap.rearrange("p (h d) -> p h d", h=8)), eng="act", join=True)
                S.dma(self.V_d.v(self.V_d.ap[tok0 + i * 128:tok0 + (i + 1) * 128, :].rearrange("p (h d) -> p h d", h=8)),
                      vt, q="sp", join=True)

    def stage_A2(self):
        S, A, psb = self.S, self.A, self.psb
        w_oa = A.alloc("w_oa", [128, 4, D], BF16)
        S.dma(w_oa, self.w_out_a.v(self.w_out_a.ap.rearrange("(kt p) n -> p kt n", p=128)), q="pool")
        w_ob = A.alloc("w_ob", [128, 4, D], BF16)
        S.dma(w_ob, self.w_out_b.v(self.w_out_b.ap.rearrange("(kt p) n -> p kt n", p=128)), q="pool")
        lng = A.alloc("lng", [128, D])
        S.dma(lng, self.ab_ln_g, q="sp")
        lnb = A.alloc("lnb", [128, D])
        S.dma(lnb, self.ab_ln_b, q="sp")
        KTs = A.alloc("KTs", [96, 8, SEQ], BF16)
        Vs = A.alloc("Vs", [128, 16, 1024], BF16)
        qTc = A.ring("qTc", 2, [96, 8, 512], BF16)
        yaTc = A.ring("yaTc", 2, [128, 4, 512], BF16)
        ybT = A.alloc("ybT", [128, 4, 512], BF16)
        PT = A.ring("PT", 3, [128, 512], BF16)
        rec = A.ring("rec", 2, [64, 512])
        xres = A.ring("xres", 2, [128, D])
        z = A.ring("z", 2, [128, D])
        zn = A.ring("zn", 2, [128, D])
        xo = A.ring("xo", 2, [128, D])
        small = A.ring("smallA2", 4, [128, 24])
        ps_s = Ring([psb[0], psb[1], psb[2]])
        ps_nd = Ring([psb[3], psb[4], psb[5]])
        ps_o = Ring([psb[6], psb[7]])
        scale = 96.0 ** -0.5
        for b in range(NB):
            for h in range(8):
                S.dma(KTs[:, h, :], self.kT_d[h, :, b * SEQ:(b + 1) * SEQ], q="sp", join=(h > 0))
            S.dma(Vs, self.V_d.v(self.V_d.ap[b * SEQ:(b + 1) * SEQ, :].rearrange("(n p) c -> p n c", p=128)), q="sp")
            for cs in range(4):
                tok0 = b * SEQ + cs * 512
                q = qTc.next()
                S.dma(q, self.qT_d.v(self.qT_d.ap.rearrange("h d t -> d h t")[:, :, tok0:tok0 + 512]), q="sp")
                ya = yaTc.next()
                S.dma(ya, self.yaT_d.v(self.yaT_d.ap.rearrange("(h p) t -> p h t", p=128)[:, :, tok0:tok0 + 512]), q="sp")
                nkt = 4 * (cs + 1)
                for h in range(8):
                    pnum = ps_nd.next()

                    def qk(kt, h=h):
                        j = kt - 4 * cs
                        c0 = 0 if j < 0 else j * 128
                        ps = ps_s.next()
                        S.mm(ps[:, c0:512], KTs[:, h, kt * 128:(kt + 1) * 128], q[:, h, c0:512],
                             start=True, stop=(j < 0))
                        if j >= 0:
                            S.mm(ps[:, c0:c0 + 128], self.ident_bf, self.maskneg, start=False, stop=True)
                        pt = PT.next()
                        S.act(pt[:, c0:512], ps[:, c0:512], AF.Exp, scale=scale)
                        return pt, c0

                    def pv(kt, pt, c0, h=h, pnum=pnum):
                        S.mm(pnum[:, c0:512], Vs[:, kt, h * 128:(h + 1) * 128], pt[:, c0:512],
                             start=(kt == 0), stop=(kt == nkt - 1))

                    pend = [qk(0)]
                    for kt in range(nkt):
                        if kt + 1 < nkt:
                            pend.append(qk(kt + 1))
                        pt, c0 = pend.pop(0)
                        pv(kt, pt, c0)
                    r = rec.next()
                    S.recip(r, pnum[64:128, :])
                    po = (h % 2) * 64
                    S.tt(ybT[po:po + 64, h // 2, :], pnum[0:64, :], r, ALU.mult, join=(h > 0))
                for i in range(4):
                    xr = xres.next()
                    S.dma(xr, self.x[tok0 + i * 128:tok0 + (i + 1) * 128, :], q="sp")
                    pso = [ps_o.next(), ps_o.next()]
                    for hf in range(2):
                        for kk in range(4):
                            S.mm(pso[hf], ya[:, kk, i * 128:(i + 1) * 128], w_oa[:, kk, hf * 512:(hf + 1) * 512],
                                 start=(kk == 0), stop=False)
                        for hh in range(4):
                            S.mm(pso[hf], ybT[:, hh, i * 128:(i + 1) * 128], w_ob[:, hh, hf * 512:(hf + 1) * 512],
                                 start=False, stop=(hh == 3))
                    sm = small.next()
                    self.ln_epilogue(pso[0], pso[1], xr, lng, lnb, z.next(),
                                     sm.v(sm.ap[:, 0:12].rearrange("p (a b) -> p a b", a=2)), sm[:, 12:14], sm[:, 14:15],
                                     sm[:, 15:16], sm[:, 16:17], zn.next(), xo.next(),
                                     self.x1_d[tok0 + i * 128:tok0 + (i + 1) * 128, :], self.eps_ln)

    def ffn_stage(self, src_d, dst_d, units, lng_d, lnb_d, router=None):
        S, A, psb = self.S, self.A, self.psb
        NT = 8
        lng = A.alloc("lng", [128, D])
        S.dma(lng, lng_d, q="sp")
        lnb = A.alloc("lnb", [128, D])
        S.dma(lnb, lnb_d, q="sp")
        maxh = max(u[3] for u in units)
        xT = A.alloc("xT", [128, KT, NT * 128], BF16)
        hT = A.alloc("hT", [128, maxh, NT * 128], BF16)
        acc = A.alloc("acc", [128, NT, D])
        xtok = A.ring("xtok", 2, [128, D])
        wgr = A.ring("wg", 4, [128, KT, 256], BF16)
        wur = A.ring("wu", 4, [128, KT, 256], BF16)
        wd = A.alloc("wd", [128, maxh, D], BF16)
        sil = A.ring("sil", 3, [128, 512])
        z = A.ring("z", 2, [128, D])
        zn = A.ring("zn", 2, [128, D])
        small = A.ring("smallF", 4, [128, 24])
        ps_gu = Ring([psb[0], psb[1], psb[2], psb[3]])
        ps_dn = Ring([psb[4], psb[5], psb[6], psb[7]])
        if router is not None:
            rt = A.alloc("rt", [128, KT, N_EXP])
            S.dma(rt, router, q="sp")
            xTf = A.ring("xTf", 2, [128, KT, 128])
            comb = A.alloc("comb", [128, NT, N_EXP])
            rsm = A.ring("rsm", 4, [128, 32])
        for c in range(TOK // (NT * 128)):
            tok0 = c * NT * 128
            if router is None:
                self.load_xT(src_d, tok0, NT, xT, xtok, ps_dn, self.ident)
            else:
                cur = {}

                def cb(i, half, srcv, ev, cur=cur):
                    if half == 0:
                        cur["t"] = xTf.next()
                    S.copy(cur["t"][:, half * 4:half * 4 + 4, :], srcv, eng=ev, join=(half == 1))
                    lvl = int(getattr(self, "rlevel", 4))
                    if half == 1 and lvl == 0:
                        S.memset(comb[:, i, :], 0.125, join=True)
                        return
                    if half == 1:
                        xf = cur["t"]
                        ps = ps_gu.next()
                        for kt in range(KT):
                            S.mm(ps[:, 0:N_EXP], xf[:, kt, :], rt[:, kt, :], start=(kt == 0), stop=(kt == KT - 1))
                        r = rsm.next()
                        lg = r[:, 0:8]
                        S.copy(lg, ps[:, 0:N_EXP])
                        lvl = int(getattr(self, "rlevel", 4))
                        if lvl < 4:
                            S.memset(comb[:, i, :], 0.125, join=True)
                        if lvl < 2:
                            return
                        mx = r[:, 8:16]
                        S.add("dve", (lambda mx, lg: lambda e: e.max(out=mx.ap, in_=lg.ap))(mx, lg), reads=[r], writes=[r], join=True)
                        if lvl < 3:
                            return
                        dd = r[:, 16:17]
                        S.tt(dd, r[:, 9:10], r[:, 8:9], ALU.subtract, join=True)
                        S.act(r[:, 17:18], dd, AF.Exp, join=True)
                        S.ts(r[:, 18:19], r[:, 17:18], 1.0, ALU.add, join=True)
                        S.recip(r[:, 19:20], r[:, 18:19], join=True)
                        S.ts(r[:, 20:21], r[:, 19:20], -1.0, ALU.mult, 1.0, ALU.add, join=True)
                        if lvl < 4:
                            return
                        r2 = rsm.next()
                        S.ts(r2[:, 0:8], lg, r[:, 8:9], ALU.is_equal)
                        S.ts(r2[:, 8:16], lg, r[:, 9:10], ALU.is_equal, join=True)
                        S.ts(r2[:, 16:24], lg, r[:, 8:9], ALU.is_lt, join=True)
                        S.tt(r2[:, 8:16], r2[:, 8:16], r2[:, 16:24], ALU.mult, join=True)
                        S.ts(r2[:, 0:8], r2[:, 0:8], r[:, 19:20], ALU.mult, join=True)
                        S.stt(comb[:, i, :], r2[:, 8:16], r[:, 20:21], r2[:, 0:8], ALU.mult, ALU.add, join=True)

                self.load_xT(src_d, tok0, NT, xT, xtok, ps_dn, self.ident, xTf_cb=cb)
            for ui, (wg_ap, wu_ap, wd_ap, nh, ex) in enumerate(units):
                wdv = wd_ap.rearrange("(j p) n -> p j n", p=128)

                def load_wd(wdv=wdv, nh=nh):
                    nsplit = 2
                    per = (nh + nsplit - 1) // nsplit
                    for sidx in range(nsplit):
                        ja, jb = sidx * per, min(nh, (sidx + 1) * per)
                        S.dma(wd[:, ja:jb, :], T(self.wsrc, wdv[:, ja:jb, :]), q="pool", join=(sidx > 0))
                wgv = wg_ap.rearrange("(kt p) n -> p kt n", p=128)
                wuv = wu_ap.rearrange("(kt p) n -> p kt n", p=128)
                for j0 in range(0, nh, 2):
                    nj = min(2, nh - j0)
                    wgb = wgr.next()
                    wub = wur.next()
                    S.dma(wgb[:, :, 0:nj * 128], T(self.wsrc, wgv[:, :, j0 * 128:(j0 + nj) * 128]), q="pool")
                    S.dma(wub[:, :, 0:nj * 128], T(self.wsrc, wuv[:, :, j0 * 128:(j0 + nj) * 128]), q="pool")
                    if j0 == 6:
                        load_wd()
                    for jj in range(nj):
                        j = j0 + jj
                        for blk in range(NT // 4):
                            pg = ps_gu.next()
                            pu = ps_gu.next()
                            for kt in range(KT):
                                S.mm(pg, wgb[:, kt, jj * 128:(jj + 1) * 128], xT[:, kt, blk * 512:(blk + 1) * 512],
                                     start=(kt == 0), stop=(kt == KT - 1))
                            for kt in range(KT):
                                S.mm(pu, wub[:, kt, jj * 128:(jj + 1) * 128], xT[:, kt, blk * 512:(blk + 1) * 512],
                                     start=(kt == 0), stop=(kt == KT - 1))
                            sl = sil.next()
                            S.act(sl, pg, AF.Silu)
                            S.tt(hT[:, j, blk * 512:(blk + 1) * 512], pu, sl, ALU.mult, join=not (j == 0 and blk == 0))
                for i in range(NT):
                    for hf in range(2):
                        pd = ps_dn.next()
                        for j in range(nh):
                            S.mm(pd, hT[:, j, i * 128:(i + 1) * 128], wd[:, j, hf * 512:(hf + 1) * 512],
                                 start=(j == 0), stop=(j == nh - 1))
                        dst = acc[:, i, hf * 512:(hf + 1) * 512]
                        first = (ui == 0)
                        if ex is None or getattr(self, "noscale", False):
                            if first:
                                S.copy(dst, pd, eng="act", join=not (i == 0 and hf == 0))
                            else:
                                S.tt(dst, pd, dst, ALU.add, join=True)
                        else:
                            cw = comb[:, i, ex:ex + 1]
                            if first:
                                S.act(dst, pd, AF.Identity, scale=cw, join=not (i == 0 and hf == 0))
                            else:
                                S.stt(dst, pd, cw, dst, ALU.mult, ALU.add, join=True)
            for i in range(NT):
                xr = xtok.next()
                S.dma(xr, src_d[tok0 + i * 128:tok0 + (i + 1) * 128, :], q="sp")
                sm = small.next()
                znt = zn.next()
                self.ln_epilogue(acc[:, i, 0:512], acc[:, i, 512:1024], xr, lng, lnb, z.next(),
                                 sm.v(sm.ap[:, 0:12].rearrange("p (a b) -> p a b", a=2)), sm[:, 12:14], sm[:, 14:15],
                                 sm[:, 15:16], sm[:, 16:17], znt, znt,
                                 dst_d[tok0 + i * 128:tok0 + (i + 1) * 128, :], self.eps_ln)

    def stage_B(self):
        self.wsrc = Buf("wsrc", dram=True)
        units = []
        half = D_FF // 2
        for u in range(2):
            units.append((self.ffd_wg.ap[:, u * half:(u + 1) * half], self.ffd_wu.ap[:, u * half:(u + 1) * half],
                          self.ffd_wd.ap[u * half:(u + 1) * half, :], half // 128, None))
        self.ffn_stage(self.x1_d, self.x2_d, units, self.ffd_ln_g, self.ffd_ln_b)

    def stage_D(self):
        self.wsrc = Buf("wsrc", dram=True)
        units = []
        half = D_EXP // 2
        for e in range(N_EXP):
            for u in range(2):
                units.append((self.moe_wg.ap[e, :, u * half:(u + 1) * half], self.moe_wu.ap[e, :, u * half:(u + 1) * half],
                              self.moe_wd.ap[e, u * half:(u + 1) * half, :], half // 128, e))
        mode = getattr(self, "mode", "full")
        if mode == "nr1":
            units = [(a, b, c, n, None) for (a, b, c, n, e) in units[:2]]
            self.ffn_stage(self.x3_d, self.out, units, self.moe_ln_g, self.moe_ln_b)
            return
        if mode == "norouter":
            units = [(a, b, c, n, None) for (a, b, c, n, e) in units]
            self.ffn_stage(self.x3_d, self.out, units, self.moe_ln_g, self.moe_ln_b)
            return
        if mode == "two":
            units = units[:4]
        self.ffn_stage(self.x3_d, self.out, units, self.moe_ln_g, self.moe_ln_b, router=self.router)

    def stage_Ca(self):
        S, A, psb = self.S, self.A, self.psb
        w = A.alloc("cwin", [128, KT, D], BF16)
        S.dma(w, self.c_w_in.v(self.c_w_in.ap.rearrange("(kt p) n -> p kt n", p=128)), q="pool")
        xtok = A.ring("xtok", 2, [128, D])
        xT = A.alloc("xT", [128, KT, 512], BF16)
        uT = A.ring("uTo", 2, [128, KT, 512])
        gen = Ring(psb)
        for c in range(TOK // 512):
            tok0 = c * 512
            self.load_xT(self.x2_d, tok0, 4, xT, xtok, gen, self.ident)
            u = uT.next()
            for m in range(KT):
                ps = gen.next()
                for kt in range(KT):
                    S.mm(ps, w[:, kt, m * 128:(m + 1) * 128], xT[:, kt, :], start=(kt == 0), stop=(kt == KT - 1))
                S.copy(u[:, m, :], ps, eng="act" if m % 2 else "dve", join=(m > 0))
            S.dma(self.uT_d.v(self.uT_d.ap.rearrange("(m p) t -> p m t", p=128)[:, :, tok0:tok0 + 512]), u, q="sp", join=True)

    s5e = ("dve", "dve", "dve", "dve", "dve", "dve", "pool")

    def stage_Cb(self):
        S, A, psb = self.S, self.A, self.psb
        NM = 32
        are = A.alloc("are", [128, NM]); S.dma(are, self.s5_are, q="sp")
        aim = A.alloc("aim", [128, NM]); S.dma(aim, self.s5_aim, q="sp")
        lst = A.alloc("lst", [128, NM]); S.dma(lst, self.s5_ls, q="sp")
        dsk = A.alloc("dsk", [128, 8]); S.dma(dsk, self.s5_d, q="sp")
        BTr = A.alloc("BTr", [128, 8, 4, 128], BF16)
        BTi = A.alloc("BTi", [128, 8, 4, 128], BF16)
        CTr = A.alloc("CTr", [128, 8, 4, 128], BF16)
        CTi = A.alloc("CTi", [128, 8, 4, 128], BF16)
        for (dst, src, k) in ((BTr, self.s5_BT, 0), (BTi, self.s5_BT, 1), (CTr, self.s5_CT, 0), (CTi, self.s5_CT, 1)):
            S.dma(dst, src.v(src.ap[k].rearrange("kt p m s -> p kt m s")), q="pool")
        S.ts(CTi, CTi, -1.0, ALU.mult, eng="pool")
        CTrn = A.alloc("CTrn", [128, 8, 4, 128], BF16)
        S.ts(CTrn, CTr, -1.0, ALU.mult, eng="pool")
        mask = A.alloc("smask", [128, 512]); S.dma(mask, self.scanmask, q="sp")
        sc = A.alloc("s5sc", [128, 16, NM])
        dl = sc[:, 0, :]
        S.act(dl, lst, AF.Exp)
        rho = sc[:, 1, :]; S.tt(rho, dl, are, ALU.mult)
        th = sc[:, 2, :]; S.tt(th, dl, aim, ALU.mult)
        Einr = A.alloc("Einr", [128, NM, 128]); Eini = A.alloc("Eini", [128, NM, 128])
        Eoutr = A.alloc("Eoutr", [128, NM, 128]); Eouti = A.alloc("Eouti", [128, NM, 128])
        tm = A.mark()
        jg = self.jgrid
        jg3 = jg.v(jg.ap[:, None, :].to_broadcast([128, NM, 128]))

        def bc(t):
            return t.v(t.ap[:, :, None].to_broadcast([128, NM, 128]))
        ang = A.alloc("ang", [128, NM, 128])
        S.tt(ang, jg3, bc(th), ALU.mult)
        kk = A.alloc("kk", [128, NM, 128])
        S.ts(kk, ang, 1.0 / TWO_PI, ALU.mult, MAGIC, ALU.add)
        S.ts(kk, kk, MAGIC, ALU.subtract)
        r1 = A.alloc("r1", [128, NM, 128])
        S.stt(r1, kk, -C1, ang, ALU.mult, ALU.add)
        S.stt(r1, kk, -C2, r1, ALU.mult, ALU.add)
        S.ts(r1, r1, -PI_SAFE, ALU.max, PI_SAFE, ALU.min)
        sn = ang
        S.act(sn, r1, AF.Sin)
        S.stt(r1, r1, -1.0, r1, ALU.mult, ALU.max)
        cs_ = kk
        S.act(cs_, r1, AF.Sin, bias=self.halfpi, scale=-1.0)
        lm = r1
        S.tt(lm, jg3, bc(rho), ALU.mult)
        mo = A.alloc("mo", [128, NM, 128])
        S.act(mo, lm, AF.Exp)
        S.tt(Eoutr, mo, cs_, ALU.mult)
        S.tt(Eouti, mo, sn, ALU.mult)
        S.act(mo, lm, AF.Exp, scale=-1.0)
        er = lm
        S.tt(er, mo, cs_, ALU.mult)
        ei = A.alloc("ei", [128, NM, 128])
        S.tt(ei, mo, sn, ALU.mult)
        S.ts(ei, ei, -1.0, ALU.mult)
        abr = sc[:, 3, :]; S.copy(abr, Eoutr[:, :, 1])
        abi = sc[:, 4, :]; S.copy(abi, Eouti[:, :, 1])
        am1 = sc[:, 5, :]; S.ts(am1, abr, -1.0, ALU.add)
        den = sc[:, 6, :]; S.tt(den, are, are, ALU.mult)
        t0 = sc[:, 7, :]; S.tt(t0, aim, aim, ALU.mult)
        S.tt(den, den, t0, ALU.add)
        rden = sc[:, 8, :]; S.recip(rden, den)
        cre = sc[:, 9, :]; S.tt(cre, am1, are, ALU.mult)
        S.tt(t0, abi, aim, ALU.mult); S.tt(cre, cre, t0, ALU.add); S.tt(cre, cre, rden, ALU.mult)
        cim = sc[:, 10, :]; S.tt(cim, abi, are, ALU.mult)
        S.tt(t0, am1, aim, ALU.mult); S.tt(cim, cim, t0, ALU.subtract); S.tt(cim, cim, rden, ALU.mult)
        S.tt(Einr, er, bc(cre), ALU.mult)
        S.tt(mo, ei, bc(cim), ALU.mult)
        S.tt(Einr, Einr, mo, ALU.subtract)
        S.tt(Eini, er, bc(cim), ALU.mult)
        S.tt(mo, ei, bc(cre), ALU.mult)
        S.tt(Eini, Eini, mo, ALU.add)
        Gr = sc[:, 11, :]; Gi = sc[:, 12, :]
        t1 = sc[:, 13, :]
        S.tt(Gr, Eoutr[:, :, 127], abr, ALU.mult)
        S.tt(t1, Eouti[:, :, 127], abi, ALU.mult)
        S.tt(Gr, Gr, t1, ALU.subtract)
        S.tt(Gi, Eoutr[:, :, 127], abi, ALU.mult)
        S.tt(t1, Eouti[:, :, 127], abr, ALU.mult)
        S.tt(Gi, Gi, t1, ALU.add)
        S.barrier()
        A.reset(tm)
        uld = A.ring("uld", 2, [128, 512])
        uf2 = A.ring("uf2", 2, [128, 512])
        ub = A.ring("ub", 8, [128, 512], BF16)
        ringB = A.ring("rB", 24, [128, 4, 128], BF16)
        ringF = A.ring("rF", 12, [128, 4, 128])
        Hc = [[A.alloc(f"Hc{b}_{kt}", [128, 2, 4]) for kt in range(KT)] for b in range(NB)]
        cin = A.ring("cin", 8, [128, 4, 4])
        yv = A.ring("yv", 2, [128, 512])
        go = A.ring("go", 2, [128, 512], BF16)
        ps_bu = Ring([psb[0], psb[1], psb[2], psb[3]])
        ps_y = Ring([psb[4], psb[5], psb[6], psb[7]])
        uTv = self.uT_d.ap.rearrange("(m p) t -> p m t", p=128)
        gTv = self.gT_d.ap.rearrange("(m p) t -> p m t", p=128)
        for b in range(NB):
            for kt in range(KT):
                S.memset(Hc[b][kt], 0.0, eng="pool")

        def st1(it):
            ch = it["ch"]; kt = ch["kt"]; k = it["k"]; m0 = kt * 4
            if k == 0:
                ul = uld.next()
                S.dma(ul, self.uT_d.v(uTv[:, kt, ch["tok0"]:ch["tok0"] + 512]), q="sp")
                ch["ub"] = ub.next()
                S.copy(ch["ub"], ul, eng="act")
            u_b = ch["ub"]
            pr = ps_bu.next(); pi = ps_bu.next()
            for ml in range(4):
                S.mm(pr[:, ml * 128:(ml + 1) * 128], BTr[:, kt, ml, :], u_b[:, k * 128:(k + 1) * 128],
                     start=True, stop=True, join=(ml > 0))
            for ml in range(4):
                S.mm(pi[:, ml * 128:(ml + 1) * 128], BTi[:, kt, ml, :], u_b[:, k * 128:(k + 1) * 128],
                     start=True, stop=True, join=(ml > 0))
            pr3 = pr.v(pr.ap.rearrange("p (a b) -> p a b", a=4))
            pi3 = pi.v(pi.ap.rearrange("p (a b) -> p a b", a=4))
            eir = Einr[:, m0:m0 + 4, :]; eii = Eini[:, m0:m0 + 4, :]
            a = [ringB.next() for _ in range(4)]
            S.tt(a[0], pr3, eir, ALU.mult)
            S.tt(a[1], pi3, eii, ALU.mult)
            S.tt(a[2], pi3, eir, ALU.mult)
            S.tt(a[3], pr3, eii, ALU.mult)
            it["a"] = a

        def st2(it):
            a = it["a"]
            kr = ringF.next(); ki = ringF.next()
            S.tt(kr, a[0], a[1], ALU.subtract, eng=self.s5e[0])
            S.tt(ki, a[2], a[3], ALU.add, eng=self.s5e[1])
            it["kr"] = kr; it["ki"] = ki

        def st3(it):
            ch = it["ch"]; kt = ch["kt"]; m0 = kt * 4
            kr = it["kr"]; ki = it["ki"]
            H = Hc[ch["b"]][kt]
            Hr = H[:, 0, :]; Hi = H[:, 1, :]
            g_r = Gr[:, m0:m0 + 4]; g_i = Gi[:, m0:m0 + 4]
            ci = cin.next()
            te = self.s5e[6]
            S.tt(ci[:, 0, :], Hr, g_r, ALU.mult, eng=te)
            S.tt(ci[:, 1, :], Hi, g_i, ALU.mult, join=True, eng=te)
            S.tt(ci[:, 2, :], Hr, g_i, ALU.mult, join=True, eng=te)
            S.tt(ci[:, 3, :], Hi, g_r, ALU.mult, join=True, eng=te)
            S.tt(ci[:, 0, :], ci[:, 0, :], ci[:, 1, :], ALU.subtract, join=True, eng=te)
            S.tt(ci[:, 2, :], ci[:, 2, :], ci[:, 3, :], ALU.add, join=True, eng=te)
            S.tt(kr[:, :, 0], kr[:, :, 0], ci[:, 0, :], ALU.add, join=True, eng=te)
            S.tt(ki[:, :, 0], ki[:, :, 0], ci[:, 2, :], ALU.add, join=True, eng=te)
            hr_t = ringF.next(); hi_t = ringF.next()
            for (src, dst) in ((kr, hr_t), (ki, hi_t)):
                sf = src.ap.rearrange("p a b -> p (a b)")
                df = dst.ap.rearrange("p a b -> p (a b)")
                S.add("dve", (lambda df, sf: lambda e: e.tensor_tensor_scan(df, mask.ap, sf, 0.0, ALU.mult, ALU.add))(df, sf),
                      reads=[mask, src], writes=[dst])
            S.copy(H[:, 0, :], hr_t[:, :, 127], eng="act")
            S.copy(H[:, 1, :], hi_t[:, :, 127], eng="act", join=True)
            it["hr_t"] = hr_t; it["hi_t"] = hi_t

        def st4(it):
            kt = it["ch"]["kt"]; m0 = kt * 4
            eor = Eoutr[:, m0:m0 + 4, :]; eoi = Eouti[:, m0:m0 + 4, :]
            hr_t = it["hr_t"]; hi_t = it["hi_t"]
            bb = [ringB.next() for _ in range(4)]
            S.tt(bb[0], hr_t, eor, ALU.mult, eng=self.s5e[2])
            S.tt(bb[1], hi_t, eoi, ALU.mult, eng=self.s5e[3])
            S.tt(bb[2], hr_t, eoi, ALU.mult, eng=self.s5e[4])
            S.tt(bb[3], hi_t, eor, ALU.mult, eng=self.s5e[5])
            it["bb"] = bb

        def st5(it):
            ch = it["ch"]; kt = ch["kt"]; k = it["k"]
            bb = it["bb"]
            if k == 0:
                ch["py"] = ps_y.next()
            py = ch["py"]
            for wi, (W, P_) in enumerate(((CTr, bb[0]), (CTrn, bb[1]), (CTi, bb[2]), (CTi, bb[3]))):
                for ml in range(4):
                    S.mm(py[:, k * 128:(k + 1) * 128], W[:, kt, ml, :], P_[:, ml, :],
                         start=(wi == 0 and ml == 0), stop=(wi == 3 and ml == 3),
                         join=not (k == 0 and wi == 0 and ml == 0))
            if k == 3:
                u_f = uf2.next()
                S.dma(u_f, self.uT_d.v(uTv[:, kt, ch["tok0"]:ch["tok0"] + 512]), q="sp")
                y = yv.next()
                S.stt(y, u_f, dsk[:, kt:kt + 1], py, ALU.mult, ALU.add)
                g = go.next()
                S.act(g, y, AF.Gelu_apprx_tanh)
                S.dma(self.gT_d.v(gTv[:, kt, ch["tok0"]:ch["tok0"] + 512]), g, q="sp", join=True)

        its = []
        for b in range(NB):
            for blk in range(SEQ // 512):
                tok0 = b * SEQ + blk * 512
                for kq in range(0, KT, 4):
                    chains = [{"kt": kt, "b": b, "tok0": tok0} for kt in range(kq, kq + 4)]
                    for k in range(4):
                        for ch in chains:
                            its.append({"ch": ch, "k": k})
        stages = (st1, st2, st3, st4, st5)
        for t in range(len(its) + 4):
            for si in range(5):
                n = t - si
                if 0 <= n < len(its):
                    stages[si](its[n])

    def stage_Cc(self):
        S, A, psb = self.S, self.A, self.psb
        wg = A.alloc("gluw", [128, KT, D], BF16)
        S.dma(wg, self.glu_w.v(self.glu_w.ap.rearrange("(kt p) n -> p kt n", p=128)), q="pool")
        wo = A.alloc("cwout", [128, KT, D], BF16)
        S.dma(wo, self.c_w_out.v(self.c_w_out.ap.rearrange("(kt p) n -> p kt n", p=128)), q="pool")
        gb = A.alloc("glub", [128, 8]); S.dma(gb, self.glu_b, q="sp")
        lng = A.alloc("lng", [128, D]); S.dma(lng, self.c_ln_g, q="sp")
        lnb = A.alloc("lnb", [128, D]); S.dma(lnb, self.c_ln_b, q="sp")
        gTr = A.ring("gTc", 2, [128, KT, 512], BF16)
        zT = A.alloc("zT", [128, KT, 512], BF16)
        sg = A.ring("sg", 2, [128, 512])
        xres = A.ring("xres", 2, [128, D])
        z = A.ring("z", 2, [128, D]); zn = A.ring("zn", 2, [128, D])
        small = A.ring("smallC", 4, [128, 24])
        gen = Ring([psb[0], psb[1], psb[2], psb[3]])
        ps_o = Ring([psb[4], psb[5], psb[6], psb[7]])
        gTv = self.gT_d.ap.rearrange("(m p) t -> p m t", p=128)
        for c in range(TOK // 512):
            tok0 = c * 512
            g = gTr.next()
            S.dma(g, self.gT_d.v(gTv[:, :, tok0:tok0 + 512]), q="sp")
            for n in range(KT):
                ps = gen.next()
                for kt in range(KT):
                    S.mm(ps, wg[:, kt, n * 128:(n + 1) * 128], g[:, kt, :], start=(kt == 0), stop=(kt == KT - 1))
                s = sg.next()
                S.act(s, ps, AF.Sigmoid, bias=gb[:, n:n + 1], scale=1.0)
                S.tt(zT[:, n, :], g[:, n, :], s, ALU.mult, join=(n > 0))
            for i in range(4):
                xr = xres.next()
                S.dma(xr, self.x2_d[tok0 + i * 128:tok0 + (i + 1) * 128, :], q="sp")
                pso = [ps_o.next(), ps_o.next()]
                for hf in range(2):
                    for kt in range(KT):
                        S.mm(pso[hf], zT[:, kt, i * 128:(i + 1) * 128], wo[:, kt, hf * 512:(hf + 1) * 512],
                             start=(kt == 0), stop=(kt == KT - 1))
                sm = small.next()
                znt = zn.next()
                self.ln_epilogue(pso[0], pso[1], xr, lng, lnb, z.next(),
                                 sm.v(sm.ap[:, 0:12].rearrange("p (a b) -> p a b", a=2)), sm[:, 12:14], sm[:, 14:15],
                                 sm[:, 15:16], sm[:, 16:17], znt, znt,
                                 self.x3_d[tok0 + i * 128:tok0 + (i + 1) * 128, :], self.eps_ln)


def nc_out(nc, name, shape, dtype):
    return nc.dram_tensor(name, list(shape), dtype, kind="ExternalOutput").ap()


def _consts():
    c = np.zeros((128, 1024), np.float32)
    c[:, 0:128] = np.eye(128, dtype=np.float32)
    s = np.arange(128)[:, None]
    t = np.arange(128)[None, :]
    c[:, 128:256] = (s <= t).astype(np.float32)
    c[:, 256:384] = np.where(s <= t, 0.0, -30000.0).astype(np.float32)
    inv_freq = (1.0 / (np.float32(10000.0) ** (np.arange(0, 32, 2, dtype=np.float32) / np.float32(32)))).astype(np.float32)
    c[64:80, 384] = inv_freq
    c[80:96, 384] = inv_freq
    c[64:80, 385] = -1.0
    c[80:96, 385] = 1.0
    c[:, 386] = np.float32(math.pi / 2)
    c[:, 387] = LN_EPS
    c[:, 388] = RMS_EPS
    c[:, 512:640] = np.arange(128, dtype=np.float32)[None, :]
    m = np.ones((128, 512), np.float32)
    m[:, 0::128] = 0.0
    return c, m


def _bcast(v, n=128):
    return np.ascontiguousarray(np.broadcast_to(np.asarray(v, np.float32)[None, :], (n, v.shape[0])))


def _cols(v, k):
    return np.ascontiguousarray(np.asarray(v, np.float32).reshape(k, 128).T)


def prepare_shared(inp):
    f = lambda a: np.ascontiguousarray(np.asarray(a, np.float32))
    sh = {}
    cst, smask = _consts()
    sh["cst"] = cst
    sh["scanmask"] = smask
    w_in = f(inp["ab_w_in"][0])
    sh["w_in_main"] = np.ascontiguousarray(w_in[:, :1664])
    kp = w_in[:, 1664:1696]
    k2 = np.zeros((D, 192), np.float32)
    k2[:, 64:96] = kp
    k2[:, 96 + 64:96 + 80] = kp[:, 16:32]
    k2[:, 96 + 80:96 + 96] = kp[:, 0:16]
    sh["w_kpe2"] = k2
    sh["gm_ln_g_b"] = _bcast(inp["gm_ln_g"][0])
    sh["gm_ln_b_b"] = _bcast(inp["gm_ln_b"][0])
    ws = f(inp["gm_w_s"][0])
    sh["wsT"] = np.ascontiguousarray(ws.transpose(2, 0, 1))
    bs = f(inp["gm_b_s"][0])
    sh["bs_b"] = np.ascontiguousarray(np.broadcast_to(np.tile(bs, (1, 4))[None], (128, 4, 512)))
    sh["qnorm_c"] = _cols(inp["mla_q_norm"][0], 3)
    sh["kvnorm_c"] = _cols(inp["mla_kv_norm"][0], 2)
    wuq = f(inp["mla_w_uq"][0])
    sh["w_uq"] = wuq
    w3 = wuq.reshape(384, 8, 96)
    sw = np.zeros_like(w3)
    sw[:, :, 64:80] = w3[:, :, 80:96]
    sw[:, :, 80:96] = w3[:, :, 64:80]
    sh["w_uq_sw"] = np.ascontiguousarray(sw.reshape(384, 768))
    wukv = f(inp["mla_w_ukv"][0]).reshape(256, 8, 128)
    sh["w_uk"] = np.ascontiguousarray(wukv[:, :, 0:64].reshape(256, 512))
    sh["w_uv"] = np.ascontiguousarray(wukv[:, :, 64:128].reshape(256, 512))
    wout = f(inp["ab_w_out"][0])
    sh["w_out_a"] = np.ascontiguousarray(wout[0:512])
    sh["w_out_b"] = np.ascontiguousarray(wout[512:1024])
    sh["ab_ln_g_b"] = _bcast(inp["ab_ln_g"][0])
    sh["ab_ln_b_b"] = _bcast(inp["ab_ln_b"][0])
    sh["ffd_w_gate"] = f(inp["ffd_w_gate"][0])
    sh["ffd_w_up"] = f(inp["ffd_w_up"][0])
    sh["ffd_w_down"] = f(inp["ffd_w_down"][0])
    sh["ffd_ln_g_b"] = _bcast(inp["ffd_ln_g"][0])
    sh["ffd_ln_b_b"] = _bcast(inp["ffd_ln_b"][0])
    sh["c_w_in"] = f(inp["c_w_in"][0])

    def sm(a):
        a = f(a).reshape(32, 2, 64)
        return np.ascontiguousarray(a.transpose(1, 2, 0).reshape(128, 32))
    sh["s5_are_sm"] = sm(inp["s5_a_re"][0])
    sh["s5_aim_sm"] = sm(inp["s5_a_im"][0])
    ls = f(inp["s5_log_step"][0])
    sh["s5_ls_sm"] = sm(np.broadcast_to(ls[:, None], (64, 64)))
    BT = np.zeros((2, 8, 128, 4, 128), np.float32)
    CT = np.zeros((2, 8, 128, 4, 128), np.float32)
    for k, (Bk, Ck) in enumerate(((inp["s5_b_re"][0], inp["s5_c_re"][0]), (inp["s5_b_im"][0], inp["s5_c_im"][0]))):
        Bk = f(Bk)
        Ck = f(Ck)
        for g in range(64):
            kt, gl = divmod(g, 8)
            ml, two = divmod(gl, 2)
            BT[k, kt, gl * 16:(gl + 1) * 16, ml, two * 64:(two + 1) * 64] = Bk[g].T
            CT[k, kt, two * 64:(two + 1) * 64, ml, gl * 16:(gl + 1) * 16] = Ck[g].T
    sh["s5_BT"] = BT
    sh["s5_CT"] = CT
    sh["s5_d_c"] = _cols(inp["s5_d"][0], 8)
    sh["glu_w"] = f(inp["glu_w"][0])
    sh["glu_b_c"] = _cols(inp["glu_b"][0], 8)
    sh["c_w_out"] = f(inp["c_w_out"][0])
    sh["c_ln_g_b"] = _bcast(inp["c_ln_g"][0])
    sh["c_ln_b_b"] = _bcast(inp["c_ln_b"][0])
    sh["moe_router_c"] = np.ascontiguousarray(f(inp["moe_router"][0]).reshape(KT, 128, N_EXP).transpose(1, 0, 2))
    sh["moe_w_gate"] = f(inp["moe_w_gate"][0])
    sh["moe_w_up"] = f(inp["moe_w_up"][0])
    sh["moe_w_down"] = f(inp["moe_w_down"][0])
    sh["moe_ln_g_b"] = _bcast(inp["moe_ln_g"][0])
    sh["moe_ln_b_b"] = _bcast(inp["moe_ln_b"][0])
    return sh


_PROG_CACHE = {}


def get_prog(debug=False, upto="D"):
    key = (debug, upto)
    if key not in _PROG_CACHE:
        p = Prog(debug=debug, upto=upto)
        p.build()
        _PROG_CACHE[key] = p
    return _PROG_CACHE[key]


def run(inputs, debug=False, upto="D", cores=N_CORES):
    p = get_prog(debug, upto)
    sh = prepare_shared(inputs)
    x = np.asarray(inputs["x"], np.float32)
    pos = np.asarray(inputs["positions"], np.int32)
    in_maps = []
    for c in range(cores):
        m = {k: v for k, v in sh.items() if k in p.inputs}
        m["x"] = np.ascontiguousarray(x[c * NB:(c + 1) * NB].reshape(TOK, D))
        m["positions"] = np.ascontiguousarray(pos[c * NB:(c + 1) * NB])
        in_maps.append(m)
    res = run_bass_kernel_spmd(p.nc, in_maps, core_ids=list(range(cores)))
    return res


def kernel(**inputs):
    res = run(inputs)
    out = np.stack([np.asarray(r["out"], np.float32).reshape(NB, SEQ, D) for r in res.results], axis=0)
    return out.reshape(N_CORES * NB, SEQ, D)
```
